# Optimizing a Trainium2 kernel written in Bass

```python
import math
import jax, jax.numpy as jnp
from jax import lax
import numpy as np

D_MODEL = 1024
BATCH = 4
SEQ = 4096
DEPTH = 1

HEAD_DIM = 64
DSA_WIDTH = D_MODEL // 2
DIFF_WIDTH = D_MODEL - DSA_WIDTH
N_DSA_HEADS = DSA_WIDTH // HEAD_DIM
DIFF_V_DIM = 2 * HEAD_DIM
N_DIFF_HEADS = DIFF_WIDTH // DIFF_V_DIM
N_IDX_HEADS = 8
IDX_DIM = 64
TOPK_MAX = 256
D_FF = 4 * D_MODEL
Q_BLOCK = 128
ROPE_THETA = 10000.0
NORM_EPS = 1e-6
LN_EPS = 1e-5

SPLIT_SIZES = (
    DSA_WIDTH,
    DSA_WIDTH,
    DSA_WIDTH,
    N_IDX_HEADS * IDX_DIM,
    IDX_DIM,
    N_IDX_HEADS,
    2 * N_DIFF_HEADS * HEAD_DIM,
    2 * N_DIFF_HEADS * HEAD_DIM,
    DIFF_WIDTH,
)
IN_WIDTH = sum(SPLIT_SIZES)
SPLIT_POINTS = tuple(int(p) for p in np.cumsum(SPLIT_SIZES)[:-1])

kernel_name = "hymba_dsa_diffattn_adaln_layer"


def lambda_init(layer_idx):
    return 0.8 - 0.6 * math.exp(-0.3 * layer_idx)


def rms_norm(x, gain):
    x32 = x.astype(jnp.float32)
    y = x32 * lax.rsqrt(jnp.mean(jnp.square(x32), axis=-1, keepdims=True) + NORM_EPS)
    return (y * gain.astype(jnp.float32)).astype(x.dtype)


def layer_norm(x, w, b):
    x32 = x.astype(jnp.float32)
    mu = jnp.mean(x32, axis=-1, keepdims=True)
    var = jnp.mean(jnp.square(x32 - mu), axis=-1, keepdims=True)
    y = (x32 - mu) * lax.rsqrt(var + LN_EPS) * w.astype(jnp.float32) + b.astype(jnp.float32)
    return y.astype(x.dtype)


def modulate(h, shift, scale):
    return h * (1.0 + scale[:, None, :]) + shift[:, None, :]


def rope(x, positions):
    d = x.shape[-1]
    half = d // 2
    inv_freq = ROPE_THETA ** (-jnp.arange(half, dtype=jnp.float32) / half)
    ang = positions.astype(jnp.float32)[..., None] * inv_freq
    cos = jnp.cos(ang)[:, :, None, :]
    sin = jnp.sin(ang)[:, :, None, :]
    x32 = x.astype(jnp.float32)
    x1, x2 = x32[..., :half], x32[..., half:]
    out = jnp.concatenate([x1 * cos - x2 * sin, x2 * cos + x1 * sin], axis=-1)
    return out.astype(x.dtype)


def dsa_attention(q, k, v, q_idx, k_idx, w_idx):
    B, L, H, dh = q.shape
    top_k = min(TOPK_MAX, L // 4)
    n_blocks = L // Q_BLOCK
    key_pos = jnp.arange(L)
    q32 = q.astype(jnp.float32) * (dh ** -0.5)
    k32 = k.astype(jnp.float32)
    v32 = v.astype(jnp.float32)
    qi32 = q_idx.astype(jnp.float32)
    ki32 = k_idx.astype(jnp.float32)
    wi32 = w_idx.astype(jnp.float32) * (N_IDX_HEADS ** -0.5) * (IDX_DIM ** -0.5)

    def block(i):
        start = i * Q_BLOCK
        qb = lax.dynamic_slice_in_dim(q32, start, Q_BLOCK, axis=1)
        qib = lax.dynamic_slice_in_dim(qi32, start, Q_BLOCK, axis=1)
        wb = lax.dynamic_slice_in_dim(wi32, start, Q_BLOCK, axis=1)
        q_pos = start + jnp.arange(Q_BLOCK)
        causal = key_pos[None, :] <= q_pos[:, None]
        rel = jax.nn.relu(jnp.einsum('bqhd,bsd->bqhs', qib, ki32))
        score = jnp.einsum('bqhs,bqh->bqs', rel, wb)
        score = jnp.where(causal[None], score, -jnp.inf)
        _, sel = lax.top_k(score, top_k)
        valid = sel <= q_pos[None, :, None]
        k_sel = jax.vmap(lambda kb, ib: kb[ib])(k32, sel)
        v_sel = jax.vmap(lambda vb, ib: vb[ib])(v32, sel)
        s = jnp.einsum('bqhd,bqkhd->bhqk', qb, k_sel)
        s = jnp.where(valid[:, None], s, -jnp.inf)
        p = jax.nn.softmax(s, axis=-1)
        return jnp.einsum('bhqk,bqkhd->bqhd', p, v_sel)

    out = lax.map(block, jnp.arange(n_blocks))
    out = jnp.transpose(out, (1, 0, 2, 3, 4)).reshape(B, L, H, dh)
    return out.astype(q.dtype)


def diff_attention(q, k, v, lam, subln_w, lam_init):
    B, L, H, _, dh = q.shape
    n_blocks = L // Q_BLOCK
    key_pos = jnp.arange(L)
    q32 = q.astype(jnp.float32) * (dh ** -0.5)
    k32 = k.astype(jnp.float32)
    v32 = v.astype(jnp.float32)

    def block(i):
        start = i * Q_BLOCK
        qb = lax.dynamic_slice_in_dim(q32, start, Q_BLOCK, axis=1)
        q_pos = start + jnp.arange(Q_BLOCK)
        causal = key_pos[None, :] <= q_pos[:, None]
        s = jnp.einsum('bqhcd,bshcd->bhcqs', qb, k32)
        s = jnp.where(causal[None, None, None], s, -jnp.inf)
        p = jax.nn.softmax(s, axis=-1)
        a = p[:, :, 0] - lam * p[:, :, 1]
        return jnp.einsum('bhqs,bshe->bqhe', a, v32)

    out = lax.map(block, jnp.arange(n_blocks))
    out = jnp.transpose(out, (1, 0, 2, 3, 4)).reshape(B, L, H, 2 * dh)
    out = out * lax.rsqrt(jnp.mean(jnp.square(out), axis=-1, keepdims=True) + NORM_EPS)
    out = out * subln_w.astype(jnp.float32) * (1.0 - lam_init)
    return out.astype(q.dtype)


def setup_inputs(seed: int = 0) -> dict:
    key = jax.random.key(seed)
    ks = jax.random.split(key, 20)
    f32 = jnp.float32
    D = D_MODEL
    x = jax.random.normal(ks[0], (BATCH, SEQ, D), f32)
    c = jax.random.normal(ks[1], (BATCH, D), f32)
    offset = jax.random.randint(ks[2], (BATCH, 1), 0, 1024, dtype=jnp.int32)
    positions = (jnp.arange(SEQ, dtype=jnp.int32)[None, :] + offset).astype(jnp.int32)
    w_ada = jax.random.normal(ks[3], (DEPTH, D, 6 * D), f32) * D ** -0.5
    b_ada = jax.random.normal(ks[4], (DEPTH, 6 * D), f32) * 0.02
    norm1_w = 1.0 + 0.02 * jax.random.normal(ks[5], (DEPTH, D), f32)
    w_in = jax.random.normal(ks[6], (DEPTH, D, IN_WIDTH), f32) * D ** -0.5
    idx_k_ln_w = 1.0 + 0.02 * jax.random.normal(ks[7], (DEPTH, IDX_DIM), f32)
    idx_k_ln_b = 0.02 * jax.random.normal(ks[8], (DEPTH, IDX_DIM), f32)
    lambda_q1 = 0.1 * jax.random.normal(ks[9], (DEPTH, HEAD_DIM), f32)
    lambda_k1 = 0.1 * jax.random.normal(ks[10], (DEPTH, HEAD_DIM), f32)
    lambda_q2 = 0.1 * jax.random.normal(ks[11], (DEPTH, HEAD_DIM), f32)
    lambda_k2 = 0.1 * jax.random.normal(ks[12], (DEPTH, HEAD_DIM), f32)
    subln_w = 1.0 + 0.02 * jax.random.normal(ks[13], (DEPTH, DIFF_V_DIM), f32)
    w_out = jax.random.normal(ks[14], (DEPTH, DSA_WIDTH + DIFF_WIDTH, D), f32) * (DSA_WIDTH + DIFF_WIDTH) ** -0.5
    norm2_w = 1.0 + 0.02 * jax.random.normal(ks[15], (DEPTH, D), f32)
    w_ff1 = jax.random.normal(ks[16], (DEPTH, D, D_FF), f32) * D ** -0.5
    w_ff2 = jax.random.normal(ks[17], (DEPTH, D_FF, D), f32) * D_FF ** -0.5
    norm_f_w = 1.0 + 0.02 * jax.random.normal(ks[18], (D,), f32)
    return {"x": x, "c": c, "positions": positions, "w_ada": w_ada, "b_ada": b_ada,
            "norm1_w": norm1_w, "w_in": w_in, "idx_k_ln_w": idx_k_ln_w, "idx_k_ln_b": idx_k_ln_b,
            "lambda_q1": lambda_q1, "lambda_k1": lambda_k1, "lambda_q2": lambda_q2, "lambda_k2": lambda_k2,
            "subln_w": subln_w, "w_out": w_out, "norm2_w": norm2_w, "w_ff1": w_ff1, "w_ff2": w_ff2,
            "norm_f_w": norm_f_w}


def reference(x, c, positions, w_ada, b_ada, norm1_w, w_in, idx_k_ln_w, idx_k_ln_b,
              lambda_q1, lambda_k1, lambda_q2, lambda_k2, subln_w, w_out, norm2_w,
              w_ff1, w_ff2, norm_f_w):
    B, L, D = x.shape
    for l in range(DEPTH):
        mod = jnp.dot(jax.nn.silu(c), w_ada[l]) + b_ada[l]
        sh1, sc1, g1, sh2, sc2, g2 = jnp.split(mod, 6, axis=-1)

        h = modulate(rms_norm(x, norm1_w[l]), sh1, sc1)
        proj = jnp.dot(h, w_in[l])
        (a_q, a_k, a_v, i_q, i_k, i_w, d_q, d_k, d_v) = jnp.split(proj, SPLIT_POINTS, axis=-1)

        a_q = rope(a_q.reshape(B, L, N_DSA_HEADS, HEAD_DIM), positions)
        a_k = rope(a_k.reshape(B, L, N_DSA_HEADS, HEAD_DIM), positions)
        a_v = a_v.reshape(B, L, N_DSA_HEADS, HEAD_DIM)
        i_q = rope(i_q.reshape(B, L, N_IDX_HEADS, IDX_DIM), positions)
        i_k = layer_norm(i_k, idx_k_ln_w[l], idx_k_ln_b[l])
        i_k = rope(i_k[:, :, None, :], positions)[:, :, 0, :]
        out_a = dsa_attention(a_q, a_k, a_v, i_q, i_k, i_w)

        d_q = rope(d_q.reshape(B, L, 2 * N_DIFF_HEADS, HEAD_DIM), positions)
        d_k = rope(d_k.reshape(B, L, 2 * N_DIFF_HEADS, HEAD_DIM), positions)
        d_q = d_q.reshape(B, L, N_DIFF_HEADS, 2, HEAD_DIM)
        d_k = d_k.reshape(B, L, N_DIFF_HEADS, 2, HEAD_DIM)
        d_v = d_v.reshape(B, L, N_DIFF_HEADS, DIFF_V_DIM)
        lam_init = lambda_init(l)
        lam = (jnp.exp(jnp.sum(lambda_q1[l].astype(jnp.float32) * lambda_k1[l].astype(jnp.float32)))
               - jnp.exp(jnp.sum(lambda_q2[l].astype(jnp.float32) * lambda_k2[l].astype(jnp.float32)))
               + lam_init)
        out_b = diff_attention(d_q, d_k, d_v, lam, subln_w[l], lam_init)

        mix = jnp.concatenate([out_a.reshape(B, L, DSA_WIDTH),
                               out_b.reshape(B, L, DIFF_WIDTH)], axis=-1)
        x = x + g1[:, None, :] * jnp.dot(mix, w_out[l])

        h = modulate(rms_norm(x, norm2_w[l]), sh2, sc2)
        ff = jnp.dot(jnp.square(jax.nn.relu(jnp.dot(h, w_ff1[l]))), w_ff2[l])
        x = x + g2[:, None, :] * ff
    return rms_norm(x, norm_f_w)
```

```python
import os
import math
import numpy as np
import concourse.bass as bass
import concourse.mybir as mybir
from concourse.bass_utils import run_bass_kernel_spmd

F32 = mybir.dt.float32
BF16 = mybir.dt.bfloat16
I32 = mybir.dt.int32
U8 = mybir.dt.uint8
FP8 = mybir.dt.float8e4
ALU = mybir.AluOpType
AF = mybir.ActivationFunctionType
AX = mybir.AxisListType

D = 1024
L = 4096
NOWN = 2048
NIT = 20
TOPK = 256
LAM_INIT = 0.8 - 0.6 * math.exp(-0.3 * 0)
NORM_EPS = 1e-6
LN_EPS = 1e-5
NEG = -1.0e30
TWO_PI = float(2 * np.pi)

C_INVF, C_SGN, C_ROLE, C_NEGROLE, C_EPS, C_LNEPS, C_ONE = 0, 1, 2, 3, 4, 5, 6
C_POW2 = 8
C_LNP = 40
C_SUBW = 44
C_N1 = 48
C_N2 = 56
NCONST = 64


class Buf:
    __slots__ = ("name", "w", "r", "x")

    def __init__(self, name="b", x=False):
        self.name = name
        self.w = None
        self.r = {}
        self.x = x


class K:
    EPOCH = 30000

    def __init__(self, nc):
        self.nc = nc
        self.eng = {"pe": nc.tensor, "act": nc.scalar, "dve": nc.vector,
                    "pool": nc.gpsimd, "sp": nc.sync}
        self.cnt = {k: 0 for k in self.eng}
        self.sems = {k: [] for k in self.eng}
        self.waited = {k: {} for k in self.eng}
        self.dma_id = 0
        self._dslots = {}
        self.n_inst = 0

    def _sem(self, key, epoch):
        lst = self.sems[key]
        while len(lst) <= epoch:
            lst.append(self.nc.alloc_semaphore(name=f"s_{key}_{len(lst)}"))
        return lst[epoch]

    def _wait(self, e, tok):
        src, c = tok
        if self.waited[e].get(src, 0) >= c:
            return
        ep = (c - 1) // self.EPOCH
        val = c - ep * self.EPOCH
        self.eng[e].wait_ge(self._sem(src, ep), val)
        self.waited[e][src] = c

    def _wait_dma(self, e, tok):
        _, sem, val, uid, slot = tok
        key = ("dma", slot)
        if self.waited[e].get(key, 0) >= val:
            return
        self.eng[e].wait_ge(sem, val)
        self.waited[e][key] = val

    def _dep(self, e, tok):
        if tok is None:
            return
        if tok[0] == "dma":
            self._wait_dma(e, tok)
        else:
            self._wait(e, tok)

    def deps(self, e, reads=(), writes=()):
        for b in reads:
            if b.w is not None:
                self._dep(e, b.w)
        for b in writes:
            if b.w is not None and (b.w[0] != e or e != "pe"):
                self._dep(e, b.w)
            for re_, rc in b.r.items():
                if re_ == e and e == "pe":
                    continue
                if isinstance(re_, tuple):
                    self._wait_dma(e, rc)
                else:
                    self._wait(e, (re_, rc))

    def done(self, e, inst, reads=(), writes=()):
        self.cnt[e] += 1
        c = self.cnt[e]
        ep = (c - 1) // self.EPOCH
        inst.then_inc(self._sem(e, ep), 1)
        tok = (e, c)
        for b in reads:
            b.r[e] = c
        for b in writes:
            b.w = tok
            b.r = {}
        return tok

    def op(self, e, fn, reads=(), writes=()):
        xs = [b for b in reads if b.x and b not in writes]
        if xs:
            reads = [b for b in reads if not b.x]
            writes = list(writes) + xs
        self.deps(e, reads, writes)
        inst = fn()
        self.n_inst += 1
        return self.done(e, inst, reads, writes)

    def dma(self, q, out, in_, reads=(), writes=(), **kw):
        self.deps(q, reads, writes)
        st = self._dslots.setdefault(q, {"sems": [], "vals": [], "i": 0, "pend": []})
        NS = 14
        i = st["i"] % NS
        st["i"] += 1
        if len(st["sems"]) <= i:
            st["sems"].append(self.nc.alloc_semaphore(name=f"d_{q}_{i}"))
            st["vals"].append(0)
            st["pend"].append(None)
        prev = st["pend"][i]
        if prev is not None:
            self._wait_dma(q, prev)
        st["vals"][i] += 16
        self.dma_id += 1
        tok = ("dma", st["sems"][i], st["vals"][i], self.dma_id, (q, i))
        st["pend"][i] = tok
        inst = self.eng[q].dma_start(out=out, in_=in_, **kw)
        inst.then_inc(st["sems"][i], 16)
        for b in writes:
            b.w = tok
            b.r = {}
        for b in reads:
            b.r[("dma", self.dma_id)] = tok
        return tok

    def barrier(self, engines=("pe", "act", "dve", "pool", "sp")):
        for e in engines:
            for f in self.eng:
                if f != e and self.cnt[f] > 0:
                    self._wait(e, (f, self.cnt[f]))
            for q, st in self._dslots.items():
                for p in st["pend"]:
                    if p is not None:
                        self._wait_dma(e, p)

    def finish(self, e="sp"):
        self.barrier(engines=(e,))


def build_program(stage=99, dbg=False):
    nc = bass.Bass("TRN2", target_bir_lowering=False)
    k = K(nc)
    V, S, P, T, G = nc.vector, nc.scalar, nc.gpsimd, nc.tensor, nc.gpsimd

    def din(name, shape, dt=F32):
        return nc.dram_tensor(name, list(shape), dt, kind="ExternalInput").ap()

    x_d = din("x", [L, D])
    pos_d = din("pos", [1, L], I32)
    ct_d = din("cT", [128, 8])
    consts_d = din("consts", [128, NCONST])
    cmat_d = din("cmat", [128, 3, 128])
    lamv_d = din("lamv", [1, 256])
    wada_d = din("w_ada", [D, 6 * D])
    badaT_d = din("b_adaT", [128, 48])
    wfm_d = din("w_fm", [21, 128, 1024])
    wfmr_d = din("w_fm_rot", [21, 128, 1024])
    wtm_d = din("w_tm", [D, 1032])
    wout_d = din("w_out", [D, D])
    wff1_d = din("w_ff1", [8, 128, 8 * 512])
    wff2_d = din("w_ff2", [4 * D, D])
    nfw_d = din("norm_f_w", [1, D])
    out_d = nc.dram_tensor("out", [NOWN, D], F32, kind="ExternalOutput").ap()
    dbg_d = {}

    KB = 1024
    ARENA = 206 * KB
    arena = nc.alloc_sbuf_tensor("arena", [128, ARENA], U8).ap()

    def sb(off, shape, dt):
        esz = {F32: 4, BF16: 2, I32: 4, FP8: 1}[dt]
        n = int(np.prod(shape)) * esz
        assert off + n <= ARENA, (off, n)
        ap = arena[:, off:off + n].bitcast(dt)
        if len(shape) == 2:
            ap = ap.rearrange("p (a b) -> p a b", b=shape[1])
        elif len(shape) == 3:
            ap = ap.rearrange("p (a b c) -> p a b c", b=shape[1], c=shape[2])
        return ap

    banks = [nc.alloc_psum_tensor(f"bank{i}", [128, 512], F32).ap() for i in range(8)]
    BK = [Buf(f"bank{i}", x=True) for i in range(8)]

    def bank_bf(i):
        return banks[i].bitcast(BF16)

    o = 0
    consts = sb(o, [NCONST], F32); o += NCONST * 4
    ident = sb(o, [128], BF16); o += 256
    triT = sb(o, [128], BF16); o += 256
    trineg = sb(o, [128], F32); o += 512
    identf = sb(o, [128], F32); o += 512
    ones_bf = sb(o, [128], BF16); o += 256
    ones_f = sb(o, [128], F32); o += 512
    modT = sb(o, [48], F32); o += 192
    GB = sb(o, [32], F32); o += 128
    scT = sb(o, [8], F32); o += 32
    stat = sb(o, [128], F32); o += 512
    bis = sb(o, [8, 32], F32); o += 1024
    iw_sb = sb(o, [16, 8], F32); o += 512
    lam_t = sb(o, [8], F32); o += 32
    assert o <= 6 * KB
    CONST = Buf("const")
    STAT = Buf("stat")
    BIS = Buf("bis")
    IW = Buf("iw")

    def cc(col, n=1):
        return consts[:, col:col + n]

    O_MIX = 6 * KB
    O_K = 38 * KB
    O_V = 70 * KB
    O_KI = 102 * KB
    O_HT = 110 * KB
    O_TAB = 174 * KB
    O_T8 = 190 * KB
    O_TMPA = 22 * KB

    mixT = sb(O_MIX, [8, NOWN], BF16)
    MIX = [Buf(f"mix{i}") for i in range(8)]
    kT = sb(O_K, [4, L], BF16)
    vtm = sb(O_V, [32, 512], BF16)
    kiT = sb(O_KI, [L], BF16)
    hT_own = sb(O_HT, [8, NOWN], BF16)
    hT_oth = sb(O_HT + 32 * KB, [8, NOWN], BF16)
    cosT = sb(O_TAB, [L], BF16)
    sinT = sb(O_TAB + 8 * KB, [L], BF16)
    qT = sb(O_HT + 32 * KB, [4, NOWN], BF16)
    iqT = sb(O_HT + 48 * KB, [4, NOWN], BF16)
    TAB = Buf("tab")
    HT = [[Buf(f"ht{t}_{p}") for p in range(2)] for t in range(32)]
    KT = [[Buf(f"kT{j}_{g}") for g in range(8)] for j in range(4)]
    KI = [Buf(f"ki{g}") for g in range(8)]
    VT = [Buf(f"v{t}") for t in range(32)]
    QT = [[Buf(f"qT{j}_{g}") for g in range(4)] for j in range(4)]
    IQ = [[Buf(f"iq{j}_{g}") for g in range(4)] for j in range(4)]

    def hT_ap(kc, t0, n):
        if t0 < NOWN:
            return hT_own[:, kc, t0:t0 + n]
        return hT_oth[:, kc, t0 - NOWN:t0 - NOWN + n]

    def ht_bufs(t0, n):
        r = []
        for tt in range(t0 // 128, (t0 + n) // 128):
            r += HT[tt]
        return r

    SCR = 38 * KB
    SCR0 = 190 * KB
    k.dma("sp", consts, consts_d, writes=[CONST])
    cm32 = sb(SCR0, [3, 128], F32)
    CM = Buf("cm")
    k.dma("sp", cm32, cmat_d, writes=[CM])
    k.op("dve", lambda: V.tensor_copy(ident, cm32[:, 0, :]), reads=[CM], writes=[CONST])
    k.op("dve", lambda: V.tensor_copy(identf, cm32[:, 0, :]), reads=[CM], writes=[CONST])
    k.op("dve", lambda: V.tensor_copy(triT, cm32[:, 1, :]), reads=[CM], writes=[CONST])
    k.op("dve", lambda: V.tensor_copy(trineg, cm32[:, 2, :]), reads=[CM], writes=[CONST])
    k.op("dve", lambda: V.memset(ones_bf, 1.0), writes=[CONST])
    k.op("dve", lambda: V.memset(ones_f, 1.0 / 128.0), writes=[CONST])
    k.op("dve", lambda: V.memset(stat, 0.0), writes=[STAT])
    ct = sb(SCR0 + 2 * KB, [8], F32)
    CT = Buf("ct")
    k.dma("sp", ct, ct_d, writes=[CT])
    k.op("act", lambda: S.activation(scT, ct, AF.Silu), reads=[CT], writes=[CONST])
    badaT = sb(SCR0 + 3 * KB, [48], F32)
    BA = Buf("ba")
    k.dma("sp", badaT, badaT_d, writes=[BA])
    lv = sb(SCR0 + 4 * KB, [256], F32)
    LV = Buf("lv")
    k.dma("sp", lv, lamv_d.partition_broadcast(128), writes=[LV])
    lj = sb(SCR0 + 5 * KB, [64], F32)
    LJ = Buf("lj")
    LAMB = Buf("lam")
    k.op("dve", lambda: V.memset(lam_t, 0.0), writes=[LAMB])
    k.op("dve", lambda: V.scalar_tensor_tensor(lj, lv[:, 0:64], 1.0, lv[:, 64:128], ALU.mult, ALU.mult,
                                               accum_out=lam_t[:, 0:1]), reads=[LV, LAMB], writes=[LJ, LAMB])
    k.op("dve", lambda: V.scalar_tensor_tensor(lj, lv[:, 128:192], 1.0, lv[:, 192:256], ALU.mult, ALU.mult,
                                               accum_out=lam_t[:, 1:2]), reads=[LV, LAMB], writes=[LJ, LAMB])
    k.op("act", lambda: S.activation(lam_t[:, 2:4], lam_t[:, 0:2], AF.Exp), reads=[LAMB], writes=[LAMB])
    k.op("dve", lambda: V.tensor_tensor(lam_t[:, 4:5], lam_t[:, 3:4], lam_t[:, 2:3], ALU.subtract),
         reads=[LAMB], writes=[LAMB])
    k.op("dve", lambda: V.tensor_scalar(lam_t[:, 5:6], lam_t[:, 4:5], -LAM_INIT, None, ALU.add),
         reads=[LAMB], writes=[LAMB])
    neglam = lam_t[:, 5:6]

    WAB = [Buf("wa0"), Buf("wa1")]
    wa = [sb(110 * KB + i * 32 * KB, [8, 1024], F32) for i in range(2)]
    wada_v = wada_d.rearrange("(kc p) c -> p kc c", p=128)
    for blk in range(6):
        b = blk % 2
        k.dma("sp", wa[b], wada_v[:, :, blk * 1024:(blk + 1) * 1024], writes=[WAB[b]])

        def mm_mod(b=b, blk=blk):
            inst = None
            for j in range(8):
                col = blk * 8 + j
                for kc in range(8):
                    inst = T.matmul(banks[0][:, col:col + 1], wa[b][:, kc, j * 128:(j + 1) * 128],
                                    scT[:, kc:kc + 1], start=(kc == 0), stop=(kc == 7))
            return inst
        k.op("pe", mm_mod, reads=[WAB[b], CONST], writes=[BK[0]])
    _early = []

    def make_tables():
        posi = sb(SCR, [L], I32)
        a0 = sb(SCR + 16 * KB, [L], F32)
        q_ = sb(SCR + 32 * KB, [L], F32)
        m_ = sb(SCR + 48 * KB, [L], F32)
        PI_, A0, Q_, M_ = Buf(), Buf(), Buf(), Buf()
        k.dma("sp", posi, pos_d.partition_broadcast(128), writes=[PI_])
        k.op("dve", lambda: V.tensor_copy(a0, posi), reads=[PI_], writes=[A0])
        k.op("dve", lambda: V.tensor_scalar(a0, a0, cc(C_INVF), None, ALU.mult), reads=[A0, CONST], writes=[A0])
        for which in range(2):
            if which == 1:
                k.op("dve", lambda: V.tensor_scalar(a0, a0, float(np.pi / 2), None, ALU.add), reads=[A0], writes=[A0])
            k.op("dve", lambda: V.tensor_scalar(q_, a0, 1.0 / TWO_PI, None, ALU.mult), reads=[A0], writes=[Q_])
            k.op("dve", lambda: V.tensor_copy(posi, q_), reads=[Q_], writes=[PI_])
            k.op("dve", lambda: V.tensor_copy(q_, posi), reads=[PI_], writes=[Q_])
            k.op("dve", lambda: V.scalar_tensor_tensor(q_, q_, -TWO_PI, a0, ALU.mult, ALU.add), reads=[Q_, A0], writes=[Q_])
            k.op("dve", lambda: V.tensor_scalar(m_, q_, float(np.pi), -TWO_PI, ALU.is_gt, ALU.mult), reads=[Q_], writes=[M_])
            k.op("dve", lambda: V.tensor_tensor(q_, q_, m_, ALU.add), reads=[Q_, M_], writes=[Q_])
            k.op("dve", lambda: V.tensor_scalar(q_, q_, 3.141592, -3.141592, ALU.min, ALU.max), reads=[Q_], writes=[Q_])
            if which == 0:
                k.op("act", lambda: S.activation(sinT, q_, AF.Sin, scale=cc(C_SGN)), reads=[Q_, CONST], writes=[TAB])
            else:
                k.op("act", lambda: S.activation(cosT, q_, AF.Sin), reads=[Q_], writes=[TAB])
        k.barrier()

    def build_hT():
        xt = [sb(O_T8 + 0, [D], F32), sb(O_TMPA, [D], F32), sb(O_T8 + 4 * KB, [D], F32)]
        xn = [sb(O_TMPA + 4 * KB, [D], BF16), sb(O_TMPA + 6 * KB, [D], BF16), sb(O_TMPA + 12 * KB, [D], BF16)]
        junk = sb(O_TMPA + 8 * KB, [D], F32)
        XT, XN, JK = [Buf(), Buf(), Buf()], [Buf(), Buf(), Buf()], Buf()
        SQ = [Buf(), Buf(), Buf()]
        k.op("dve", lambda: V.memset(stat, 0.0), reads=[STAT], writes=[STAT])

        def stage_a(tt):
            b = tt % 3
            k.dma("sp", xt[b], x_d[tt * 128:(tt + 1) * 128, :], writes=[XT[b]])
            k.op("dve", lambda: V.scalar_tensor_tensor(junk, xt[b], 1.0, xt[b], ALU.mult, ALU.mult,
                                                       accum_out=stat[:, tt:tt + 1]),
                 reads=[XT[b], STAT], writes=[JK, STAT])
            k.op("act", lambda: S.activation(stat[:, 32 + tt:33 + tt], stat[:, tt:tt + 1], AF.Sqrt,
                                             bias=cc(C_EPS), scale=1.0 / D), reads=[STAT, CONST], writes=[SQ[b]])

        def stage_a2(tt):
            b = tt % 3
            k.op("dve", lambda: V.reciprocal(stat[:, 64 + tt:65 + tt], stat[:, 32 + tt:33 + tt]),
                 reads=[SQ[b]], writes=[STAT])
            k.op("dve", lambda: V.tensor_scalar(xn[b], xt[b], stat[:, 64 + tt:65 + tt], None, ALU.mult),
                 reads=[XT[b], STAT], writes=[XN[b]])

        def stage_b(tt):
            b = tt % 3
            pbs = (4 + 2 * (tt % 2), 5 + 2 * (tt % 2))
            psbs = [bank_bf(pb_).rearrange("p (a b) -> p a b", b=128) for pb_ in pbs]

            def tps():
                inst = None
                for kc in range(8):
                    inst = T.transpose(psbs[kc // 4][:, kc % 4, :], xn[b][:, kc * 128:(kc + 1) * 128], ident)
                return inst
            k.op("pe", tps, reads=[XN[b], CONST], writes=[BK[pbs[0]], BK[pbs[1]]])
            for kc in range(8):
                dst = hT_ap(kc, tt * 128, 128)
                src = psbs[kc // 4][:, kc % 4, :]
                if kc < 4:
                    k.op("act", lambda: S.activation(dst, src, AF.Identity, bias=GB[:, 8 + kc:9 + kc], scale=GB[:, kc:kc + 1]),
                         reads=[BK[pbs[0]], CONST], writes=[HT[tt][0]])
                else:
                    k.op("dve", lambda: V.tensor_scalar(dst, src, GB[:, kc:kc + 1], GB[:, 8 + kc:9 + kc], ALU.mult, ALU.add),
                         reads=[BK[pbs[1]], CONST], writes=[HT[tt][1]])
        for t in range(33):
            if t < 32:
                stage_a(t)
            if t >= 1:
                stage_b(t - 1)
            if t < 32:
                stage_a2(t)

    wb_i = [0]

    def proj_rope(chunk, dest_fn, dest_bufs, tgs, ki_mode=False):
        i = wb_i[0] % 2
        wb_i[0] += 1
        w0 = sb(O_T8 + i * 4 * KB, [8, 128], BF16)
        w1 = sb(O_T8 + i * 4 * KB + 2 * KB, [8, 128], BF16)
        WB = proj_rope.WB[i]
        k.dma("pool", w0, wfm_d[chunk].rearrange("p (a b) -> p a b", b=128), writes=[WB])
        k.dma("pool", w1, wfmr_d[chunk].rearrange("p (a b) -> p a b", b=128), writes=[WB])
        for gi, tg in enumerate(tgs):
            pa, pr = (0, 1) if proj_rope.par == 0 else (2, 3)
            proj_rope.par ^= 1
            t0 = tg * 512

            def mms(pa=pa, pr=pr, t0=t0):
                inst = None
                for kc in range(8):
                    T.matmul(banks[pa], w0[:, kc, :], hT_ap(kc, t0, 512), start=(kc == 0), stop=(kc == 7))
                for kc in range(8):
                    inst = T.matmul(banks[pr], w1[:, kc, :], hT_ap(kc, t0, 512), start=(kc == 0), stop=(kc == 7))
                return inst
            k.op("pe", mms, reads=[WB] + ht_bufs(t0, 512), writes=[BK[pa], BK[pr]])
            tb = proj_rope.tb
            proj_rope.tb ^= 1
            t1 = sb(O_TMPA + tb * 4 * KB, [512], F32)
            t2 = sb(O_TMPA + tb * 4 * KB + 2 * KB, [512], F32)
            T1, T2 = proj_rope.T1[tb], proj_rope.T2[tb]
            cs = cosT[:, t0:t0 + 512]
            sn = sinT[:, t0:t0 + 512]
            dst = dest_fn(tg)
            if not ki_mode:
                k.op("dve", lambda: V.tensor_tensor(t1, banks[pa], cs, ALU.mult), reads=[BK[pa], TAB], writes=[T1])
                k.op("dve", lambda: V.tensor_tensor(t2, banks[pr], sn, ALU.mult), reads=[BK[pr], TAB], writes=[T2])
                k.op("dve", lambda: V.tensor_tensor(dst, t1, t2, ALU.add), reads=[T1, T2], writes=[dest_bufs[gi]])
            else:
                KIS = proj_rope.KIS
                kb = 8 * KB
                psb_ = sb(SCRK + 0 * kb // 4, [512], F32)
                psq = sb(SCRK + 1 * kb // 4, [512], F32)
                mean = sb(SCRK + 2 * kb // 4, [512], F32)
                var = sb(SCRK + 3 * kb // 4, [512], F32)
                yy = sb(SCRK + 4 * kb // 4, [512], F32)
                yr = sb(SCRK + 5 * kb // 4, [512], F32)
                k.op("act", lambda: S.copy(psb_, banks[pa]), reads=[BK[pa]], writes=[KIS[0]])
                k.op("act", lambda: S.activation(psq, banks[pa], AF.Square), reads=[BK[pa]], writes=[KIS[1]])

                def mm2():
                    T.matmul(banks[4], ones_f, psb_, start=True, stop=True)
                    return T.matmul(banks[5], ones_f, psq, start=True, stop=True)
                k.op("pe", mm2, reads=[KIS[0], KIS[1], CONST], writes=[BK[4], BK[5]])
                k.op("act", lambda: S.copy(mean, banks[4]), reads=[BK[4]], writes=[KIS[2]])
                k.op("dve", lambda: V.tensor_tensor(var, mean, mean, ALU.mult), reads=[KIS[2]], writes=[KIS[3]])
                k.op("dve", lambda: V.tensor_tensor(var, banks[5], var, ALU.subtract), reads=[BK[5], KIS[3]], writes=[KIS[3]])
                k.op("act", lambda: S.activation(var, var, AF.Sqrt, bias=cc(C_LNEPS), scale=1.0), reads=[KIS[3], CONST], writes=[KIS[3]])
                k.op("dve", lambda: V.reciprocal(var, var), reads=[KIS[3]], writes=[KIS[3]])
                k.op("dve", lambda: V.tensor_tensor(yy, psb_, mean, ALU.subtract), reads=[KIS[0], KIS[2]], writes=[KIS[4]])
                k.op("dve", lambda: V.tensor_tensor(yy, yy, var, ALU.mult), reads=[KIS[4], KIS[3]], writes=[KIS[4]])
                k.op("dve", lambda: V.tensor_scalar(yy, yy, cc(C_LNP), cc(C_LNP + 1), ALU.mult, ALU.add), reads=[KIS[4], CONST], writes=[KIS[4]])
                k.op("dve", lambda: V.tensor_tensor(yr, banks[pr], mean, ALU.subtract), reads=[BK[pr], KIS[2]], writes=[KIS[5]])
                k.op("dve", lambda: V.tensor_tensor(yr, yr, var, ALU.mult), reads=[KIS[5], KIS[3]], writes=[KIS[5]])
                k.op("dve", lambda: V.tensor_scalar(yr, yr, cc(C_LNP + 2), cc(C_LNP + 3), ALU.mult, ALU.add), reads=[KIS[5], CONST], writes=[KIS[5]])
                k.op("dve", lambda: V.tensor_tensor(t1, yy, cs, ALU.mult), reads=[KIS[4], TAB], writes=[T1])
                k.op("dve", lambda: V.tensor_tensor(t2, yr, sn, ALU.mult), reads=[KIS[5], TAB], writes=[T2])
                k.op("dve", lambda: V.tensor_tensor(dst, t1, t2, ALU.add), reads=[T1, T2], writes=[dest_bufs[gi]])
    proj_rope.WB = [Buf(), Buf()]
    proj_rope.par = 0
    proj_rope.tb = 0
    proj_rope.T1 = [Buf(), Buf()]
    proj_rope.T2 = [Buf(), Buf()]
    proj_rope.KIS = [Buf() for _ in range(6)]
    SCRK = O_MIX

    def proj_v(col0):
        wv = sb(O_T8, [8, 512], BF16)
        WV = Buf()
        k.dma("pool", wv, wtm_d.rearrange("(kc p) c -> p kc c", p=128)[:, :, col0:col0 + 512], writes=[WV])
        for tt in range(32):
            pb = 4 + tt % 2

            def mms(tt=tt, pb=pb):
                inst = None
                for kc in range(8):
                    inst = T.matmul(banks[pb], hT_ap(kc, tt * 128, 128), wv[:, kc, :], start=(kc == 0), stop=(kc == 7))
                return inst
            k.op("pe", mms, reads=[WV] + HT[tt], writes=[BK[pb]])
            if tt % 2 == 0:
                k.op("act", lambda tt=tt, pb=pb: S.copy(vtm[:, tt, :], banks[pb]), reads=[BK[pb]], writes=[VT[tt]])
            else:
                k.op("dve", lambda tt=tt, pb=pb: V.tensor_copy(vtm[:, tt, :], banks[pb]), reads=[BK[pb]], writes=[VT[tt]])

    def dump(name, ap, bufs, shape, dt=F32):
        d = nc.dram_tensor(name, list(shape), dt, kind="ExternalOutput").ap()
        dbg_d[name] = d
        k.dma("sp", d, ap, reads=bufs)

    make_tables()
    k.op("dve", lambda: V.tensor_tensor(modT, banks[0][:, 0:48], badaT, ALU.add), reads=[BK[0], BA], writes=[CONST])
    k.op("dve", lambda: V.scalar_tensor_tensor(GB[:, 0:8], modT[:, 8:16], 1.0, cc(C_N1, 8), ALU.add, ALU.mult),
         reads=[CONST], writes=[CONST])
    k.op("dve", lambda: V.tensor_copy(GB[:, 8:16], modT[:, 0:8]), reads=[CONST], writes=[CONST])
    k.op("dve", lambda: V.scalar_tensor_tensor(GB[:, 16:24], modT[:, 32:40], 1.0, cc(C_N2, 8), ALU.add, ALU.mult),
         reads=[CONST], writes=[CONST])
    k.op("dve", lambda: V.tensor_copy(GB[:, 24:32], modT[:, 24:32]), reads=[CONST], writes=[CONST])
    k.barrier()
    if stage == 0:
        dump("d_modT", modT, [CONST], [128, 48], F32)
        dump("d_lam", lam_t, [LAMB], [128, 8], F32)
        k.finish("sp")
        return nc, dbg_d
    if stage == 0.5:
        dump("d_cos", cosT, [TAB], [128, L], BF16)
        dump("d_sin", sinT, [TAB], [128, L], BF16)
        k.finish("sp")
        return nc, dbg_d
    build_hT()
    k.barrier()
    if stage == 0.7:
        dump("d_hT", hT_own, sum(HT[:16], []), [128, 8, NOWN], BF16)
        k.finish("sp")
        return nc, dbg_d
    for j in range(4):
        proj_rope(j, lambda tg, j=j: kT[:, j, tg * 512:(tg + 1) * 512], KT[j], list(range(8)))
    proj_rope(4, lambda tg: kiT[:, tg * 512:(tg + 1) * 512], KI, list(range(8)), ki_mode=True)
    k.barrier()
    proj_v(0)
    k.barrier()
    for j in range(4):
        proj_rope(5 + j, lambda tg, j=j: qT[:, j, tg * 512:(tg + 1) * 512], QT[j], list(range(4)))
    for j in range(4):
        proj_rope(9 + j, lambda tg, j=j: iqT[:, j, tg * 512:(tg + 1) * 512], IQ[j], list(range(4)))
    k.barrier()
    wiw = sb(O_T8, [8, 8], BF16)
    WIW = Buf()
    k.dma("pool", wiw, wtm_d.rearrange("(kc p) c -> p kc c", p=128)[:, :, 1024:1032], writes=[WIW])
    for tt in range(16):
        def mms(tt=tt):
            inst = None
            for kc in range(8):
                inst = T.matmul(banks[4][:, tt * 8:(tt + 1) * 8], hT_ap(kc, tt * 128, 128), wiw[:, kc, :],
                                start=(kc == 0), stop=(kc == 7))
            return inst
        k.op("pe", mms, reads=[WIW] + HT[tt], writes=[BK[4]])
    k.op("dve", lambda: V.tensor_scalar(iw_sb.rearrange("p a b -> p (a b)"), banks[4][:, 0:128],
                                        float(8 ** -0.5 * 64 ** -0.5), None, ALU.mult), reads=[BK[4]], writes=[IW])

    if stage == 1:
        dump("d_hT", hT_own, sum(HT[:16], []), [128, 8, NOWN], BF16)
        dump("d_kT", kT, sum(KT, []), [128, 4, L], BF16)
        dump("d_kiT", kiT, KI, [128, L], BF16)
        dump("d_v", vtm, VT, [128, 32, 512], BF16)
        dump("d_qT", qT, sum(QT, []), [128, 4, NOWN], BF16)
        dump("d_iqT", iqT, sum(IQ, []), [128, 4, NOWN], BF16)
        dump("d_iw", iw_sb, [IW], [128, 16, 8], F32)
        dump("d_modT", modT, [CONST], [128, 48], F32)
        dump("d_lam", lam_t, [LAMB], [128, 8], F32)
        k.finish("sp")
        return nc, dbg_d
    k.barrier()

    MT = sb(O_HT, [32, 512], BF16)
    Ibufs = [sb(O_TAB, [L], F32), sb(O_T8, [L], F32)]
    JUNK = sb(O_TMPA + 8 * KB, [L], FP8)
    Mh = sb(O_TMPA + 8 * KB, [NOWN], BF16)
    NEB = 6
    EB = [sb(O_TMPA + i * KB, [512], BF16) for i in range(4)] + [sb(O_TMPA + (12 + i) * KB, [512], BF16) for i in range(2)]
    RB = [sb(O_TMPA + 4 * KB + i * 2 * KB, [512], F32) for i in range(2)]
    REC = [sb(O_TMPA + 14 * KB, [512], F32)] * 2
    MTB, MB = Buf("MT"), Buf("M")
    IBS = [Buf("I0"), Buf("I1")]
    EBB = [Buf() for _ in range(6)]
    RBB = [Buf() for _ in range(2)]
    RECB = [Buf("rec")] * 2
    mid, sacc, Hh, nH, uu, cD, negb = (bis[:, r_, :] for r_ in range(7))
    MIDB, SAB, CDB, HB, JA, JD = Buf(), Buf(), Buf(), Buf(), Buf(), Buf()
    ectr = [0]
    rctr = [0]
    sctr = [0]

    def n_icomp(i):
        return 2 * ((i + 1) * 128 + 511) // 512 * 0 + 2 * (((i + 1) * 128 + 511) // 512) * 8

    def gen_icomp(i):
        Ibuf, IB = Ibufs[i % 2], IBS[i % 2]
        n1 = (i + 1) * 128
        for (ks, c0) in ((0, 0), (NOWN, n1)):
            for p0 in range(0, n1, 512):
                n = min(512, n1 - p0)
                for h in range(8):
                    pb = sctr[0] % 3
                    sctr[0] += 1
                    hp, hh = h // 2, h % 2
                    k.op("pe", lambda: T.matmul(
                        banks[pb][:, 0:n], iqT[hh * 64:(hh + 1) * 64, hp, i * 128:(i + 1) * 128],
                        kiT[hh * 64:(hh + 1) * 64, ks + p0:ks + p0 + n], start=True, stop=True),
                        reads=[IQ[hp][i // 4], KI[(ks + p0) // 512]], writes=[BK[pb]])
                    rb = rctr[0] % 2
                    rctr[0] += 1
                    dst = Ibuf[:, c0 + p0:c0 + p0 + n]
                    wcol = iw_sb[:, i, h:h + 1]
                    if h == 0:
                        k.op("dve", lambda: V.tensor_scalar(dst, banks[pb][:, 0:n], 0.0, wcol, ALU.max, ALU.mult),
                             reads=[BK[pb], IW], writes=[IB])
                    elif h % 2 == 1:
                        k.op("dve", lambda: V.tensor_scalar(RB[rb][:, 0:n], banks[pb][:, 0:n], 0.0, wcol, ALU.max, ALU.mult),
                             reads=[BK[pb], IW], writes=[RBB[rb]])
                        k.op("dve", lambda: V.tensor_tensor(dst, dst, RB[rb][:, 0:n], ALU.add),
                             reads=[RBB[rb], IB], writes=[IB])
                    else:
                        k.op("act", lambda: S.activation(RB[rb][:, 0:n], banks[pb][:, 0:n], AF.Relu),
                             reads=[BK[pb]], writes=[RBB[rb]])
                        k.op("dve", lambda: V.scalar_tensor_tensor(dst, RB[rb][:, 0:n], wcol, dst, ALU.mult, ALU.add),
                             reads=[RBB[rb], IW, IB], writes=[IB])
                    yield

    N_POST = NIT + 4

    def gen_post(i, g):
        Ibuf, IB = Ibufs[i % 2], IBS[i % 2]
        n1 = (i + 1) * 128
        j = i - 4 * g
        W2 = 2 * n1
        Iv = Ibuf[:, 0:W2]
        k.op("dve", lambda: V.tensor_reduce(stat[:, 100:101], Iv, AX.X, ALU.max, apply_absolute_value=True),
             reads=[IB, STAT], writes=[STAT])
        k.op("dve", lambda: V.tensor_tensor(Ibuf[:, i * 128:(i + 1) * 128], Ibuf[:, i * 128:(i + 1) * 128], trineg, ALU.add),
             reads=[IB, CONST], writes=[IB])
        k.op("dve", lambda: V.tensor_scalar(Ibuf[:, n1 + i * 128:n1 + (i + 1) * 128], Ibuf[:, n1 + i * 128:n1 + (i + 1) * 128],
                                            cc(C_NEGROLE), None, ALU.add), reads=[IB, CONST], writes=[IB])
        k.op("dve", lambda: V.tensor_reduce(stat[:, 101:102], Iv, AX.X, ALU.max), reads=[IB, STAT], writes=[STAT])
        yield
        split = W2 >= 1024
        na = (int(W2 * 0.56) // 128) * 128 if split else W2
        k.op("dve", lambda: V.tensor_tensor(stat[:, 102:103], stat[:, 101:102], stat[:, 100:101], ALU.add),
             reads=[STAT], writes=[STAT])
        k.op("dve", lambda: V.memset(stat[:, 105:106], float(2 * TOPK - 1 - W2)), reads=[STAT], writes=[STAT])
        k.op("dve", lambda: V.tensor_scalar(Hh[:, 0:NIT + 1], cc(C_POW2, NIT + 1), stat[:, 102:103], None, ALU.mult),
             reads=[STAT, CONST, HB], writes=[HB])
        k.op("dve", lambda: V.tensor_scalar(nH[:, 0:NIT + 1], cc(C_POW2, NIT + 1), stat[:, 102:103], -1.0, ALU.mult, ALU.mult),
             reads=[STAT, CONST, HB], writes=[HB])
        k.op("dve", lambda: V.tensor_tensor(mid[:, 0:1], Hh[:, 0:1], stat[:, 100:101], ALU.subtract),
             reads=[HB, STAT, MIDB], writes=[MIDB])
        k.op("dve", lambda: V.memset(cD, 0.0), reads=[CDB], writes=[CDB])
        k.op("dve", lambda: V.memset(sacc, 0.0), reads=[SAB], writes=[SAB])
        yield
        for it in range(NIT):
            if split:
                k.op("dve", lambda: V.tensor_scalar(JUNK[:, na:W2], Ibuf[:, na:W2], mid[:, it:it + 1], 0.0, ALU.is_ge, ALU.add,
                                                    accum_out=cD[:, it:it + 1], saturate=False), reads=[IB, MIDB, CDB, JD], writes=[JD, CDB] + ([MB] if it == 0 else []))
                k.op("dve", lambda: V.tensor_scalar(negb[:, it:it + 1], cD[:, it:it + 1], -2.0, float(2 * TOPK - 1 - na), ALU.mult, ALU.add),
                     reads=[CDB], writes=[CDB])
            k.op("act", lambda: S.activation(JUNK[:, 0:na], Ibuf[:, 0:na], AF.Sign, bias=mid[:, it:it + 1], scale=-1.0,
                                             accum_out=sacc[:, it:it + 1], saturate=False), reads=[IB, MIDB, SAB, JA], writes=[JA, SAB] + ([MB] if it == 0 else []))
            bcol = negb[:, it:it + 1] if split else stat[:, 105:106]
            k.op("act", lambda: S.activation(uu[:, it:it + 1], sacc[:, it:it + 1], AF.Sign, bias=bcol, scale=1.0),
                 reads=[SAB, CDB, STAT], writes=[SAB])
            k.op("act", lambda: S.activation(mid[:, it + 1:it + 2], uu[:, it:it + 1], AF.Identity,
                                             bias=mid[:, it:it + 1], scale=nH[:, it + 1:it + 2]), reads=[SAB, MIDB, HB], writes=[MIDB])
            yield
        k.op("dve", lambda: V.tensor_tensor(stat[:, 104:105], mid[:, NIT:NIT + 1], Hh[:, NIT:NIT + 1], ALU.subtract),
             reads=[MIDB, HB, STAT], writes=[STAT])
        for (c0, slot0) in ((0, 0), (n1, 4 * g + 4)):
            k.op("dve", lambda: V.tensor_scalar(Mh[:, 0:n1], Ibuf[:, c0:c0 + n1], stat[:, 104:105], None, ALU.is_ge),
                 reads=[IB, STAT, MB, JA, JD], writes=[MB, JA, JD])
            for cb in range(0, i + 1, 8):
                nb = min(8, i + 1 - cb)
                pb = 3 + (sctr[0] % 2)
                sctr[0] += 1
                psb = bank_bf(pb).rearrange("p (a b) -> p a b", b=128)

                def tps():
                    inst = None
                    for q in range(nb):
                        inst = T.transpose(psb[:, q, :], Mh[:, (cb + q) * 128:(cb + q + 1) * 128], ident)
                    return inst
                k.op("pe", tps, reads=[MB, CONST], writes=[BK[pb]])
                k.op("act", lambda: S.copy(MT[:, slot0 + cb:slot0 + cb + nb, j * 128:(j + 1) * 128], psb[:, 0:nb, :]),
                     reads=[BK[pb]], writes=[MTB])
            yield

    def interleave(ga, na, gb, nb_):
        ia = ib = 0
        da = db = False
        while not (da and db):
            if not da and (db or ia * max(nb_, 1) <= ib * max(na, 1)):
                try:
                    next(ga); ia += 1
                except StopIteration:
                    da = True
            elif not db:
                try:
                    next(gb); ib += 1
                except StopIteration:
                    db = True

    def attn_slots(g):
        r = []
        for c in range(4 * g + 4):
            r.append((c * 128, c, max(0, c - 4 * g), "own"))
        for c in range(4 * g + 4):
            r.append((NOWN + c * 128, 4 * g + 4 + c, max(0, c - 4 * g), "oth"))
        return r

    LAG = 2

    def dsa_attention(g):
        slots = attn_slots(g)
        items = [(p, si) for p in range(4) for si in range(len(slots))]
        st = {}

        def stage_a(t):
            p, si = items[t]
            ks, slot, j0, kind = slots[si]
            c0 = j0 * 128
            pbs = []
            for hh in range(2):
                pb = sctr[0] % 3
                sctr[0] += 1
                pbs.append(pb)

            def qk():
                inst = None
                for hh in range(2):
                    inst = T.matmul(banks[pbs[hh]][:, c0:512], kT[hh * 64:(hh + 1) * 64, p, ks:ks + 128],
                                    qT[hh * 64:(hh + 1) * 64, p, g * 512 + c0:(g + 1) * 512], start=True, stop=True)
                return inst
            k.op("pe", qk, reads=[KT[p][ks // 512], QT[p][g]], writes=[BK[pbs[0]], BK[pbs[1]]])
            ebs = []
            for hh in range(2):
                eb = ectr[0] % NEB
                ectr[0] += 1
                ebs.append(eb)
                k.op("act", lambda: S.activation(EB[eb][:, c0:512], banks[pbs[hh]][:, c0:512], AF.Exp, scale=0.125),
                     reads=[BK[pbs[hh]]], writes=[EBB[eb]])
                k.op("dve", lambda: V.tensor_tensor(EB[eb][:, c0:512], EB[eb][:, c0:512], MT[:, slot, c0:512], ALU.mult),
                     reads=[EBB[eb], MTB], writes=[EBB[eb]])
            st[t] = ebs

        def stage_b(t):
            p, si = items[t]
            ks, slot, j0, kind = slots[si]
            c0 = j0 * 128
            kc = ks // 128
            ebs = st.pop(t)
            ob, sbk = (4, 5) if p % 2 == 0 else (6, 7)
            first, last = (si == 0), (si == len(slots) - 1)

            def pv():
                inst = None
                for hh in range(2):
                    T.matmul(banks[ob][hh * 64:(hh + 1) * 64, c0:512], vtm[:, kc, (2 * p + hh) * 64:(2 * p + hh + 1) * 64],
                             EB[ebs[hh]][:, c0:512], start=first, stop=last)
                for hh in range(2):
                    inst = T.matmul(banks[sbk][hh * 64:(hh + 1) * 64, c0:512], ones_bf[:, 0:64],
                                    EB[ebs[hh]][:, c0:512], start=first, stop=last)
                return inst
            k.op("pe", pv, reads=[EBB[ebs[0]], EBB[ebs[1]], VT[kc], CONST], writes=[BK[ob], BK[sbk]])
            if last:
                rc = p % 2
                k.op("dve", lambda: V.reciprocal(REC[rc], banks[sbk]), reads=[BK[sbk]], writes=[RECB[rc]])
                k.op("dve", lambda: V.tensor_tensor(mixT[:, p, g * 512:(g + 1) * 512], banks[ob], REC[rc], ALU.mult),
                     reads=[BK[ob], RECB[rc]], writes=[MIX[p]])
        n = len(items)
        for t in range(n + LAG):
            if t < n:
                stage_a(t)
            if t >= LAG:
                stage_b(t - LAG)
            yield

    def run(gen):
        for _ in gen:
            pass

    run(gen_icomp(0))
    for i in range(16):
        g = i // 4
        if i + 1 < 16:
            interleave(gen_post(i, g), N_POST, gen_icomp(i + 1), n_icomp(i + 1))
        else:
            run(gen_post(i, g))
        if i % 4 == 3:
            run(dsa_attention(g))
            if stage == 2 and g == 1:
                dump("d_MT", MT, [MTB], [128, 32, 512], BF16)
                dump("d_mix", mixT, MIX, [128, 8, NOWN], BF16)
                k.finish("sp")
                return nc, dbg_d
    k.barrier()

    make_tables()
    build_hT()
    k.barrier()
    for j in range(4):
        proj_rope(13 + j, lambda tg, j=j: kT[:, j, tg * 512:(tg + 1) * 512], KT[j], list(range(8)))
    k.barrier()
    proj_v(512)
    k.barrier()
    for j in range(4):
        proj_rope(17 + j, lambda tg, j=j: qT[:, j, tg * 512:(tg + 1) * 512], QT[j], list(range(4)))
    k.barrier()

    FT = [sb(O_HT + i * 2 * KB, [512], F32) for i in range(8)]
    FTB = [Buf() for _ in range(8)]
    EB = [sb(O_HT + 16 * KB + i * KB, [512], BF16) for i in range(6)]

    EACC4 = [[sb(O_HT + 24 * KB + (2 * hp + i) * 2 * KB, [512], F32) for i in range(2)] for hp in range(2)]
    EACCB4 = [[Buf(), Buf()], [Buf(), Buf()]]

    def diff_attention(g):
        slots = attn_slots(g)
        items = [(h, si) for h in range(4) for si in range(len(slots))]
        st = {}

        def stage_a(t):
            h, si = items[t]
            EACC, EACCB = EACC4[h % 2], EACCB4[h % 2]
            ks, slot, j0, kind = slots[si]
            c0 = j0 * 128
            c_abs = (ks % NOWN) // 128
            pbs = []
            for c in range(2):
                pbs.append(sctr[0] % 3)
                sctr[0] += 1

            def qk():
                inst = None
                for c in range(2):
                    inst = T.matmul(banks[pbs[c]][:, c0:512], kT[c * 64:(c + 1) * 64, h, ks:ks + 128],
                                    qT[c * 64:(c + 1) * 64, h, g * 512 + c0:(g + 1) * 512], start=True, stop=True)
                return inst
            k.op("pe", qk, reads=[KT[h][ks // 512], QT[h][g]], writes=[BK[pbs[0]], BK[pbs[1]]])
            ebs = []
            for c in range(2):
                eb = ectr[0] % NEB
                ectr[0] += 1
                ebs.append(eb)
                k.op("act", lambda: S.activation(EB[eb][:, c0:512], banks[pbs[c]][:, c0:512], AF.Exp, scale=0.125),
                     reads=[BK[pbs[c]]], writes=[EBB[eb]])
                if c_abs >= 4 * g:
                    blk = EB[eb][:, c0:c0 + 128]
                    if kind == "own":
                        k.op("dve", lambda: V.tensor_tensor(blk, blk, triT, ALU.mult), reads=[EBB[eb], CONST], writes=[EBB[eb]])
                    else:
                        k.op("dve", lambda: V.tensor_scalar(blk, blk, cc(C_ROLE), None, ALU.mult), reads=[EBB[eb], CONST], writes=[EBB[eb]])
                if si == 0:
                    k.op("dve", lambda: V.tensor_copy(EACC[c], EB[eb]), reads=[EBB[eb]], writes=[EACCB[c]])
                else:
                    k.op("dve", lambda: V.tensor_tensor(EACC[c][:, c0:512], EACC[c][:, c0:512], EB[eb][:, c0:512], ALU.add),
                         reads=[EBB[eb], EACCB[c]], writes=[EACCB[c]])
            st[t] = ebs

        def stage_b(t):
            h, si = items[t]
            ks, slot, j0, kind = slots[si]
            c0 = j0 * 128
            kc = ks // 128
            ebs = st.pop(t)
            EACC, EACCB = EACC4[h % 2], EACCB4[h % 2]
            first, last = (si == 0), (si == len(slots) - 1)

            def pv():
                inst = None
                for c in range(2):
                    inst = T.matmul(banks[3 + c][:, c0:512], vtm[:, kc, h * 128:(h + 1) * 128], EB[ebs[c]][:, c0:512],
                                    start=first, stop=last)
                return inst
            k.op("pe", pv, reads=[EBB[ebs[0]], EBB[ebs[1]], VT[kc], CONST], writes=[BK[3], BK[4]])
            if not last:
                return
            def sm():
                T.matmul(banks[5], ones_f, EACC[0], start=True, stop=True)
                return T.matmul(banks[6], ones_f, EACC[1], start=True, stop=True)
            k.op("pe", sm, reads=[EACCB[0], EACCB[1], CONST], writes=[BK[5], BK[6]])
            k.op("dve", lambda: V.reciprocal(FT[0], banks[5]), reads=[BK[5]], writes=[FTB[0]])
            k.op("dve", lambda: V.reciprocal(FT[1], banks[6]), reads=[BK[6]], writes=[FTB[1]])
            k.op("dve", lambda: V.scalar_tensor_tensor(FT[2], banks[3], 1.0 / 128.0, FT[0], ALU.mult, ALU.mult), reads=[BK[3], FTB[0]], writes=[FTB[2]])
            k.op("dve", lambda: V.scalar_tensor_tensor(FT[3], banks[4], 1.0 / 128.0, FT[1], ALU.mult, ALU.mult), reads=[BK[4], FTB[1]], writes=[FTB[3]])
            k.op("dve", lambda: V.scalar_tensor_tensor(FT[4], FT[3], neglam, FT[2], ALU.mult, ALU.add),
                 reads=[FTB[3], FTB[2], LAMB], writes=[FTB[4]])
            k.op("act", lambda: S.activation(FT[5], FT[4], AF.Square), reads=[FTB[4]], writes=[FTB[5]])
            k.op("pe", lambda: T.matmul(banks[7], ones_f, FT[5], start=True, stop=True), reads=[FTB[5], CONST], writes=[BK[7]])
            k.op("act", lambda: S.activation(FT[6], banks[7], AF.Sqrt, bias=cc(C_EPS), scale=1.0), reads=[BK[7], CONST], writes=[FTB[6]])
            k.op("dve", lambda: V.reciprocal(FT[6], FT[6]), reads=[FTB[6]], writes=[FTB[6]])
            k.op("dve", lambda: V.scalar_tensor_tensor(mixT[:, 4 + h, g * 512:(g + 1) * 512], FT[4], cc(C_SUBW), FT[6],
                                                       ALU.mult, ALU.mult), reads=[FTB[4], FTB[6], CONST], writes=[MIX[4 + h]])
        n = len(items)
        for t in range(n + LAG):
            if t < n:
                stage_a(t)
            if t >= LAG:
                stage_b(t - LAG)
            yield

    for g in range(4):
        run(diff_attention(g))
    if stage == 3:
        dump("d_mix", mixT, MIX, [128, 8, NOWN], BF16)
        k.finish("sp")
        return nc, dbg_d
    k.barrier()

    O_WO = 38 * KB
    O_X1 = 54 * KB
    O_H2 = 118 * KB
    O_C = 150 * KB
    wout = sb(O_WO, [8, D], BF16)
    x1 = sb(O_X1, [16, D], F32)
    h2T = sb(O_H2, [8, NOWN], BF16)
    gbc = [sb(O_C + i * 4 * KB, [D], F32) for i in range(2)]
    nfw = sb(O_C + 8 * KB, [D], F32)
    WO, GBC, NFW = Buf(), Buf(), Buf()
    X1 = [Buf() for _ in range(16)]
    H2 = [[Buf(), Buf()] for _ in range(16)]
    k.dma("pool", wout, wout_d.rearrange("(c p) n -> p c n", p=128), writes=[WO])
    k.dma("sp", nfw, nfw_d.partition_broadcast(128), writes=[NFW])
    dg = sb(O_C + 12 * KB, [128], F32)
    DG = Buf()
    onesf1 = sb(O_C + 13 * KB, [128], F32)
    k.op("dve", lambda: V.memset(onesf1, 1.0), writes=[DG])
    for which, base in ((0, 16), (1, 40)):
        for half in range(2):
            for jj in range(4):
                j = half * 4 + jj
                k.op("dve", lambda j=j, base=base: V.tensor_scalar(dg, identf, modT[:, base + j:base + j + 1], None, ALU.mult),
                     reads=[CONST, DG], writes=[DG])
                k.op("pe", lambda jj=jj: T.matmul(banks[0][:, jj * 128:(jj + 1) * 128], onesf1, dg, start=True, stop=True),
                     reads=[DG], writes=[BK[0]])
            k.op("act", lambda which=which, half=half: S.copy(gbc[which][:, half * 512:(half + 1) * 512], banks[0]),
                 reads=[BK[0]], writes=[GBC])

    xt2 = [sb(O_C + 16 * KB + i * 4 * KB, [D], F32) for i in range(2)]
    tmpc = [sb(O_C + 24 * KB + i * 4 * KB, [D], F32) for i in range(2)]
    xn2 = [sb(O_C + 32 * KB + i * 2 * KB, [D], BF16) for i in range(2)]
    junkc = sb(O_C + 36 * KB, [D], F32)
    XT2, TMPC, XN2, JKC = [Buf(), Buf()], [Buf(), Buf()], [Buf(), Buf()], Buf()
    k.op("dve", lambda: V.memset(stat, 0.0), reads=[STAT], writes=[STAT])
    for i in range(16):
        b = i % 2
        k.dma("sp", xt2[b], x_d[i * 128:(i + 1) * 128, :], writes=[XT2[b]])
        for half in range(2):
            pb = 2 * b + half

            def mmo(pb=pb, half=half, i=i):
                inst = None
                for ch in range(8):
                    inst = T.matmul(banks[pb], mixT[:, ch, i * 128:(i + 1) * 128], wout[:, ch, half * 512:(half + 1) * 512],
                                    start=(ch == 0), stop=(ch == 7))
                return inst
            k.op("pe", mmo, reads=MIX + [WO], writes=[BK[pb]])
            k.op("dve", lambda pb=pb, half=half, b=b: V.tensor_tensor(tmpc[b][:, half * 512:(half + 1) * 512], banks[pb],
                                                                     gbc[0][:, half * 512:(half + 1) * 512], ALU.mult),
                 reads=[BK[pb], GBC], writes=[TMPC[b]])
        k.op("pool", lambda i=i, b=b: G.tensor_tensor(x1[:, i, :], tmpc[b], xt2[b], ALU.add), reads=[TMPC[b], XT2[b]], writes=[X1[i]])
        k.op("dve", lambda i=i: V.scalar_tensor_tensor(junkc, x1[:, i, :], 1.0, x1[:, i, :], ALU.mult, ALU.mult,
                                                       accum_out=stat[:, i:i + 1]), reads=[X1[i], STAT], writes=[JKC, STAT])
        k.op("act", lambda i=i: S.activation(stat[:, 32 + i:33 + i], stat[:, i:i + 1], AF.Sqrt, bias=cc(C_EPS), scale=1.0 / D),
             reads=[STAT, CONST], writes=[STAT])
        k.op("dve", lambda i=i: V.reciprocal(stat[:, 64 + i:65 + i], stat[:, 32 + i:33 + i]), reads=[STAT], writes=[STAT])
        k.op("dve", lambda i=i, b=b: V.tensor_scalar(xn2[b], x1[:, i, :], stat[:, 64 + i:65 + i], None, ALU.mult),
             reads=[X1[i], STAT], writes=[XN2[b]])
        pbs = (4 + 2 * b, 5 + 2 * b)
        psbs = [bank_bf(pb_).rearrange("p (a b) -> p a b", b=128) for pb_ in pbs]

        def tps(b=b, psbs=psbs):
            inst = None
            for kc in range(8):
                inst = T.transpose(psbs[kc // 4][:, kc % 4, :], xn2[b][:, kc * 128:(kc + 1) * 128], ident)
            return inst
        k.op("pe", tps, reads=[XN2[b], CONST], writes=[BK[pbs[0]], BK[pbs[1]]])
        for kc in range(8):
            dst = h2T[:, kc, i * 128:(i + 1) * 128]
            src = psbs[kc // 4][:, kc % 4, :]
            if kc < 4:
                k.op("act", lambda kc=kc, dst=dst, src=src: S.activation(dst, src, AF.Identity,
                                                                        bias=GB[:, 24 + kc:25 + kc], scale=GB[:, 16 + kc:17 + kc]),
                     reads=[BK[pbs[0]], CONST], writes=[H2[i][0]])
            else:
                k.op("dve", lambda kc=kc, dst=dst, src=src: V.tensor_scalar(dst, src, GB[:, 16 + kc:17 + kc],
                                                                           GB[:, 24 + kc:25 + kc], ALU.mult, ALU.add),
                     reads=[BK[pbs[1]], CONST], writes=[H2[i][1]])
    if stage == 4:
        dump("d_x1", x1, X1, [128, 16, D], F32)
        dump("d_h2T", h2T, sum(H2, []), [128, 8, NOWN], BF16)
        dump("d_gbc", gbc[0], [GBC], [128, D], F32)
        k.finish("sp")
        return nc, dbg_d
    k.barrier()

    O_HID = 150 * KB + 16 * KB
    hid = sb(O_HID, [32, 512], BF16)
    HID = [Buf() for _ in range(32)]
    w1b = [sb(O_MIX + i * 8 * KB, [8, 512], BF16) for i in range(2)]
    w2b = [sb(O_MIX + 16 * KB + i * 8 * KB, [4, D], BF16) for i in range(2)]
    W1B, W2B = [Buf(), Buf()], [Buf(), Buf()]
    sqb = [sb(O_WO + i * 2 * KB, [512], F32) for i in range(2)]
    SQB = [Buf(), Buf()]
    tmpd = [sb(O_WO + 4 * KB + i * 4 * KB, [D], F32) for i in range(2)]
    TMPD = [Buf(), Buf()]
    junkd = sb(O_WO + 12 * KB, [D], F32)
    JKD = Buf()
    OT = [Buf(), Buf()]
    ot = [sb(O_C + 12 * KB, [D], F32), sb(O_C + 0 * KB, [D], F32)]
    w1v = wff1_d
    w2v = wff2_d.rearrange("(fb f p) c -> fb p f c", f=4, p=128)
    k.op("dve", lambda: V.memset(stat, 0.0), reads=[STAT], writes=[STAT])
    wctr = [0, 0]
    for g in range(4):
        for fb in range(8):
            wi = wctr[0] % 2
            wctr[0] += 1
            k.dma("pool", w1b[wi], w1v[fb].rearrange("p (a b) -> p a b", b=512), writes=[W1B[wi]])
            for f4 in range(4):
                fc = fb * 4 + f4
                pb = fc % 4

                def mm1(wi=wi, f4=f4, pb=pb, g=g):
                    inst = None
                    for kc in range(8):
                        inst = T.matmul(banks[pb], w1b[wi][:, kc, f4 * 128:(f4 + 1) * 128], h2T[:, kc, g * 512:(g + 1) * 512],
                                        start=(kc == 0), stop=(kc == 7))
                    return inst
                k.op("pe", mm1, reads=[W1B[wi]] + sum(H2[4 * g:4 * g + 4], []), writes=[BK[pb]])
                sq = fc % 2
                k.op("act", lambda sq=sq, pb=pb: S.activation(sqb[sq], banks[pb], AF.Relu), reads=[BK[pb]], writes=[SQB[sq]])
                k.op("dve", lambda sq=sq, fc=fc: V.tensor_tensor(hid[:, fc, :], sqb[sq], sqb[sq], ALU.mult),
                     reads=[SQB[sq]], writes=[HID[fc]])
        for fb in range(8):
            wi = wctr[1] % 2
            wctr[1] += 1
            k.dma("pool", w2b[wi], w2v[fb], writes=[W2B[wi]])

            def mm2(wi=wi, fb=fb):
                inst = None
                for f4 in range(4):
                    fc = fb * 4 + f4
                    for t in range(4):
                        for half in range(2):
                            inst = T.matmul(banks[t * 2 + half], hid[:, fc, t * 128:(t + 1) * 128],
                                            w2b[wi][:, f4, half * 512:(half + 1) * 512], start=(fc == 0), stop=(fc == 31))
                return inst
            k.op("pe", mm2, reads=[W2B[wi]] + HID[fb * 4:fb * 4 + 4], writes=BK)
        for t in range(4):
            i = 4 * g + t
            b = i % 2
            for half in range(2):
                pb = t * 2 + half
                k.op("dve", lambda pb=pb, half=half, b=b: V.tensor_tensor(tmpd[b][:, half * 512:(half + 1) * 512], banks[pb],
                                                                         gbc[1][:, half * 512:(half + 1) * 512], ALU.mult),
                     reads=[BK[pb], GBC], writes=[TMPD[b]])
            k.op("pool", lambda i=i, b=b: G.tensor_tensor(tmpd[b], tmpd[b], x1[:, i, :], ALU.add), reads=[TMPD[b], X1[i]], writes=[TMPD[b]])
            k.op("dve", lambda i=i, b=b: V.scalar_tensor_tensor(junkd, tmpd[b], 1.0, tmpd[b], ALU.mult, ALU.mult,
                                                                accum_out=stat[:, i:i + 1]), reads=[TMPD[b], STAT], writes=[JKD, STAT])
            k.op("act", lambda i=i: S.activation(stat[:, 32 + i:33 + i], stat[:, i:i + 1], AF.Sqrt, bias=cc(C_EPS), scale=1.0 / D),
                 reads=[STAT, CONST], writes=[STAT])
            k.op("dve", lambda i=i: V.reciprocal(stat[:, 64 + i:65 + i], stat[:, 32 + i:33 + i]), reads=[STAT], writes=[STAT])
            k.op("dve", lambda i=i, b=b: V.scalar_tensor_tensor(ot[b], tmpd[b], stat[:, 64 + i:65 + i], nfw, ALU.mult, ALU.mult),
                 reads=[TMPD[b], STAT, NFW], writes=[OT[b]])
            k.dma("sp", out_d[i * 128:(i + 1) * 128, :], ot[b], reads=[OT[b]])
    k.finish("sp")
    return nc, dbg_d


def _partner_perm(n):
    idx = np.arange(n)
    return np.where((idx % 64) < 32, idx + 32, idx - 32)


def _chunked(w):
    n = w.shape[1] // 128
    return np.ascontiguousarray(w.reshape(8, 128, n, 128).transpose(2, 1, 0, 3).reshape(n, 128, 1024))


def prepare_inputs(x, c, positions, w_ada, b_ada, norm1_w, w_in, idx_k_ln_w, idx_k_ln_b,
                   lambda_q1, lambda_k1, lambda_q2, lambda_k2, subln_w, w_out, norm2_w,
                   w_ff1, w_ff2, norm_f_w):
    f32 = np.float32
    w_in = np.asarray(w_in[0], f32)
    aq, ak, av = w_in[:, 0:512], w_in[:, 512:1024], w_in[:, 1024:1536]
    iq, ik, iw = w_in[:, 1536:2048], w_in[:, 2048:2112], w_in[:, 2112:2120]
    dq, dk, dv = w_in[:, 2120:2632], w_in[:, 2632:3144], w_in[:, 3144:3656]
    ikd = np.concatenate([ik, ik], axis=1)
    fm = np.concatenate([ak, ikd, aq, iq, dk, dq], axis=1)
    fm_rot = fm[:, _partner_perm(fm.shape[1])]
    w_fm = _chunked(fm)
    w_fm_rot = _chunked(fm_rot)
    w_tm = np.ascontiguousarray(np.concatenate([av, dv, iw], axis=1))
    wff1 = np.asarray(w_ff1[0], f32)
    wff1_l = np.ascontiguousarray(wff1.reshape(8, 128, 8, 512).transpose(2, 1, 0, 3).reshape(8, 128, 4096))
    p = np.arange(128)
    consts = np.zeros((128, NCONST), f32)
    inv_freq = (10000.0 ** (-np.arange(32, dtype=f32) / 32)).astype(f32)
    consts[:, C_INVF] = inv_freq[p % 32]
    consts[:, C_SGN] = np.where((p % 64) < 32, -1.0, 1.0)
    consts[:, C_EPS] = NORM_EPS
    consts[:, C_LNEPS] = LN_EPS
    consts[:, C_ONE] = 1.0
    consts[:, C_POW2:C_POW2 + NIT + 1] = (0.5 ** np.arange(1, NIT + 2))[None, :]
    lnw = np.asarray(idx_k_ln_w[0], f32)
    lnb = np.asarray(idx_k_ln_b[0], f32)
    pp = _partner_perm(64)
    consts[:, C_LNP + 0] = lnw[p % 64]
    consts[:, C_LNP + 1] = lnb[p % 64]
    consts[:, C_LNP + 2] = lnw[pp][p % 64]
    consts[:, C_LNP + 3] = lnb[pp][p % 64]
    consts[:, C_SUBW] = np.asarray(subln_w[0], f32) * f32(1.0 - LAM_INIT)
    consts[:, C_N1:C_N1 + 8] = np.asarray(norm1_w[0], f32).reshape(8, 128).T
    consts[:, C_N2:C_N2 + 8] = np.asarray(norm2_w[0], f32).reshape(8, 128).T
    cmat = np.zeros((128, 3, 128), f32)
    cmat[:, 0, :] = np.eye(128)
    cmat[:, 1, :] = (p[:, None] <= p[None, :])
    cmat[:, 2, :] = np.where(p[None, :] <= p[:, None], 0.0, NEG)
    lamv = np.concatenate([lambda_q1[0], lambda_k1[0], lambda_q2[0], lambda_k2[0]]).astype(f32)[None, :]
    shared = {
        "w_ada": np.ascontiguousarray(w_ada[0], dtype=f32),
        "b_adaT": np.ascontiguousarray(np.asarray(b_ada[0], f32).reshape(48, 128).T),
        "w_fm": w_fm, "w_fm_rot": w_fm_rot, "w_tm": w_tm,
        "w_out": np.ascontiguousarray(w_out[0], dtype=f32),
        "w_ff1": wff1_l, "w_ff2": np.ascontiguousarray(w_ff2[0], dtype=f32),
        "norm_f_w": np.asarray(norm_f_w, f32)[None, :],
        "cmat": cmat, "lamv": lamv,
    }
    in_maps = []
    perms = []
    for core in range(8):
        b, r = core // 2, core % 2
        blocks = np.concatenate([2 * np.arange(16) + r, 2 * np.arange(16) + 1 - r])
        tok = (blocks[:, None] * 128 + np.arange(128)[None, :]).reshape(-1)
        perms.append(tok[:NOWN])
        cst = consts.copy()
        cst[:, C_ROLE] = float(r)
        cst[:, C_NEGROLE] = 0.0 if r == 1 else NEG
        m = dict(shared)
        m["x"] = np.ascontiguousarray(np.asarray(x[b], f32)[tok])
        m["pos"] = np.ascontiguousarray(np.asarray(positions[b], np.int32)[tok])[None, :]
        m["cT"] = np.ascontiguousarray(np.asarray(c[b], f32).reshape(8, 128).T)
        m["consts"] = cst
        in_maps.append(m)
    return in_maps, perms


_CACHE = {}


def kernel(**inputs):
    in_maps, perms = prepare_inputs(**{k_: np.asarray(v) for k_, v in inputs.items()})
    if "nc" not in _CACHE:
        _CACHE["nc"] = build_program()[0]
    res = run_bass_kernel_spmd(_CACHE["nc"], in_maps, core_ids=list(range(8)))
    out = np.zeros((4, L, D), np.float32)
    for core in range(8):
        out[core // 2, perms[core]] = res.results[core]["out"]
    return out
```

```python
import os
import math
import numpy as np
import concourse.bass as bass
import concourse.mybir as mybir
from concourse.bass_utils import run_bass_kernel_spmd

F32 = mybir.dt.float32
BF16 = mybir.dt.bfloat16
I32 = mybir.dt.int32
U8 = mybir.dt.uint8
FP8 = mybir.dt.float8e4
ALU = mybir.AluOpType
AF = mybir.ActivationFunctionType
AX = mybir.AxisListType

D = 1024
L = 4096
NOWN = 2048
NIT = 20
TOPK = 256
LAM_INIT = 0.8 - 0.6 * math.exp(-0.3 * 0)
NORM_EPS = 1e-6
LN_EPS = 1e-5
NEG = -1.0e30
TWO_PI = float(2 * np.pi)

C_INVF, C_SGN, C_ROLE, C_NEGROLE, C_EPS, C_LNEPS, C_ONE = 0, 1, 2, 3, 4, 5, 6
C_POW2 = 8
C_LNP = 40
C_SUBW = 44
C_N1 = 48
C_N2 = 56
NCONST = 64


class Buf:
    __slots__ = ("name", "w", "r", "x")

    def __init__(self, name="b", x=False):
        self.name = name
        self.w = None
        self.r = {}
        self.x = x


class K:
    EPOCH = 30000

    def __init__(self, nc):
        self.nc = nc
        self.eng = {"pe": nc.tensor, "act": nc.scalar, "dve": nc.vector,
                    "pool": nc.gpsimd, "sp": nc.sync}
        self.cnt = {k: 0 for k in self.eng}
        self.sems = {k: [] for k in self.eng}
        self.waited = {k: {} for k in self.eng}
        self.dma_id = 0
        self._dslots = {}
        self.n_inst = 0

    def _sem(self, key, epoch):
        lst = self.sems[key]
        while len(lst) <= epoch:
            lst.append(self.nc.alloc_semaphore(name=f"s_{key}_{len(lst)}"))
        return lst[epoch]

    def _wait(self, e, tok):
        src, c = tok
        if self.waited[e].get(src, 0) >= c:
            return
        ep = (c - 1) // self.EPOCH
        val = c - ep * self.EPOCH
        self.eng[e].wait_ge(self._sem(src, ep), val)
        self.waited[e][src] = c

    def _wait_dma(self, e, tok):
        _, sem, val, uid, slot = tok
        key = ("dma", slot)
        if self.waited[e].get(key, 0) >= val:
            return
        self.eng[e].wait_ge(sem, val)
        self.waited[e][key] = val

    def _dep(self, e, tok):
        if tok is None:
            return
        if tok[0] == "dma":
            self._wait_dma(e, tok)
        else:
            self._wait(e, tok)

    def deps(self, e, reads=(), writes=()):
        for b in reads:
            if b.w is not None:
                self._dep(e, b.w)
        for b in writes:
            if b.w is not None and (b.w[0] != e or e != "pe"):
                self._dep(e, b.w)
            for re_, rc in b.r.items():
                if re_ == e and e == "pe":
                    continue
                if isinstance(re_, tuple):
                    self._wait_dma(e, rc)
                else:
                    self._wait(e, (re_, rc))

    def done(self, e, inst, reads=(), writes=()):
        self.cnt[e] += 1
        c = self.cnt[e]
        ep = (c - 1) // self.EPOCH
        inst.then_inc(self._sem(e, ep), 1)
        tok = (e, c)
        for b in reads:
            b.r[e] = c
        for b in writes:
            b.w = tok
            b.r = {}
        return tok

    def op(self, e, fn, reads=(), writes=()):
        xs = [b for b in reads if b.x and b not in writes]
        if xs:
            reads = [b for b in reads if not b.x]
            writes = list(writes) + xs
        self.deps(e, reads, writes)
        inst = fn()
        self.n_inst += 1
        return self.done(e, inst, reads, writes)

    def dma(self, q, out, in_, reads=(), writes=(), **kw):
        self.deps(q, reads, writes)
        st = self._dslots.setdefault(q, {"sems": [], "vals": [], "i": 0, "pend": []})
        NS = 14
        i = st["i"] % NS
        st["i"] += 1
        if len(st["sems"]) <= i:
            st["sems"].append(self.nc.alloc_semaphore(name=f"d_{q}_{i}"))
            st["vals"].append(0)
            st["pend"].append(None)
        prev = st["pend"][i]
        if prev is not None:
            self._wait_dma(q, prev)
        st["vals"][i] += 16
        self.dma_id += 1
        tok = ("dma", st["sems"][i], st["vals"][i], self.dma_id, (q, i))
        st["pend"][i] = tok
        inst = self.eng[q].dma_start(out=out, in_=in_, **kw)
        inst.then_inc(st["sems"][i], 16)
        for b in writes:
            b.w = tok
            b.r = {}
        for b in reads:
            b.r[("dma", self.dma_id)] = tok
        return tok

    def barrier(self, engines=("pe", "act", "dve", "pool", "sp")):
        for e in engines:
            for f in self.eng:
                if f != e and self.cnt[f] > 0:
                    self._wait(e, (f, self.cnt[f]))
            for q, st in self._dslots.items():
                for p in st["pend"]:
                    if p is not None:
                        self._wait_dma(e, p)

    def finish(self, e="sp"):
        self.barrier(engines=(e,))


def build_program(stage=99, dbg=False):
    nc = bass.Bass("TRN2", target_bir_lowering=False)
    k = K(nc)
    V, S, P, T, G = nc.vector, nc.scalar, nc.gpsimd, nc.tensor, nc.gpsimd

    def din(name, shape, dt=F32):
        return nc.dram_tensor(name, list(shape), dt, kind="ExternalInput").ap()

    x_d = din("x", [L, D])
    pos_d = din("pos", [1, L], I32)
    ct_d = din("cT", [128, 8])
    consts_d = din("consts", [128, NCONST])
    cmat_d = din("cmat", [128, 3, 128])
    lamv_d = din("lamv", [1, 256])
    wada_d = din("w_ada", [D, 6 * D])
    badaT_d = din("b_adaT", [128, 48])
    wfm_d = din("w_fm", [21, 128, 1024])
    wfmr_d = din("w_fm_rot", [21, 128, 1024])
    wtm_d = din("w_tm", [D, 1032])
    wout_d = din("w_out", [D, D])
    wff1_d = din("w_ff1", [8, 128, 8 * 512])
    wff2_d = din("w_ff2", [4 * D, D])
    nfw_d = din("norm_f_w", [1, D])
    out_d = nc.dram_tensor("out", [NOWN, D], F32, kind="ExternalOutput").ap()
    dbg_d = {}

    KB = 1024
    ARENA = 206 * KB
    arena = nc.alloc_sbuf_tensor("arena", [128, ARENA], U8).ap()

    def sb(off, shape, dt):
        esz = {F32: 4, BF16: 2, I32: 4, FP8: 1}[dt]
        n = int(np.prod(shape)) * esz
        assert off + n <= ARENA, (off, n)
        ap = arena[:, off:off + n].bitcast(dt)
        if len(shape) == 2:
            ap = ap.rearrange("p (a b) -> p a b", b=shape[1])
        elif len(shape) == 3:
            ap = ap.rearrange("p (a b c) -> p a b c", b=shape[1], c=shape[2])
        return ap

    banks = [nc.alloc_psum_tensor(f"bank{i}", [128, 512], F32).ap() for i in range(8)]
    BK = [Buf(f"bank{i}", x=True) for i in range(8)]

    def bank_bf(i):
        return banks[i].bitcast(BF16)

    o = 0
    consts = sb(o, [NCONST], F32); o += NCONST * 4
    ident = sb(o, [128], BF16); o += 256
    triT = sb(o, [128], BF16); o += 256
    trineg = sb(o, [128], F32); o += 512
    identf = sb(o, [128], F32); o += 512
    ones_bf = sb(o, [128], BF16); o += 256
    ones_f = sb(o, [128], F32); o += 512
    modT = sb(o, [48], F32); o += 192
    GB = sb(o, [32], F32); o += 128
    scT = sb(o, [8], F32); o += 32
    stat = sb(o, [128], F32); o += 512
    bis = sb(o, [6, 32], F32); o += 768
    iw_sb = sb(o, [16, 8], F32); o += 512
    lam_t = sb(o, [8], F32); o += 32
    assert o <= 6 * KB
    CONST = Buf("const")
    STAT = Buf("stat")
    BIS = Buf("bis")
    IW = Buf("iw")

    def cc(col, n=1):
        return consts[:, col:col + n]

    O_MIX = 6 * KB
    O_K = 38 * KB
    O_V = 70 * KB
    O_KI = 102 * KB
    O_HT = 110 * KB
    O_TAB = 174 * KB
    O_T8 = 190 * KB
    O_TMPA = 22 * KB

    mixT = sb(O_MIX, [8, NOWN], BF16)
    MIX = [Buf(f"mix{i}") for i in range(8)]
    kT = sb(O_K, [4, L], BF16)
    vtm = sb(O_V, [32, 512], BF16)
    kiT = sb(O_KI, [L], BF16)
    hT_own = sb(O_HT, [8, NOWN], BF16)
    hT_oth = sb(O_HT + 32 * KB, [8, NOWN], BF16)
    cosT = sb(O_TAB, [L], BF16)
    sinT = sb(O_TAB + 8 * KB, [L], BF16)
    qT = sb(O_HT + 32 * KB, [4, NOWN], BF16)
    iqT = sb(O_HT + 48 * KB, [4, NOWN], BF16)
    TAB = Buf("tab")
    HT = [[Buf(f"ht{t}_{p}") for p in range(2)] for t in range(32)]
    KT = [[Buf(f"kT{j}_{g}") for g in range(8)] for j in range(4)]
    KI = [Buf(f"ki{g}") for g in range(8)]
    VT = [Buf(f"v{t}") for t in range(32)]
    QT = [[Buf(f"qT{j}_{g}") for g in range(4)] for j in range(4)]
    IQ = [[Buf(f"iq{j}_{g}") for g in range(4)] for j in range(4)]

    def hT_ap(kc, t0, n):
        if t0 < NOWN:
            return hT_own[:, kc, t0:t0 + n]
        return hT_oth[:, kc, t0 - NOWN:t0 - NOWN + n]

    def ht_bufs(t0, n):
        r = []
        for tt in range(t0 // 128, (t0 + n) // 128):
            r += HT[tt]
        return r

    SCR = 38 * KB
    SCR0 = 190 * KB
    k.dma("sp", consts, consts_d, writes=[CONST])
    cm32 = sb(SCR0, [3, 128], F32)
    CM = Buf("cm")
    k.dma("sp", cm32, cmat_d, writes=[CM])
    k.op("dve", lambda: V.tensor_copy(ident, cm32[:, 0, :]), reads=[CM], writes=[CONST])
    k.op("dve", lambda: V.tensor_copy(identf, cm32[:, 0, :]), reads=[CM], writes=[CONST])
    k.op("dve", lambda: V.tensor_copy(triT, cm32[:, 1, :]), reads=[CM], writes=[CONST])
    k.op("dve", lambda: V.tensor_copy(trineg, cm32[:, 2, :]), reads=[CM], writes=[CONST])
    k.op("dve", lambda: V.memset(ones_bf, 1.0), writes=[CONST])
    k.op("dve", lambda: V.memset(ones_f, 1.0 / 128.0), writes=[CONST])
    k.op("dve", lambda: V.memset(stat, 0.0), writes=[STAT])
    ct = sb(SCR0 + 2 * KB, [8], F32)
    CT = Buf("ct")
    k.dma("sp", ct, ct_d, writes=[CT])
    k.op("act", lambda: S.activation(scT, ct, AF.Silu), reads=[CT], writes=[CONST])
    badaT = sb(SCR0 + 3 * KB, [48], F32)
    BA = Buf("ba")
    k.dma("sp", badaT, badaT_d, writes=[BA])
    lv = sb(SCR0 + 4 * KB, [256], F32)
    LV = Buf("lv")
    k.dma("sp", lv, lamv_d.partition_broadcast(128), writes=[LV])
    lj = sb(SCR0 + 5 * KB, [64], F32)
    LJ = Buf("lj")
    LAMB = Buf("lam")
    k.op("dve", lambda: V.memset(lam_t, 0.0), writes=[LAMB])
    k.op("dve", lambda: V.scalar_tensor_tensor(lj, lv[:, 0:64], 1.0, lv[:, 64:128], ALU.mult, ALU.mult,
                                               accum_out=lam_t[:, 0:1]), reads=[LV, LAMB], writes=[LJ, LAMB])
    k.op("dve", lambda: V.scalar_tensor_tensor(lj, lv[:, 128:192], 1.0, lv[:, 192:256], ALU.mult, ALU.mult,
                                               accum_out=lam_t[:, 1:2]), reads=[LV, LAMB], writes=[LJ, LAMB])
    k.op("act", lambda: S.activation(lam_t[:, 2:4], lam_t[:, 0:2], AF.Exp), reads=[LAMB], writes=[LAMB])
    k.op("dve", lambda: V.tensor_tensor(lam_t[:, 4:5], lam_t[:, 3:4], lam_t[:, 2:3], ALU.subtract),
         reads=[LAMB], writes=[LAMB])
    k.op("dve", lambda: V.tensor_scalar(lam_t[:, 5:6], lam_t[:, 4:5], -LAM_INIT, None, ALU.add),
         reads=[LAMB], writes=[LAMB])
    neglam = lam_t[:, 5:6]

    WAB = [Buf("wa0"), Buf("wa1")]
    wa = [sb(110 * KB + i * 32 * KB, [8, 1024], F32) for i in range(2)]
    wada_v = wada_d.rearrange("(kc p) c -> p kc c", p=128)
    for blk in range(6):
        b = blk % 2
        k.dma("sp", wa[b], wada_v[:, :, blk * 1024:(blk + 1) * 1024], writes=[WAB[b]])

        def mm_mod(b=b, blk=blk):
            inst = None
            for j in range(8):
                col = blk * 8 + j
                for kc in range(8):
                    inst = T.matmul(banks[0][:, col:col + 1], wa[b][:, kc, j * 128:(j + 1) * 128],
                                    scT[:, kc:kc + 1], start=(kc == 0), stop=(kc == 7))
            return inst
        k.op("pe", mm_mod, reads=[WAB[b], CONST], writes=[BK[0]])
    _early = []

    def make_tables():
        posi = sb(SCR, [L], I32)
        a0 = sb(SCR + 16 * KB, [L], F32)
        q_ = sb(SCR + 32 * KB, [L], F32)
        m_ = sb(SCR + 48 * KB, [L], F32)
        PI_, A0, Q_, M_ = Buf(), Buf(), Buf(), Buf()
        k.dma("sp", posi, pos_d.partition_broadcast(128), writes=[PI_])
        k.op("dve", lambda: V.tensor_copy(a0, posi), reads=[PI_], writes=[A0])
        k.op("dve", lambda: V.tensor_scalar(a0, a0, cc(C_INVF), None, ALU.mult), reads=[A0, CONST], writes=[A0])
        for which in range(2):
            if which == 1:
                k.op("dve", lambda: V.tensor_scalar(a0, a0, float(np.pi / 2), None, ALU.add), reads=[A0], writes=[A0])
            k.op("dve", lambda: V.tensor_scalar(q_, a0, 1.0 / TWO_PI, None, ALU.mult), reads=[A0], writes=[Q_])
            k.op("dve", lambda: V.tensor_copy(posi, q_), reads=[Q_], writes=[PI_])
            k.op("dve", lambda: V.tensor_copy(q_, posi), reads=[PI_], writes=[Q_])
            k.op("dve", lambda: V.scalar_tensor_tensor(q_, q_, -TWO_PI, a0, ALU.mult, ALU.add), reads=[Q_, A0], writes=[Q_])
            k.op("dve", lambda: V.tensor_scalar(m_, q_, float(np.pi), -TWO_PI, ALU.is_gt, ALU.mult), reads=[Q_], writes=[M_])
            k.op("dve", lambda: V.tensor_tensor(q_, q_, m_, ALU.add), reads=[Q_, M_], writes=[Q_])
            k.op("dve", lambda: V.tensor_scalar(q_, q_, 3.141592, -3.141592, ALU.min, ALU.max), reads=[Q_], writes=[Q_])
            if which == 0:
                k.op("act", lambda: S.activation(sinT, q_, AF.Sin, scale=cc(C_SGN)), reads=[Q_, CONST], writes=[TAB])
            else:
                k.op("act", lambda: S.activation(cosT, q_, AF.Sin), reads=[Q_], writes=[TAB])
        k.barrier()

    def build_hT():
        xt = [sb(O_T8 + 0, [D], F32), sb(O_TMPA, [D], F32), sb(O_T8 + 4 * KB, [D], F32)]
        xn = [sb(O_TMPA + 4 * KB, [D], BF16), sb(O_TMPA + 6 * KB, [D], BF16), sb(O_TMPA + 12 * KB, [D], BF16)]
        junk = sb(O_TMPA + 8 * KB, [D], F32)
        XT, XN, JK = [Buf(), Buf(), Buf()], [Buf(), Buf(), Buf()], Buf()
        SQ = [Buf(), Buf(), Buf()]
        k.op("dve", lambda: V.memset(stat, 0.0), reads=[STAT], writes=[STAT])

        def stage_a(tt):
            b = tt % 3
            k.dma("sp", xt[b], x_d[tt * 128:(tt + 1) * 128, :], writes=[XT[b]])
            k.op("act", lambda: S.activation(junk, xt[b], AF.Square, accum_out=stat[:, tt:tt + 1]),
                 reads=[XT[b], STAT], writes=[JK, STAT])
            k.op("act", lambda: S.activation(stat[:, 32 + tt:33 + tt], stat[:, tt:tt + 1], AF.Sqrt,
                                             bias=cc(C_EPS), scale=1.0 / D), reads=[STAT, CONST], writes=[SQ[b]])

        def stage_a2(tt):
            b = tt % 3
            k.op("dve", lambda: V.reciprocal(stat[:, 64 + tt:65 + tt], stat[:, 32 + tt:33 + tt]),
                 reads=[SQ[b]], writes=[STAT])
            k.op("dve", lambda: V.tensor_scalar(xn[b], xt[b], stat[:, 64 + tt:65 + tt], None, ALU.mult),
                 reads=[XT[b], STAT], writes=[XN[b]])

        def stage_b(tt):
            b = tt % 3
            pbs = (4 + 2 * (tt % 2), 5 + 2 * (tt % 2))
            psbs = [bank_bf(pb_).rearrange("p (a b) -> p a b", b=128) for pb_ in pbs]

            def tps():
                inst = None
                for kc in range(8):
                    inst = T.transpose(psbs[kc // 4][:, kc % 4, :], xn[b][:, kc * 128:(kc + 1) * 128], ident)
                return inst
            k.op("pe", tps, reads=[XN[b], CONST], writes=[BK[pbs[0]], BK[pbs[1]]])
            for kc in range(8):
                dst = hT_ap(kc, tt * 128, 128)
                src = psbs[kc // 4][:, kc % 4, :]
                if kc < 4:
                    k.op("act", lambda: S.activation(dst, src, AF.Identity, bias=GB[:, 8 + kc:9 + kc], scale=GB[:, kc:kc + 1]),
                         reads=[BK[pbs[0]], CONST], writes=[HT[tt][0]])
                else:
                    k.op("dve", lambda: V.tensor_scalar(dst, src, GB[:, kc:kc + 1], GB[:, 8 + kc:9 + kc], ALU.mult, ALU.add),
                         reads=[BK[pbs[1]], CONST], writes=[HT[tt][1]])
        for t in range(33):
            if t < 32:
                stage_a(t)
            if t >= 1:
                stage_b(t - 1)
            if t < 32:
                stage_a2(t)

    wb_i = [0]

    def proj_rope(chunk, dest_fn, dest_bufs, tgs, ki_mode=False):
        i = wb_i[0] % 2
        wb_i[0] += 1
        w0 = sb(O_T8 + i * 4 * KB, [8, 128], BF16)
        w1 = sb(O_T8 + i * 4 * KB + 2 * KB, [8, 128], BF16)
        WB = proj_rope.WB[i]
        k.dma("pool", w0, wfm_d[chunk].rearrange("p (a b) -> p a b", b=128), writes=[WB])
        k.dma("pool", w1, wfmr_d[chunk].rearrange("p (a b) -> p a b", b=128), writes=[WB])
        for gi, tg in enumerate(tgs):
            pa, pr = (0, 1) if proj_rope.par == 0 else (2, 3)
            proj_rope.par ^= 1
            t0 = tg * 512

            def mms(pa=pa, pr=pr, t0=t0):
                inst = None
                for kc in range(8):
                    T.matmul(banks[pa], w0[:, kc, :], hT_ap(kc, t0, 512), start=(kc == 0), stop=(kc == 7))
                for kc in range(8):
                    inst = T.matmul(banks[pr], w1[:, kc, :], hT_ap(kc, t0, 512), start=(kc == 0), stop=(kc == 7))
                return inst
            k.op("pe", mms, reads=[WB] + ht_bufs(t0, 512), writes=[BK[pa], BK[pr]])
            tb = proj_rope.tb
            proj_rope.tb ^= 1
            t1 = sb(O_TMPA + tb * 4 * KB, [512], F32)
            t2 = sb(O_TMPA + tb * 4 * KB + 2 * KB, [512], F32)
            T1, T2 = proj_rope.T1[tb], proj_rope.T2[tb]
            cs = cosT[:, t0:t0 + 512]
            sn = sinT[:, t0:t0 + 512]
            dst = dest_fn(tg)
            if not ki_mode:
                k.op("dve", lambda: V.tensor_tensor(t1, banks[pa], cs, ALU.mult), reads=[BK[pa], TAB], writes=[T1])
                k.op("dve", lambda: V.tensor_tensor(t2, banks[pr], sn, ALU.mult), reads=[BK[pr], TAB], writes=[T2])
                k.op("dve", lambda: V.tensor_tensor(dst, t1, t2, ALU.add), reads=[T1, T2], writes=[dest_bufs[gi]])
            else:
                KIS = proj_rope.KIS
                kb = 8 * KB
                psb_ = sb(SCRK + 0 * kb // 4, [512], F32)
                psq = sb(SCRK + 1 * kb // 4, [512], F32)
                mean = sb(SCRK + 2 * kb // 4, [512], F32)
                var = sb(SCRK + 3 * kb // 4, [512], F32)
                yy = sb(SCRK + 4 * kb // 4, [512], F32)
                yr = sb(SCRK + 5 * kb // 4, [512], F32)
                k.op("act", lambda: S.copy(psb_, banks[pa]), reads=[BK[pa]], writes=[KIS[0]])
                k.op("act", lambda: S.activation(psq, banks[pa], AF.Square), reads=[BK[pa]], writes=[KIS[1]])

                def mm2():
                    T.matmul(banks[4], ones_f, psb_, start=True, stop=True)
                    return T.matmul(banks[5], ones_f, psq, start=True, stop=True)
                k.op("pe", mm2, reads=[KIS[0], KIS[1], CONST], writes=[BK[4], BK[5]])
                k.op("act", lambda: S.copy(mean, banks[4]), reads=[BK[4]], writes=[KIS[2]])
                k.op("dve", lambda: V.tensor_tensor(var, mean, mean, ALU.mult), reads=[KIS[2]], writes=[KIS[3]])
                k.op("dve", lambda: V.tensor_tensor(var, banks[5], var, ALU.subtract), reads=[BK[5], KIS[3]], writes=[KIS[3]])
                k.op("act", lambda: S.activation(var, var, AF.Sqrt, bias=cc(C_LNEPS), scale=1.0), reads=[KIS[3], CONST], writes=[KIS[3]])
                k.op("dve", lambda: V.reciprocal(var, var), reads=[KIS[3]], writes=[KIS[3]])
                k.op("dve", lambda: V.tensor_tensor(yy, psb_, mean, ALU.subtract), reads=[KIS[0], KIS[2]], writes=[KIS[4]])
                k.op("dve", lambda: V.tensor_tensor(yy, yy, var, ALU.mult), reads=[KIS[4], KIS[3]], writes=[KIS[4]])
                k.op("dve", lambda: V.tensor_scalar(yy, yy, cc(C_LNP), cc(C_LNP + 1), ALU.mult, ALU.add), reads=[KIS[4], CONST], writes=[KIS[4]])
                k.op("dve", lambda: V.tensor_tensor(yr, banks[pr], mean, ALU.subtract), reads=[BK[pr], KIS[2]], writes=[KIS[5]])
                k.op("dve", lambda: V.tensor_tensor(yr, yr, var, ALU.mult), reads=[KIS[5], KIS[3]], writes=[KIS[5]])
                k.op("dve", lambda: V.tensor_scalar(yr, yr, cc(C_LNP + 2), cc(C_LNP + 3), ALU.mult, ALU.add), reads=[KIS[5], CONST], writes=[KIS[5]])
                k.op("dve", lambda: V.tensor_tensor(t1, yy, cs, ALU.mult), reads=[KIS[4], TAB], writes=[T1])
                k.op("dve", lambda: V.tensor_tensor(t2, yr, sn, ALU.mult), reads=[KIS[5], TAB], writes=[T2])
                k.op("dve", lambda: V.tensor_tensor(dst, t1, t2, ALU.add), reads=[T1, T2], writes=[dest_bufs[gi]])
    proj_rope.WB = [Buf(), Buf()]
    proj_rope.par = 0
    proj_rope.tb = 0
    proj_rope.T1 = [Buf(), Buf()]
    proj_rope.T2 = [Buf(), Buf()]
    proj_rope.KIS = [Buf() for _ in range(6)]
    SCRK = O_MIX

    def proj_v(col0):
        wv = sb(O_T8, [8, 512], BF16)
        WV = Buf()
        k.dma("pool", wv, wtm_d.rearrange("(kc p) c -> p kc c", p=128)[:, :, col0:col0 + 512], writes=[WV])
        for tt in range(32):
            pb = 4 + tt % 2

            def mms(tt=tt, pb=pb):
                inst = None
                for kc in range(8):
                    inst = T.matmul(banks[pb], hT_ap(kc, tt * 128, 128), wv[:, kc, :], start=(kc == 0), stop=(kc == 7))
                return inst
            k.op("pe", mms, reads=[WV] + HT[tt], writes=[BK[pb]])
            if tt % 2 == 0:
                k.op("act", lambda tt=tt, pb=pb: S.copy(vtm[:, tt, :], banks[pb]), reads=[BK[pb]], writes=[VT[tt]])
            else:
                k.op("dve", lambda tt=tt, pb=pb: V.tensor_copy(vtm[:, tt, :], banks[pb]), reads=[BK[pb]], writes=[VT[tt]])

    def dump(name, ap, bufs, shape, dt=F32):
        d = nc.dram_tensor(name, list(shape), dt, kind="ExternalOutput").ap()
        dbg_d[name] = d
        k.dma("sp", d, ap, reads=bufs)

    make_tables()
    k.op("dve", lambda: V.tensor_tensor(modT, banks[0][:, 0:48], badaT, ALU.add), reads=[BK[0], BA], writes=[CONST])
    k.op("dve", lambda: V.scalar_tensor_tensor(GB[:, 0:8], modT[:, 8:16], 1.0, cc(C_N1, 8), ALU.add, ALU.mult),
         reads=[CONST], writes=[CONST])
    k.op("dve", lambda: V.tensor_copy(GB[:, 8:16], modT[:, 0:8]), reads=[CONST], writes=[CONST])
    k.op("dve", lambda: V.scalar_tensor_tensor(GB[:, 16:24], modT[:, 32:40], 1.0, cc(C_N2, 8), ALU.add, ALU.mult),
         reads=[CONST], writes=[CONST])
    k.op("dve", lambda: V.tensor_copy(GB[:, 24:32], modT[:, 24:32]), reads=[CONST], writes=[CONST])
    k.barrier()
    if stage == 0:
        dump("d_modT", modT, [CONST], [128, 48], F32)
        dump("d_lam", lam_t, [LAMB], [128, 8], F32)
        k.finish("sp")
        return nc, dbg_d
    if stage == 0.5:
        dump("d_cos", cosT, [TAB], [128, L], BF16)
        dump("d_sin", sinT, [TAB], [128, L], BF16)
        k.finish("sp")
        return nc, dbg_d
    build_hT()
    k.barrier()
    if stage == 0.7:
        dump("d_hT", hT_own, sum(HT[:16], []), [128, 8, NOWN], BF16)
        k.finish("sp")
        return nc, dbg_d
    for j in range(4):
        proj_rope(j, lambda tg, j=j: kT[:, j, tg * 512:(tg + 1) * 512], KT[j], list(range(8)))
    proj_rope(4, lambda tg: kiT[:, tg * 512:(tg + 1) * 512], KI, list(range(8)), ki_mode=True)
    k.barrier()
    proj_v(0)
    k.barrier()
    for j in range(4):
        proj_rope(5 + j, lambda tg, j=j: qT[:, j, tg * 512:(tg + 1) * 512], QT[j], list(range(4)))
    for j in range(4):
        proj_rope(9 + j, lambda tg, j=j: iqT[:, j, tg * 512:(tg + 1) * 512], IQ[j], list(range(4)))
    k.barrier()
    wiw = sb(O_T8, [8, 8], BF16)
    WIW = Buf()
    k.dma("pool", wiw, wtm_d.rearrange("(kc p) c -> p kc c", p=128)[:, :, 1024:1032], writes=[WIW])
    for tt in range(16):
        def mms(tt=tt):
            inst = None
            for kc in range(8):
                inst = T.matmul(banks[4][:, tt * 8:(tt + 1) * 8], hT_ap(kc, tt * 128, 128), wiw[:, kc, :],
                                start=(kc == 0), stop=(kc == 7))
            return inst
        k.op("pe", mms, reads=[WIW] + HT[tt], writes=[BK[4]])
    k.op("dve", lambda: V.tensor_scalar(iw_sb.rearrange("p a b -> p (a b)"), banks[4][:, 0:128],
                                        float(8 ** -0.5 * 64 ** -0.5), None, ALU.mult), reads=[BK[4]], writes=[IW])

    if stage == 1:
        dump("d_hT", hT_own, sum(HT[:16], []), [128, 8, NOWN], BF16)
        dump("d_kT", kT, sum(KT, []), [128, 4, L], BF16)
        dump("d_kiT", kiT, KI, [128, L], BF16)
        dump("d_v", vtm, VT, [128, 32, 512], BF16)
        dump("d_qT", qT, sum(QT, []), [128, 4, NOWN], BF16)
        dump("d_iqT", iqT, sum(IQ, []), [128, 4, NOWN], BF16)
        dump("d_iw", iw_sb, [IW], [128, 16, 8], F32)
        dump("d_modT", modT, [CONST], [128, 48], F32)
        dump("d_lam", lam_t, [LAMB], [128, 8], F32)
        k.finish("sp")
        return nc, dbg_d
    k.barrier()

    MT = sb(O_HT, [32, 512], BF16)
    Ibufs = [sb(O_TAB, [L], F32), sb(O_T8, [L], F32)]
    JUNK = sb(O_TMPA + 8 * KB, [L], FP8)
    Mh = sb(O_TMPA + 8 * KB, [NOWN], BF16)
    NEB = 6
    EB = [sb(O_TMPA + i * KB, [512], BF16) for i in range(4)] + [sb(O_TMPA + (12 + i) * KB, [512], BF16) for i in range(2)]
    RB = [sb(O_TMPA + 4 * KB + i * 2 * KB, [512], F32) for i in range(2)]
    REC = [sb(O_TMPA + 14 * KB, [512], F32)] * 2
    MTB, MB = Buf("MT"), Buf("M")
    IBS = [Buf("I0"), Buf("I1")]
    EBB = [Buf() for _ in range(6)]
    RBB = [Buf() for _ in range(2)]
    RECB = [Buf("rec")] * 2
    nm, sacc, Hh, nH, uu = bis[:, 0, :], bis[:, 1, :], bis[:, 2, :], bis[:, 3, :], bis[:, 4, :]
    ectr = [0]
    rctr = [0]
    sctr = [0]

    def n_icomp(i):
        return 2 * ((i + 1) * 128 + 511) // 512 * 0 + 2 * (((i + 1) * 128 + 511) // 512) * 8

    def gen_icomp(i):
        Ibuf, IB = Ibufs[i % 2], IBS[i % 2]
        n1 = (i + 1) * 128
        for (ks, c0) in ((0, 0), (NOWN, n1)):
            for p0 in range(0, n1, 512):
                n = min(512, n1 - p0)
                for h in range(8):
                    pb = sctr[0] % 3
                    sctr[0] += 1
                    hp, hh = h // 2, h % 2
                    k.op("pe", lambda: T.matmul(
                        banks[pb][:, 0:n], iqT[hh * 64:(hh + 1) * 64, hp, i * 128:(i + 1) * 128],
                        kiT[hh * 64:(hh + 1) * 64, ks + p0:ks + p0 + n], start=True, stop=True),
                        reads=[IQ[hp][i // 4], KI[(ks + p0) // 512]], writes=[BK[pb]])
                    rb = rctr[0] % 2
                    rctr[0] += 1
                    dst = Ibuf[:, c0 + p0:c0 + p0 + n]
                    wcol = iw_sb[:, i, h:h + 1]
                    if h == 0:
                        k.op("dve", lambda: V.tensor_scalar(dst, banks[pb][:, 0:n], 0.0, wcol, ALU.max, ALU.mult),
                             reads=[BK[pb], IW], writes=[IB])
                    elif h % 2 == 1:
                        k.op("dve", lambda: V.tensor_scalar(RB[rb][:, 0:n], banks[pb][:, 0:n], 0.0, wcol, ALU.max, ALU.mult),
                             reads=[BK[pb], IW], writes=[RBB[rb]])
                        k.op("dve", lambda: V.tensor_tensor(dst, dst, RB[rb][:, 0:n], ALU.add),
                             reads=[RBB[rb], IB], writes=[IB])
                    else:
                        k.op("act", lambda: S.activation(RB[rb][:, 0:n], banks[pb][:, 0:n], AF.Relu),
                             reads=[BK[pb]], writes=[RBB[rb]])
                        k.op("dve", lambda: V.scalar_tensor_tensor(dst, RB[rb][:, 0:n], wcol, dst, ALU.mult, ALU.add),
                             reads=[RBB[rb], IW, IB], writes=[IB])
                    yield

    N_POST = NIT + 4

    def gen_post(i, g):
        Ibuf, IB = Ibufs[i % 2], IBS[i % 2]
        n1 = (i + 1) * 128
        j = i - 4 * g
        W2 = 2 * n1
        Iv = Ibuf[:, 0:W2]
        k.op("dve", lambda: V.tensor_reduce(stat[:, 100:101], Iv, AX.X, ALU.max, apply_absolute_value=True),
             reads=[IB, STAT], writes=[STAT])
        k.op("dve", lambda: V.tensor_tensor(Ibuf[:, i * 128:(i + 1) * 128], Ibuf[:, i * 128:(i + 1) * 128], trineg, ALU.add),
             reads=[IB, CONST], writes=[IB])
        k.op("dve", lambda: V.tensor_scalar(Ibuf[:, n1 + i * 128:n1 + (i + 1) * 128], Ibuf[:, n1 + i * 128:n1 + (i + 1) * 128],
                                            cc(C_NEGROLE), None, ALU.add), reads=[IB, CONST], writes=[IB])
        k.op("dve", lambda: V.tensor_reduce(stat[:, 101:102], Iv, AX.X, ALU.max), reads=[IB, STAT], writes=[STAT])
        yield
        k.op("dve", lambda: V.tensor_tensor(stat[:, 102:103], stat[:, 101:102], stat[:, 100:101], ALU.add),
             reads=[STAT], writes=[STAT])
        k.op("dve", lambda: V.memset(stat[:, 105:106], float(W2 - 2 * TOPK + 1)), reads=[STAT], writes=[STAT])
        k.op("dve", lambda: V.tensor_scalar(Hh[:, 0:NIT + 1], cc(C_POW2, NIT + 1), stat[:, 102:103], None, ALU.mult),
             reads=[STAT, CONST, BIS], writes=[BIS])
        k.op("dve", lambda: V.tensor_scalar(nH[:, 0:NIT + 1], cc(C_POW2, NIT + 1), stat[:, 102:103], -1.0, ALU.mult, ALU.mult),
             reads=[STAT, CONST, BIS], writes=[BIS])
        k.op("dve", lambda: V.tensor_tensor(nm[:, 0:1], stat[:, 100:101], Hh[:, 0:1], ALU.subtract),
             reads=[BIS, STAT], writes=[BIS])
        k.op("dve", lambda: V.memset(sacc, 0.0), reads=[BIS], writes=[BIS])
        yield
        for it in range(NIT):
            k.op("act", lambda: S.activation(JUNK[:, 0:W2], Iv, AF.Sign, bias=nm[:, it:it + 1], scale=1.0,
                                             accum_out=sacc[:, it:it + 1], saturate=False), reads=[IB, BIS, MB], writes=[MB, BIS])
            k.op("act", lambda: S.activation(uu[:, it:it + 1], sacc[:, it:it + 1], AF.Sign, bias=stat[:, 105:106], scale=1.0),
                 reads=[BIS, STAT], writes=[BIS])
            k.op("act", lambda: S.activation(nm[:, it + 1:it + 2], uu[:, it:it + 1], AF.Identity,
                                             bias=nm[:, it:it + 1], scale=nH[:, it + 1:it + 2]), reads=[BIS], writes=[BIS])
            yield
        k.op("dve", lambda: V.tensor_scalar(stat[:, 104:105], nm[:, NIT:NIT + 1], -1.0, Hh[:, NIT:NIT + 1], ALU.mult, ALU.subtract),
             reads=[BIS, STAT], writes=[STAT])
        for (c0, slot0) in ((0, 0), (n1, 4 * g + 4)):
            k.op("dve", lambda: V.tensor_scalar(Mh[:, 0:n1], Ibuf[:, c0:c0 + n1], stat[:, 104:105], None, ALU.is_ge),
                 reads=[IB, STAT, MB], writes=[MB])
            for cb in range(0, i + 1, 8):
                nb = min(8, i + 1 - cb)
                pb = 3 + (sctr[0] % 2)
                sctr[0] += 1
                psb = bank_bf(pb).rearrange("p (a b) -> p a b", b=128)

                def tps():
                    inst = None
                    for q in range(nb):
                        inst = T.transpose(psb[:, q, :], Mh[:, (cb + q) * 128:(cb + q + 1) * 128], ident)
                    return inst
                k.op("pe", tps, reads=[MB, CONST], writes=[BK[pb]])
                k.op("act", lambda: S.copy(MT[:, slot0 + cb:slot0 + cb + nb, j * 128:(j + 1) * 128], psb[:, 0:nb, :]),
                     reads=[BK[pb]], writes=[MTB])
            yield

    def interleave(ga, na, gb, nb_):
        ia = ib = 0
        da = db = False
        while not (da and db):
            if not da and (db or ia * max(nb_, 1) <= ib * max(na, 1)):
                try:
                    next(ga); ia += 1
                except StopIteration:
                    da = True
            elif not db:
                try:
                    next(gb); ib += 1
                except StopIteration:
                    db = True

    def attn_slots(g):
        r = []
        for c in range(4 * g + 4):
            r.append((c * 128, c, max(0, c - 4 * g), "own"))
        for c in range(4 * g + 4):
            r.append((NOWN + c * 128, 4 * g + 4 + c, max(0, c - 4 * g), "oth"))
        return r

    LAG = 2

    def dsa_attention(g):
        slots = attn_slots(g)
        items = [(p, si) for p in range(4) for si in range(len(slots))]
        st = {}

        def stage_a(t):
            p, si = items[t]
            ks, slot, j0, kind = slots[si]
            c0 = j0 * 128
            pbs = []
            for hh in range(2):
                pb = sctr[0] % 3
                sctr[0] += 1
                pbs.append(pb)

            def qk():
                inst = None
                for hh in range(2):
                    inst = T.matmul(banks[pbs[hh]][:, c0:512], kT[hh * 64:(hh + 1) * 64, p, ks:ks + 128],
                                    qT[hh * 64:(hh + 1) * 64, p, g * 512 + c0:(g + 1) * 512], start=True, stop=True)
                return inst
            k.op("pe", qk, reads=[KT[p][ks // 512], QT[p][g]], writes=[BK[pbs[0]], BK[pbs[1]]])
            ebs = []
            for hh in range(2):
                eb = ectr[0] % NEB
                ectr[0] += 1
                ebs.append(eb)
                k.op("act", lambda: S.activation(EB[eb][:, c0:512], banks[pbs[hh]][:, c0:512], AF.Exp, scale=0.125),
                     reads=[BK[pbs[hh]]], writes=[EBB[eb]])
                k.op("dve", lambda: V.tensor_tensor(EB[eb][:, c0:512], EB[eb][:, c0:512], MT[:, slot, c0:512], ALU.mult),
                     reads=[EBB[eb], MTB], writes=[EBB[eb]])
            st[t] = ebs

        def stage_b(t):
            p, si = items[t]
            ks, slot, j0, kind = slots[si]
            c0 = j0 * 128
            kc = ks // 128
            ebs = st.pop(t)
            ob, sbk = (4, 5) if p % 2 == 0 else (6, 7)
            first, last = (si == 0), (si == len(slots) - 1)

            def pv():
                inst = None
                for hh in range(2):
                    T.matmul(banks[ob][hh * 64:(hh + 1) * 64, c0:512], vtm[:, kc, (2 * p + hh) * 64:(2 * p + hh + 1) * 64],
                             EB[ebs[hh]][:, c0:512], start=first, stop=last)
                for hh in range(2):
                    inst = T.matmul(banks[sbk][hh * 64:(hh + 1) * 64, c0:512], ones_bf[:, 0:64],
                                    EB[ebs[hh]][:, c0:512], start=first, stop=last)
                return inst
            k.op("pe", pv, reads=[EBB[ebs[0]], EBB[ebs[1]], VT[kc], CONST], writes=[BK[ob], BK[sbk]])
            if last:
                rc = p % 2
                k.op("dve", lambda: V.reciprocal(REC[rc], banks[sbk]), reads=[BK[sbk]], writes=[RECB[rc]])
                k.op("dve", lambda: V.tensor_tensor(mixT[:, p, g * 512:(g + 1) * 512], banks[ob], REC[rc], ALU.mult),
                     reads=[BK[ob], RECB[rc]], writes=[MIX[p]])
        n = len(items)
        for t in range(n + LAG):
            if t < n:
                stage_a(t)
            if t >= LAG:
                stage_b(t - LAG)
            yield

    def run(gen):
        for _ in gen:
            pass

    run(gen_icomp(0))
    for i in range(16):
        g = i // 4
        if i + 1 < 16:
            interleave(gen_post(i, g), N_POST, gen_icomp(i + 1), n_icomp(i + 1))
        else:
            run(gen_post(i, g))
        if i % 4 == 3:
            run(dsa_attention(g))
            if stage == 2 and g == 1:
                dump("d_MT", MT, [MTB], [128, 32, 512], BF16)
                dump("d_mix", mixT, MIX, [128, 8, NOWN], BF16)
                k.finish("sp")
                return nc, dbg_d
    k.barrier()

    make_tables()
    build_hT()
    k.barrier()
    for j in range(4):
        proj_rope(13 + j, lambda tg, j=j: kT[:, j, tg * 512:(tg + 1) * 512], KT[j], list(range(8)))
    k.barrier()
    proj_v(512)
    k.barrier()
    for j in range(4):
        proj_rope(17 + j, lambda tg, j=j: qT[:, j, tg * 512:(tg + 1) * 512], QT[j], list(range(4)))
    k.barrier()

    FT = [sb(O_HT + i * 2 * KB, [512], F32) for i in range(8)]
    FTB = [Buf() for _ in range(8)]
    EB = [sb(O_HT + 16 * KB + i * KB, [512], BF16) for i in range(6)]

    EACC4 = [[sb(O_HT + 24 * KB + (2 * hp + i) * 2 * KB, [512], F32) for i in range(2)] for hp in range(2)]
    EACCB4 = [[Buf(), Buf()], [Buf(), Buf()]]

    def diff_attention(g):
        slots = attn_slots(g)
        items = [(h, si) for h in range(4) for si in range(len(slots))]
        st = {}

        def stage_a(t):
            h, si = items[t]
            EACC, EACCB = EACC4[h % 2], EACCB4[h % 2]
            ks, slot, j0, kind = slots[si]
            c0 = j0 * 128
            c_abs = (ks % NOWN) // 128
            pbs = []
            for c in range(2):
                pbs.append(sctr[0] % 3)
                sctr[0] += 1

            def qk():
                inst = None
                for c in range(2):
                    inst = T.matmul(banks[pbs[c]][:, c0:512], kT[c * 64:(c + 1) * 64, h, ks:ks + 128],
                                    qT[c * 64:(c + 1) * 64, h, g * 512 + c0:(g + 1) * 512], start=True, stop=True)
                return inst
            k.op("pe", qk, reads=[KT[h][ks // 512], QT[h][g]], writes=[BK[pbs[0]], BK[pbs[1]]])
            ebs = []
            for c in range(2):
                eb = ectr[0] % NEB
                ectr[0] += 1
                ebs.append(eb)
                k.op("act", lambda: S.activation(EB[eb][:, c0:512], banks[pbs[c]][:, c0:512], AF.Exp, scale=0.125),
                     reads=[BK[pbs[c]]], writes=[EBB[eb]])
                if c_abs >= 4 * g:
                    blk = EB[eb][:, c0:c0 + 128]
                    if kind == "own":
                        k.op("dve", lambda: V.tensor_tensor(blk, blk, triT, ALU.mult), reads=[EBB[eb], CONST], writes=[EBB[eb]])
                    else:
                        k.op("dve", lambda: V.tensor_scalar(blk, blk, cc(C_ROLE), None, ALU.mult), reads=[EBB[eb], CONST], writes=[EBB[eb]])
                if si == 0:
                    k.op("dve", lambda: V.tensor_copy(EACC[c], EB[eb]), reads=[EBB[eb]], writes=[EACCB[c]])
                else:
                    k.op("dve", lambda: V.tensor_tensor(EACC[c][:, c0:512], EACC[c][:, c0:512], EB[eb][:, c0:512], ALU.add),
                         reads=[EBB[eb], EACCB[c]], writes=[EACCB[c]])
            st[t] = ebs

        def stage_b(t):
            h, si = items[t]
            ks, slot, j0, kind = slots[si]
            c0 = j0 * 128
            kc = ks // 128
            ebs = st.pop(t)
            EACC, EACCB = EACC4[h % 2], EACCB4[h % 2]
            first, last = (si == 0), (si == len(slots) - 1)

            def pv():
                inst = None
                for c in range(2):
                    inst = T.matmul(banks[3 + c][:, c0:512], vtm[:, kc, h * 128:(h + 1) * 128], EB[ebs[c]][:, c0:512],
                                    start=first, stop=last)
                return inst
            k.op("pe", pv, reads=[EBB[ebs[0]], EBB[ebs[1]], VT[kc], CONST], writes=[BK[3], BK[4]])
            if not last:
                return
            def sm():
                T.matmul(banks[5], ones_f, EACC[0], start=True, stop=True)
                return T.matmul(banks[6], ones_f, EACC[1], start=True, stop=True)
            k.op("pe", sm, reads=[EACCB[0], EACCB[1], CONST], writes=[BK[5], BK[6]])
            k.op("dve", lambda: V.reciprocal(FT[0], banks[5]), reads=[BK[5]], writes=[FTB[0]])
            k.op("dve", lambda: V.reciprocal(FT[1], banks[6]), reads=[BK[6]], writes=[FTB[1]])
            k.op("dve", lambda: V.scalar_tensor_tensor(FT[2], banks[3], 1.0 / 128.0, FT[0], ALU.mult, ALU.mult), reads=[BK[3], FTB[0]], writes=[FTB[2]])
            k.op("dve", lambda: V.scalar_tensor_tensor(FT[3], banks[4], 1.0 / 128.0, FT[1], ALU.mult, ALU.mult), reads=[BK[4], FTB[1]], writes=[FTB[3]])
            k.op("dve", lambda: V.scalar_tensor_tensor(FT[4], FT[3], neglam, FT[2], ALU.mult, ALU.add),
                 reads=[FTB[3], FTB[2], LAMB], writes=[FTB[4]])
            k.op("act", lambda: S.activation(FT[5], FT[4], AF.Square), reads=[FTB[4]], writes=[FTB[5]])
            k.op("pe", lambda: T.matmul(banks[7], ones_f, FT[5], start=True, stop=True), reads=[FTB[5], CONST], writes=[BK[7]])
            k.op("act", lambda: S.activation(FT[6], banks[7], AF.Sqrt, bias=cc(C_EPS), scale=1.0), reads=[BK[7], CONST], writes=[FTB[6]])
            k.op("dve", lambda: V.reciprocal(FT[6], FT[6]), reads=[FTB[6]], writes=[FTB[6]])
            k.op("dve", lambda: V.scalar_tensor_tensor(mixT[:, 4 + h, g * 512:(g + 1) * 512], FT[4], cc(C_SUBW), FT[6],
                                                       ALU.mult, ALU.mult), reads=[FTB[4], FTB[6], CONST], writes=[MIX[4 + h]])
        n = len(items)
        for t in range(n + LAG):
            if t < n:
                stage_a(t)
            if t >= LAG:
                stage_b(t - LAG)
            yield

    for g in range(4):
        run(diff_attention(g))
    if stage == 3:
        dump("d_mix", mixT, MIX, [128, 8, NOWN], BF16)
        k.finish("sp")
        return nc, dbg_d
    k.barrier()

    O_WO = 38 * KB
    O_X1 = 54 * KB
    O_H2 = 118 * KB
    O_C = 150 * KB
    wout = sb(O_WO, [8, D], BF16)
    x1 = sb(O_X1, [16, D], F32)
    h2T = sb(O_H2, [8, NOWN], BF16)
    gbc = [sb(O_C + i * 4 * KB, [D], F32) for i in range(2)]
    nfw = sb(O_C + 8 * KB, [D], F32)
    WO, GBC, NFW = Buf(), Buf(), Buf()
    X1 = [Buf() for _ in range(16)]
    H2 = [[Buf(), Buf()] for _ in range(16)]
    k.dma("pool", wout, wout_d.rearrange("(c p) n -> p c n", p=128), writes=[WO])
    k.dma("sp", nfw, nfw_d.partition_broadcast(128), writes=[NFW])
    dg = sb(O_C + 12 * KB, [128], F32)
    DG = Buf()
    onesf1 = sb(O_C + 13 * KB, [128], F32)
    k.op("dve", lambda: V.memset(onesf1, 1.0), writes=[DG])
    for which, base in ((0, 16), (1, 40)):
        for half in range(2):
            for jj in range(4):
                j = half * 4 + jj
                k.op("dve", lambda j=j, base=base: V.tensor_scalar(dg, identf, modT[:, base + j:base + j + 1], None, ALU.mult),
                     reads=[CONST, DG], writes=[DG])
                k.op("pe", lambda jj=jj: T.matmul(banks[0][:, jj * 128:(jj + 1) * 128], onesf1, dg, start=True, stop=True),
                     reads=[DG], writes=[BK[0]])
            k.op("act", lambda which=which, half=half: S.copy(gbc[which][:, half * 512:(half + 1) * 512], banks[0]),
                 reads=[BK[0]], writes=[GBC])

    xt2 = [sb(O_C + 16 * KB + i * 4 * KB, [D], F32) for i in range(2)]
    tmpc = [sb(O_C + 24 * KB + i * 4 * KB, [D], F32) for i in range(2)]
    xn2 = [sb(O_C + 32 * KB + i * 2 * KB, [D], BF16) for i in range(2)]
    junkc = sb(O_C + 36 * KB, [D], F32)
    XT2, TMPC, XN2, JKC = [Buf(), Buf()], [Buf(), Buf()], [Buf(), Buf()], Buf()
    k.op("dve", lambda: V.memset(stat, 0.0), reads=[STAT], writes=[STAT])
    for i in range(16):
        b = i % 2
        k.dma("sp", xt2[b], x_d[i * 128:(i + 1) * 128, :], writes=[XT2[b]])
        for half in range(2):
            pb = 2 * b + half

            def mmo(pb=pb, half=half, i=i):
                inst = None
                for ch in range(8):
                    inst = T.matmul(banks[pb], mixT[:, ch, i * 128:(i + 1) * 128], wout[:, ch, half * 512:(half + 1) * 512],
                                    start=(ch == 0), stop=(ch == 7))
                return inst
            k.op("pe", mmo, reads=MIX + [WO], writes=[BK[pb]])
            k.op("dve", lambda pb=pb, half=half, b=b: V.tensor_tensor(tmpc[b][:, half * 512:(half + 1) * 512], banks[pb],
                                                                     gbc[0][:, half * 512:(half + 1) * 512], ALU.mult),
                 reads=[BK[pb], GBC], writes=[TMPC[b]])
        k.op("pool", lambda i=i, b=b: G.tensor_tensor(x1[:, i, :], tmpc[b], xt2[b], ALU.add), reads=[TMPC[b], XT2[b]], writes=[X1[i]])
        k.op("dve", lambda i=i: V.scalar_tensor_tensor(junkc, x1[:, i, :], 1.0, x1[:, i, :], ALU.mult, ALU.mult,
                                                       accum_out=stat[:, i:i + 1]), reads=[X1[i], STAT], writes=[JKC, STAT])
        k.op("act", lambda i=i: S.activation(stat[:, 32 + i:33 + i], stat[:, i:i + 1], AF.Sqrt, bias=cc(C_EPS), scale=1.0 / D),
             reads=[STAT, CONST], writes=[STAT])
        k.op("dve", lambda i=i: V.reciprocal(stat[:, 64 + i:65 + i], stat[:, 32 + i:33 + i]), reads=[STAT], writes=[STAT])
        k.op("dve", lambda i=i, b=b: V.tensor_scalar(xn2[b], x1[:, i, :], stat[:, 64 + i:65 + i], None, ALU.mult),
             reads=[X1[i], STAT], writes=[XN2[b]])
        pbs = (4 + 2 * b, 5 + 2 * b)
        psbs = [bank_bf(pb_).rearrange("p (a b) -> p a b", b=128) for pb_ in pbs]

        def tps(b=b, psbs=psbs):
            inst = None
            for kc in range(8):
                inst = T.transpose(psbs[kc // 4][:, kc % 4, :], xn2[b][:, kc * 128:(kc + 1) * 128], ident)
            return inst
        k.op("pe", tps, reads=[XN2[b], CONST], writes=[BK[pbs[0]], BK[pbs[1]]])
        for kc in range(8):
            dst = h2T[:, kc, i * 128:(i + 1) * 128]
            src = psbs[kc // 4][:, kc % 4, :]
            if kc < 4:
                k.op("act", lambda kc=kc, dst=dst, src=src: S.activation(dst, src, AF.Identity,
                                                                        bias=GB[:, 24 + kc:25 + kc], scale=GB[:, 16 + kc:17 + kc]),
                     reads=[BK[pbs[0]], CONST], writes=[H2[i][0]])
            else:
                k.op("dve", lambda kc=kc, dst=dst, src=src: V.tensor_scalar(dst, src, GB[:, 16 + kc:17 + kc],
                                                                           GB[:, 24 + kc:25 + kc], ALU.mult, ALU.add),
                     reads=[BK[pbs[1]], CONST], writes=[H2[i][1]])
    if stage == 4:
        dump("d_x1", x1, X1, [128, 16, D], F32)
        dump("d_h2T", h2T, sum(H2, []), [128, 8, NOWN], BF16)
        dump("d_gbc", gbc[0], [GBC], [128, D], F32)
        k.finish("sp")
        return nc, dbg_d
    k.barrier()

    O_HID = 150 * KB + 16 * KB
    hid = sb(O_HID, [32, 512], BF16)
    HID = [Buf() for _ in range(32)]
    w1b = [sb(O_MIX + i * 8 * KB, [8, 512], BF16) for i in range(2)]
    w2b = [sb(O_MIX + 16 * KB + i * 8 * KB, [4, D], BF16) for i in range(2)]
    W1B, W2B = [Buf(), Buf()], [Buf(), Buf()]
    sqb = [sb(O_WO + i * 2 * KB, [512], F32) for i in range(2)]
    SQB = [Buf(), Buf()]
    tmpd = [sb(O_WO + 4 * KB + i * 4 * KB, [D], F32) for i in range(2)] + [sb(198 * KB + i * 4 * KB, [D], F32) for i in range(2)]
    TMPD = [Buf(), Buf(), Buf(), Buf()]
    junkd = sb(O_WO + 12 * KB, [D], F32)
    JKD = Buf()
    OT = [Buf(), Buf()]
    ot = [sb(O_C + 12 * KB, [D], F32), sb(O_C + 0 * KB, [D], F32)]
    w1v = wff1_d
    w2v = wff2_d.rearrange("(fb f p) c -> fb p f c", f=4, p=128)
    k.op("dve", lambda: V.memset(stat, 0.0), reads=[STAT], writes=[STAT])
    wctr = [0, 0]
    for g in range(4):
        for fb in range(8):
            wi = wctr[0] % 2
            wctr[0] += 1
            k.dma("pool", w1b[wi], w1v[fb].rearrange("p (a b) -> p a b", b=512), writes=[W1B[wi]])
            for f4 in range(4):
                fc = fb * 4 + f4
                pb = fc % 4

                def mm1(wi=wi, f4=f4, pb=pb, g=g):
                    inst = None
                    for kc in range(8):
                        inst = T.matmul(banks[pb], w1b[wi][:, kc, f4 * 128:(f4 + 1) * 128], h2T[:, kc, g * 512:(g + 1) * 512],
                                        start=(kc == 0), stop=(kc == 7))
                    return inst
                k.op("pe", mm1, reads=[W1B[wi]] + sum(H2[4 * g:4 * g + 4], []), writes=[BK[pb]])
                sq = fc % 2
                k.op("act", lambda sq=sq, pb=pb: S.activation(sqb[sq], banks[pb], AF.Relu), reads=[BK[pb]], writes=[SQB[sq]])
                k.op("dve", lambda sq=sq, fc=fc: V.tensor_tensor(hid[:, fc, :], sqb[sq], sqb[sq], ALU.mult),
                     reads=[SQB[sq]], writes=[HID[fc]])
        for fb in range(8):
            wi = wctr[1] % 2
            wctr[1] += 1
            k.dma("pool", w2b[wi], w2v[fb], writes=[W2B[wi]])

            def mm2(wi=wi, fb=fb):
                inst = None
                for f4 in range(4):
                    fc = fb * 4 + f4
                    for t in range(4):
                        for half in range(2):
                            inst = T.matmul(banks[t * 2 + half], hid[:, fc, t * 128:(t + 1) * 128],
                                            w2b[wi][:, f4, half * 512:(half + 1) * 512], start=(fc == 0), stop=(fc == 31))
                return inst
            k.op("pe", mm2, reads=[W2B[wi]] + HID[fb * 4:fb * 4 + 4], writes=BK)
        for t in range(4):
            for half in range(2):
                pb = t * 2 + half
                k.op("dve", lambda pb=pb, half=half, t=t: V.tensor_tensor(tmpd[t][:, half * 512:(half + 1) * 512], banks[pb],
                                                                         gbc[1][:, half * 512:(half + 1) * 512], ALU.mult),
                     reads=[BK[pb], GBC], writes=[TMPD[t]])
        for t in range(4):
            i = 4 * g + t
            b = i % 2
            k.op("pool", lambda i=i, t=t: G.tensor_tensor(tmpd[t], tmpd[t], x1[:, i, :], ALU.add), reads=[TMPD[t], X1[i]], writes=[TMPD[t]])
            k.op("act", lambda i=i, t=t: S.activation(junkd, tmpd[t], AF.Square, accum_out=stat[:, i:i + 1]),
                 reads=[TMPD[t], STAT], writes=[JKD, STAT])
            k.op("act", lambda i=i: S.activation(stat[:, 32 + i:33 + i], stat[:, i:i + 1], AF.Sqrt, bias=cc(C_EPS), scale=1.0 / D),
                 reads=[STAT, CONST], writes=[STAT])
            k.op("dve", lambda i=i: V.reciprocal(stat[:, 64 + i:65 + i], stat[:, 32 + i:33 + i]), reads=[STAT], writes=[STAT])
            k.op("dve", lambda i=i, b=b, t=t: V.scalar_tensor_tensor(ot[b], tmpd[t], stat[:, 64 + i:65 + i], nfw, ALU.mult, ALU.mult),
                 reads=[TMPD[t], STAT, NFW], writes=[OT[b]])
            k.dma("sp", out_d[i * 128:(i + 1) * 128, :], ot[b], reads=[OT[b]])
    k.finish("sp")
    return nc, dbg_d


def _partner_perm(n):
    idx = np.arange(n)
    return np.where((idx % 64) < 32, idx + 32, idx - 32)


def _chunked(w):
    n = w.shape[1] // 128
    return np.ascontiguousarray(w.reshape(8, 128, n, 128).transpose(2, 1, 0, 3).reshape(n, 128, 1024))


def prepare_inputs(x, c, positions, w_ada, b_ada, norm1_w, w_in, idx_k_ln_w, idx_k_ln_b,
                   lambda_q1, lambda_k1, lambda_q2, lambda_k2, subln_w, w_out, norm2_w,
                   w_ff1, w_ff2, norm_f_w):
    f32 = np.float32
    w_in = np.asarray(w_in[0], f32)
    aq, ak, av = w_in[:, 0:512], w_in[:, 512:1024], w_in[:, 1024:1536]
    iq, ik, iw = w_in[:, 1536:2048], w_in[:, 2048:2112], w_in[:, 2112:2120]
    dq, dk, dv = w_in[:, 2120:2632], w_in[:, 2632:3144], w_in[:, 3144:3656]
    ikd = np.concatenate([ik, ik], axis=1)
    fm = np.concatenate([ak, ikd, aq, iq, dk, dq], axis=1)
    fm_rot = fm[:, _partner_perm(fm.shape[1])]
    w_fm = _chunked(fm)
    w_fm_rot = _chunked(fm_rot)
    w_tm = np.ascontiguousarray(np.concatenate([av, dv, iw], axis=1))
    wff1 = np.asarray(w_ff1[0], f32)
    wff1_l = np.ascontiguousarray(wff1.reshape(8, 128, 8, 512).transpose(2, 1, 0, 3).reshape(8, 128, 4096))
    p = np.arange(128)
    consts = np.zeros((128, NCONST), f32)
    inv_freq = (10000.0 ** (-np.arange(32, dtype=f32) / 32)).astype(f32)
    consts[:, C_INVF] = inv_freq[p % 32]
    consts[:, C_SGN] = np.where((p % 64) < 32, -1.0, 1.0)
    consts[:, C_EPS] = NORM_EPS
    consts[:, C_LNEPS] = LN_EPS
    consts[:, C_ONE] = 1.0
    consts[:, C_POW2:C_POW2 + NIT + 1] = (0.5 ** np.arange(1, NIT + 2))[None, :]
    lnw = np.asarray(idx_k_ln_w[0], f32)
    lnb = np.asarray(idx_k_ln_b[0], f32)
    pp = _partner_perm(64)
    consts[:, C_LNP + 0] = lnw[p % 64]
    consts[:, C_LNP + 1] = lnb[p % 64]
    consts[:, C_LNP + 2] = lnw[pp][p % 64]
    consts[:, C_LNP + 3] = lnb[pp][p % 64]
    consts[:, C_SUBW] = np.asarray(subln_w[0], f32) * f32(1.0 - LAM_INIT)
    consts[:, C_N1:C_N1 + 8] = np.asarray(norm1_w[0], f32).reshape(8, 128).T
    consts[:, C_N2:C_N2 + 8] = np.asarray(norm2_w[0], f32).reshape(8, 128).T
    cmat = np.zeros((128, 3, 128), f32)
    cmat[:, 0, :] = np.eye(128)
    cmat[:, 1, :] = (p[:, None] <= p[None, :])
    cmat[:, 2, :] = np.where(p[None, :] <= p[:, None], 0.0, NEG)
    lamv = np.concatenate([lambda_q1[0], lambda_k1[0], lambda_q2[0], lambda_k2[0]]).astype(f32)[None, :]
    shared = {
        "w_ada": np.ascontiguousarray(w_ada[0], dtype=f32),
        "b_adaT": np.ascontiguousarray(np.asarray(b_ada[0], f32).reshape(48, 128).T),
        "w_fm": w_fm, "w_fm_rot": w_fm_rot, "w_tm": w_tm,
        "w_out": np.ascontiguousarray(w_out[0], dtype=f32),
        "w_ff1": wff1_l, "w_ff2": np.ascontiguousarray(w_ff2[0], dtype=f32),
        "norm_f_w": np.asarray(norm_f_w, f32)[None, :],
        "cmat": cmat, "lamv": lamv,
    }
    in_maps = []
    perms = []
    for core in range(8):
        b, r = core // 2, core % 2
        blocks = np.concatenate([2 * np.arange(16) + r, 2 * np.arange(16) + 1 - r])
        tok = (blocks[:, None] * 128 + np.arange(128)[None, :]).reshape(-1)
        perms.append(tok[:NOWN])
        cst = consts.copy()
        cst[:, C_ROLE] = float(r)
        cst[:, C_NEGROLE] = 0.0 if r == 1 else NEG
        m = dict(shared)
        m["x"] = np.ascontiguousarray(np.asarray(x[b], f32)[tok])
        m["pos"] = np.ascontiguousarray(np.asarray(positions[b], np.int32)[tok])[None, :]
        m["cT"] = np.ascontiguousarray(np.asarray(c[b], f32).reshape(8, 128).T)
        m["consts"] = cst
        in_maps.append(m)
    return in_maps, perms


_CACHE = {}


def kernel(**inputs):
    in_maps, perms = prepare_inputs(**{k_: np.asarray(v) for k_, v in inputs.items()})
    if "nc" not in _CACHE:
        _CACHE["nc"] = build_program()[0]
    res = run_bass_kernel_spmd(_CACHE["nc"], in_maps, core_ids=list(range(8)))
    out = np.zeros((4, L, D), np.float32)
    for core in range(8):
        out[core // 2, perms[core]] = res.results[core]["out"]
    return out
```

```python
import os
import math
import numpy as np
import concourse.bass as bass
import concourse.mybir as mybir
from concourse.bass_utils import run_bass_kernel_spmd

F32 = mybir.dt.float32
BF16 = mybir.dt.bfloat16
I32 = mybir.dt.int32
U8 = mybir.dt.uint8
FP8 = mybir.dt.float8e4
ALU = mybir.AluOpType
AF = mybir.ActivationFunctionType
AX = mybir.AxisListType

D = 1024
L = 4096
NOWN = 2048
NIT = 18
TOPK = 256
LAM_INIT = 0.8 - 0.6 * math.exp(-0.3 * 0)
NORM_EPS = 1e-6
LN_EPS = 1e-5
NEG = -1.0e30
TWO_PI = float(2 * np.pi)

C_INVF, C_SGN, C_ROLE, C_NEGROLE, C_EPS, C_LNEPS, C_ONE = 0, 1, 2, 3, 4, 5, 6
C_POW2 = 8
C_LNP = 40
C_SUBW = 44
C_N1 = 48
C_N2 = 56
NCONST = 64


class Buf:
    __slots__ = ("name", "w", "r", "x")

    def __init__(self, name="b", x=False):
        self.name = name
        self.w = None
        self.r = {}
        self.x = x


class K:
    EPOCH = 30000

    def __init__(self, nc):
        self.nc = nc
        self.eng = {"pe": nc.tensor, "act": nc.scalar, "dve": nc.vector,
                    "pool": nc.gpsimd, "sp": nc.sync}
        self.cnt = {k: 0 for k in self.eng}
        self.sems = {k: [] for k in self.eng}
        self.waited = {k: {} for k in self.eng}
        self.dma_id = 0
        self._dslots = {}
        self.n_inst = 0

    def _sem(self, key, epoch):
        lst = self.sems[key]
        while len(lst) <= epoch:
            lst.append(self.nc.alloc_semaphore(name=f"s_{key}_{len(lst)}"))
        return lst[epoch]

    def _wait(self, e, tok):
        src, c = tok
        if self.waited[e].get(src, 0) >= c:
            return
        ep = (c - 1) // self.EPOCH
        val = c - ep * self.EPOCH
        self.eng[e].wait_ge(self._sem(src, ep), val)
        self.waited[e][src] = c

    def _wait_dma(self, e, tok):
        _, sem, val, uid, slot = tok
        key = ("dma", slot)
        if self.waited[e].get(key, 0) >= val:
            return
        self.eng[e].wait_ge(sem, val)
        self.waited[e][key] = val

    def _dep(self, e, tok):
        if tok is None:
            return
        if tok[0] == "dma":
            self._wait_dma(e, tok)
        else:
            self._wait(e, tok)

    def deps(self, e, reads=(), writes=()):
        for b in reads:
            if b.w is not None:
                self._dep(e, b.w)
        for b in writes:
            if b.w is not None and (b.w[0] != e or e != "pe"):
                self._dep(e, b.w)
            for re_, rc in b.r.items():
                if re_ == e and e == "pe":
                    continue
                if isinstance(re_, tuple):
                    self._wait_dma(e, rc)
                else:
                    self._wait(e, (re_, rc))

    def done(self, e, inst, reads=(), writes=()):
        self.cnt[e] += 1
        c = self.cnt[e]
        ep = (c - 1) // self.EPOCH
        inst.then_inc(self._sem(e, ep), 1)
        tok = (e, c)
        for b in reads:
            b.r[e] = c
        for b in writes:
            b.w = tok
            b.r = {}
        return tok

    def op(self, e, fn, reads=(), writes=()):
        xs = [b for b in reads if b.x and b not in writes]
        if xs:
            reads = [b for b in reads if not b.x]
            writes = list(writes) + xs
        self.deps(e, reads, writes)
        inst = fn()
        self.n_inst += 1
        return self.done(e, inst, reads, writes)

    def dma(self, q, out, in_, reads=(), writes=(), **kw):
        self.deps(q, reads, writes)
        st = self._dslots.setdefault(q, {"sems": [], "vals": [], "i": 0, "pend": []})
        NS = 14
        i = st["i"] % NS
        st["i"] += 1
        if len(st["sems"]) <= i:
            st["sems"].append(self.nc.alloc_semaphore(name=f"d_{q}_{i}"))
            st["vals"].append(0)
            st["pend"].append(None)
        prev = st["pend"][i]
        if prev is not None:
            self._wait_dma(q, prev)
        st["vals"][i] += 16
        self.dma_id += 1
        tok = ("dma", st["sems"][i], st["vals"][i], self.dma_id, (q, i))
        st["pend"][i] = tok
        inst = self.eng[q].dma_start(out=out, in_=in_, **kw)
        inst.then_inc(st["sems"][i], 16)
        for b in writes:
            b.w = tok
            b.r = {}
        for b in reads:
            b.r[("dma", self.dma_id)] = tok
        return tok

    def barrier(self, engines=("pe", "act", "dve", "pool", "sp")):
        for e in engines:
            for f in self.eng:
                if f != e and self.cnt[f] > 0:
                    self._wait(e, (f, self.cnt[f]))
            for q, st in self._dslots.items():
                for p in st["pend"]:
                    if p is not None:
                        self._wait_dma(e, p)

    def finish(self, e="sp"):
        self.barrier(engines=(e,))


def build_program(stage=99, dbg=False):
    nc = bass.Bass("TRN2", target_bir_lowering=False)
    k = K(nc)
    V, S, P, T, G = nc.vector, nc.scalar, nc.gpsimd, nc.tensor, nc.gpsimd

    def din(name, shape, dt=F32):
        return nc.dram_tensor(name, list(shape), dt, kind="ExternalInput").ap()

    x_d = din("x", [L, D])
    pos_d = din("pos", [1, L], I32)
    ct_d = din("cT", [128, 8])
    consts_d = din("consts", [128, NCONST])
    cmat_d = din("cmat", [128, 3, 128])
    lamv_d = din("lamv", [1, 256])
    wada_d = din("w_ada", [D, 6 * D])
    badaT_d = din("b_adaT", [128, 48])
    wfm_d = din("w_fm", [21, 128, 1024])
    wfmr_d = din("w_fm_rot", [21, 128, 1024])
    wtm_d = din("w_tm", [D, 1032])
    wout_d = din("w_out", [D, D])
    wff1_d = din("w_ff1", [8, 128, 8 * 512])
    wff2_d = din("w_ff2", [4 * D, D])
    nfw_d = din("norm_f_w", [1, D])
    out_d = nc.dram_tensor("out", [NOWN, D], F32, kind="ExternalOutput").ap()
    dbg_d = {}

    KB = 1024
    ARENA = 206 * KB
    arena = nc.alloc_sbuf_tensor("arena", [128, ARENA], U8).ap()

    def sb(off, shape, dt):
        esz = {F32: 4, BF16: 2, I32: 4, FP8: 1}[dt]
        n = int(np.prod(shape)) * esz
        assert off + n <= ARENA, (off, n)
        ap = arena[:, off:off + n].bitcast(dt)
        if len(shape) == 2:
            ap = ap.rearrange("p (a b) -> p a b", b=shape[1])
        elif len(shape) == 3:
            ap = ap.rearrange("p (a b c) -> p a b c", b=shape[1], c=shape[2])
        return ap

    banks = [nc.alloc_psum_tensor(f"bank{i}", [128, 512], F32).ap() for i in range(8)]
    BK = [Buf(f"bank{i}", x=True) for i in range(8)]

    def bank_bf(i):
        return banks[i].bitcast(BF16)

    o = 0
    consts = sb(o, [NCONST], F32); o += NCONST * 4
    ident = sb(o, [128], BF16); o += 256
    triT = sb(o, [128], BF16); o += 256
    trineg = sb(o, [128], F32); o += 512
    identf = sb(o, [128], F32); o += 512
    ones_bf = sb(o, [128], BF16); o += 256
    ones_f = sb(o, [128], F32); o += 512
    modT = sb(o, [48], F32); o += 192
    GB = sb(o, [32], F32); o += 128
    scT = sb(o, [8], F32); o += 32
    stat = sb(o, [128], F32); o += 512
    bis = sb(o, [6, 32], F32); o += 768
    iw_sb = sb(o, [16, 8], F32); o += 512
    lam_t = sb(o, [8], F32); o += 32
    assert o <= 6 * KB
    CONST = Buf("const")
    STAT = Buf("stat")
    BIS = Buf("bis")
    IW = Buf("iw")

    def cc(col, n=1):
        return consts[:, col:col + n]

    O_MIX = 6 * KB
    O_K = 38 * KB
    O_V = 70 * KB
    O_KI = 102 * KB
    O_HT = 110 * KB
    O_TAB = 174 * KB
    O_T8 = 190 * KB
    O_TMPA = 22 * KB

    mixT = sb(O_MIX, [8, NOWN], BF16)
    MIX = [Buf(f"mix{i}") for i in range(8)]
    kT = sb(O_K, [4, L], BF16)
    vtm = sb(O_V, [32, 512], BF16)
    kiT = sb(O_KI, [L], BF16)
    hT_own = sb(O_HT, [8, NOWN], BF16)
    hT_oth = sb(O_HT + 32 * KB, [8, NOWN], BF16)
    cosT = sb(O_TAB, [L], BF16)
    sinT = sb(O_TAB + 8 * KB, [L], BF16)
    qT = sb(O_HT + 32 * KB, [4, NOWN], BF16)
    iqT = sb(O_HT + 48 * KB, [4, NOWN], BF16)
    TAB = Buf("tab")
    HT = [[Buf(f"ht{t}_{p}") for p in range(2)] for t in range(32)]
    KT = [[Buf(f"kT{j}_{g}") for g in range(8)] for j in range(4)]
    KI = [Buf(f"ki{g}") for g in range(8)]
    VT = [Buf(f"v{t}") for t in range(32)]
    QT = [[Buf(f"qT{j}_{g}") for g in range(4)] for j in range(4)]
    IQ = [[Buf(f"iq{j}_{g}") for g in range(4)] for j in range(4)]

    def hT_ap(kc, t0, n):
        if t0 < NOWN:
            return hT_own[:, kc, t0:t0 + n]
        return hT_oth[:, kc, t0 - NOWN:t0 - NOWN + n]

    def ht_bufs(t0, n):
        r = []
        for tt in range(t0 // 128, (t0 + n) // 128):
            r += HT[tt]
        return r

    SCR = 38 * KB
    SCR0 = 190 * KB
    k.dma("sp", consts, consts_d, writes=[CONST])
    cm32 = sb(SCR0, [3, 128], F32)
    CM = Buf("cm")
    k.dma("sp", cm32, cmat_d, writes=[CM])
    k.op("dve", lambda: V.tensor_copy(ident, cm32[:, 0, :]), reads=[CM], writes=[CONST])
    k.op("dve", lambda: V.tensor_copy(identf, cm32[:, 0, :]), reads=[CM], writes=[CONST])
    k.op("dve", lambda: V.tensor_copy(triT, cm32[:, 1, :]), reads=[CM], writes=[CONST])
    k.op("dve", lambda: V.tensor_copy(trineg, cm32[:, 2, :]), reads=[CM], writes=[CONST])
    k.op("dve", lambda: V.memset(ones_bf, 1.0), writes=[CONST])
    k.op("dve", lambda: V.memset(ones_f, 1.0 / 128.0), writes=[CONST])
    k.op("dve", lambda: V.memset(stat, 0.0), writes=[STAT])
    ct = sb(SCR0 + 2 * KB, [8], F32)
    CT = Buf("ct")
    k.dma("sp", ct, ct_d, writes=[CT])
    k.op("act", lambda: S.activation(scT, ct, AF.Silu), reads=[CT], writes=[CONST])
    badaT = sb(SCR0 + 3 * KB, [48], F32)
    BA = Buf("ba")
    k.dma("sp", badaT, badaT_d, writes=[BA])
    lv = sb(SCR0 + 4 * KB, [256], F32)
    LV = Buf("lv")
    k.dma("sp", lv, lamv_d.partition_broadcast(128), writes=[LV])
    lj = sb(SCR0 + 5 * KB, [64], F32)
    LJ = Buf("lj")
    LAMB = Buf("lam")
    k.op("dve", lambda: V.memset(lam_t, 0.0), writes=[LAMB])
    k.op("dve", lambda: V.scalar_tensor_tensor(lj, lv[:, 0:64], 1.0, lv[:, 64:128], ALU.mult, ALU.mult,
                                               accum_out=lam_t[:, 0:1]), reads=[LV, LAMB], writes=[LJ, LAMB])
    k.op("dve", lambda: V.scalar_tensor_tensor(lj, lv[:, 128:192], 1.0, lv[:, 192:256], ALU.mult, ALU.mult,
                                               accum_out=lam_t[:, 1:2]), reads=[LV, LAMB], writes=[LJ, LAMB])
    k.op("act", lambda: S.activation(lam_t[:, 2:4], lam_t[:, 0:2], AF.Exp), reads=[LAMB], writes=[LAMB])
    k.op("dve", lambda: V.tensor_tensor(lam_t[:, 4:5], lam_t[:, 3:4], lam_t[:, 2:3], ALU.subtract),
         reads=[LAMB], writes=[LAMB])
    k.op("dve", lambda: V.tensor_scalar(lam_t[:, 5:6], lam_t[:, 4:5], -LAM_INIT, None, ALU.add),
         reads=[LAMB], writes=[LAMB])
    neglam = lam_t[:, 5:6]

    WAB = [Buf("wa0"), Buf("wa1")]
    wa = [sb(110 * KB + i * 32 * KB, [8, 1024], F32) for i in range(2)]
    wada_v = wada_d.rearrange("(kc p) c -> p kc c", p=128)
    for blk in range(6):
        b = blk % 2
        k.dma("sp", wa[b], wada_v[:, :, blk * 1024:(blk + 1) * 1024], writes=[WAB[b]])

        def mm_mod(b=b, blk=blk):
            inst = None
            for j in range(8):
                col = blk * 8 + j
                for kc in range(8):
                    inst = T.matmul(banks[0][:, col:col + 1], wa[b][:, kc, j * 128:(j + 1) * 128],
                                    scT[:, kc:kc + 1], start=(kc == 0), stop=(kc == 7))
            return inst
        k.op("pe", mm_mod, reads=[WAB[b], CONST], writes=[BK[0]])
    _early = []

    def make_tables():
        posi = sb(SCR, [L], I32)
        a0 = sb(SCR + 16 * KB, [L], F32)
        q_ = sb(SCR + 32 * KB, [L], F32)
        m_ = sb(SCR + 48 * KB, [L], F32)
        PI_, A0, Q_, M_ = Buf(), Buf(), Buf(), Buf()
        k.dma("sp", posi, pos_d.partition_broadcast(128), writes=[PI_])
        k.op("dve", lambda: V.tensor_copy(a0, posi), reads=[PI_], writes=[A0])
        k.op("dve", lambda: V.tensor_scalar(a0, a0, cc(C_INVF), None, ALU.mult), reads=[A0, CONST], writes=[A0])
        for which in range(2):
            if which == 1:
                k.op("dve", lambda: V.tensor_scalar(a0, a0, float(np.pi / 2), None, ALU.add), reads=[A0], writes=[A0])
            k.op("dve", lambda: V.tensor_scalar(q_, a0, 1.0 / TWO_PI, None, ALU.mult), reads=[A0], writes=[Q_])
            k.op("dve", lambda: V.tensor_copy(posi, q_), reads=[Q_], writes=[PI_])
            k.op("dve", lambda: V.tensor_copy(q_, posi), reads=[PI_], writes=[Q_])
            k.op("dve", lambda: V.scalar_tensor_tensor(q_, q_, -TWO_PI, a0, ALU.mult, ALU.add), reads=[Q_, A0], writes=[Q_])
            k.op("dve", lambda: V.tensor_scalar(m_, q_, float(np.pi), -TWO_PI, ALU.is_gt, ALU.mult), reads=[Q_], writes=[M_])
            k.op("dve", lambda: V.tensor_tensor(q_, q_, m_, ALU.add), reads=[Q_, M_], writes=[Q_])
            k.op("dve", lambda: V.tensor_scalar(q_, q_, 3.141592, -3.141592, ALU.min, ALU.max), reads=[Q_], writes=[Q_])
            if which == 0:
                k.op("act", lambda: S.activation(sinT, q_, AF.Sin, scale=cc(C_SGN)), reads=[Q_, CONST], writes=[TAB])
            else:
                k.op("act", lambda: S.activation(cosT, q_, AF.Sin), reads=[Q_], writes=[TAB])
        k.barrier()

    def build_hT():
        xt = [sb(O_T8 + 0, [D], F32), sb(O_TMPA, [D], F32), sb(O_T8 + 4 * KB, [D], F32)]
        xn = [sb(O_TMPA + 4 * KB, [D], BF16), sb(O_TMPA + 6 * KB, [D], BF16), sb(O_TMPA + 12 * KB, [D], BF16)]
        junk = sb(O_TMPA + 8 * KB, [D], F32)
        XT, XN, JK = [Buf(), Buf(), Buf()], [Buf(), Buf(), Buf()], Buf()
        SQ = [Buf(), Buf(), Buf()]
        k.op("dve", lambda: V.memset(stat, 0.0), reads=[STAT], writes=[STAT])

        def stage_a(tt):
            b = tt % 3
            k.dma("sp", xt[b], x_d[tt * 128:(tt + 1) * 128, :], writes=[XT[b]])
            k.op("act", lambda: S.activation(junk, xt[b], AF.Square, accum_out=stat[:, tt:tt + 1]),
                 reads=[XT[b], STAT], writes=[JK, STAT])
            k.op("act", lambda: S.activation(stat[:, 32 + tt:33 + tt], stat[:, tt:tt + 1], AF.Sqrt,
                                             bias=cc(C_EPS), scale=1.0 / D), reads=[STAT, CONST], writes=[SQ[b]])

        def stage_a2(tt):
            b = tt % 3
            k.op("dve", lambda: V.reciprocal(stat[:, 64 + tt:65 + tt], stat[:, 32 + tt:33 + tt]),
                 reads=[SQ[b]], writes=[STAT])
            k.op("dve", lambda: V.tensor_scalar(xn[b], xt[b], stat[:, 64 + tt:65 + tt], None, ALU.mult),
                 reads=[XT[b], STAT], writes=[XN[b]])

        def stage_b(tt):
            b = tt % 3
            pbs = (4 + 2 * (tt % 2), 5 + 2 * (tt % 2))
            psbs = [bank_bf(pb_).rearrange("p (a b) -> p a b", b=128) for pb_ in pbs]

            def tps():
                inst = None
                for kc in range(8):
                    inst = T.transpose(psbs[kc // 4][:, kc % 4, :], xn[b][:, kc * 128:(kc + 1) * 128], ident)
                return inst
            k.op("pe", tps, reads=[XN[b], CONST], writes=[BK[pbs[0]], BK[pbs[1]]])
            for kc in range(8):
                dst = hT_ap(kc, tt * 128, 128)
                src = psbs[kc // 4][:, kc % 4, :]
                if kc < 4:
                    k.op("act", lambda: S.activation(dst, src, AF.Identity, bias=GB[:, 8 + kc:9 + kc], scale=GB[:, kc:kc + 1]),
                         reads=[BK[pbs[0]], CONST], writes=[HT[tt][0]])
                else:
                    k.op("dve", lambda: V.tensor_scalar(dst, src, GB[:, kc:kc + 1], GB[:, 8 + kc:9 + kc], ALU.mult, ALU.add),
                         reads=[BK[pbs[1]], CONST], writes=[HT[tt][1]])
        for t in range(33):
            if t < 32:
                stage_a(t)
            if t >= 1:
                stage_b(t - 1)
            if t < 32:
                stage_a2(t)

    wb_i = [0]

    def proj_rope(chunk, dest_fn, dest_bufs, tgs, ki_mode=False):
        i = wb_i[0] % 2
        wb_i[0] += 1
        w0 = sb(O_T8 + i * 4 * KB, [8, 128], BF16)
        w1 = sb(O_T8 + i * 4 * KB + 2 * KB, [8, 128], BF16)
        WB = proj_rope.WB[i]
        k.dma("pool", w0, wfm_d[chunk].rearrange("p (a b) -> p a b", b=128), writes=[WB])
        k.dma("pool", w1, wfmr_d[chunk].rearrange("p (a b) -> p a b", b=128), writes=[WB])
        for gi, tg in enumerate(tgs):
            pa, pr = (0, 1) if proj_rope.par == 0 else (2, 3)
            proj_rope.par ^= 1
            t0 = tg * 512

            def mms(pa=pa, pr=pr, t0=t0):
                inst = None
                for kc in range(8):
                    T.matmul(banks[pa], w0[:, kc, :], hT_ap(kc, t0, 512), start=(kc == 0), stop=(kc == 7))
                for kc in range(8):
                    inst = T.matmul(banks[pr], w1[:, kc, :], hT_ap(kc, t0, 512), start=(kc == 0), stop=(kc == 7))
                return inst
            k.op("pe", mms, reads=[WB] + ht_bufs(t0, 512), writes=[BK[pa], BK[pr]])
            tb = proj_rope.tb
            proj_rope.tb ^= 1
            t1 = sb(O_TMPA + tb * 4 * KB, [512], F32)
            t2 = sb(O_TMPA + tb * 4 * KB + 2 * KB, [512], F32)
            T1, T2 = proj_rope.T1[tb], proj_rope.T2[tb]
            cs = cosT[:, t0:t0 + 512]
            sn = sinT[:, t0:t0 + 512]
            dst = dest_fn(tg)
            if not ki_mode:
                k.op("dve", lambda: V.tensor_tensor(t1, banks[pa], cs, ALU.mult), reads=[BK[pa], TAB], writes=[T1])
                k.op("dve", lambda: V.tensor_tensor(t2, banks[pr], sn, ALU.mult), reads=[BK[pr], TAB], writes=[T2])
                k.op("dve", lambda: V.tensor_tensor(dst, t1, t2, ALU.add), reads=[T1, T2], writes=[dest_bufs[gi]])
            else:
                KIS = proj_rope.KIS
                kb = 8 * KB
                psb_ = sb(SCRK + 0 * kb // 4, [512], F32)
                psq = sb(SCRK + 1 * kb // 4, [512], F32)
                mean = sb(SCRK + 2 * kb // 4, [512], F32)
                var = sb(SCRK + 3 * kb // 4, [512], F32)
                yy = sb(SCRK + 4 * kb // 4, [512], F32)
                yr = sb(SCRK + 5 * kb // 4, [512], F32)
                k.op("act", lambda: S.copy(psb_, banks[pa]), reads=[BK[pa]], writes=[KIS[0]])
                k.op("act", lambda: S.activation(psq, banks[pa], AF.Square), reads=[BK[pa]], writes=[KIS[1]])

                def mm2():
                    T.matmul(banks[4], ones_f, psb_, start=True, stop=True)
                    return T.matmul(banks[5], ones_f, psq, start=True, stop=True)
                k.op("pe", mm2, reads=[KIS[0], KIS[1], CONST], writes=[BK[4], BK[5]])
                k.op("act", lambda: S.copy(mean, banks[4]), reads=[BK[4]], writes=[KIS[2]])
                k.op("dve", lambda: V.tensor_tensor(var, mean, mean, ALU.mult), reads=[KIS[2]], writes=[KIS[3]])
                k.op("dve", lambda: V.tensor_tensor(var, banks[5], var, ALU.subtract), reads=[BK[5], KIS[3]], writes=[KIS[3]])
                k.op("act", lambda: S.activation(var, var, AF.Sqrt, bias=cc(C_LNEPS), scale=1.0), reads=[KIS[3], CONST], writes=[KIS[3]])
                k.op("dve", lambda: V.reciprocal(var, var), reads=[KIS[3]], writes=[KIS[3]])
                k.op("dve", lambda: V.tensor_tensor(yy, psb_, mean, ALU.subtract), reads=[KIS[0], KIS[2]], writes=[KIS[4]])
                k.op("dve", lambda: V.tensor_tensor(yy, yy, var, ALU.mult), reads=[KIS[4], KIS[3]], writes=[KIS[4]])
                k.op("dve", lambda: V.tensor_scalar(yy, yy, cc(C_LNP), cc(C_LNP + 1), ALU.mult, ALU.add), reads=[KIS[4], CONST], writes=[KIS[4]])
                k.op("dve", lambda: V.tensor_tensor(yr, banks[pr], mean, ALU.subtract), reads=[BK[pr], KIS[2]], writes=[KIS[5]])
                k.op("dve", lambda: V.tensor_tensor(yr, yr, var, ALU.mult), reads=[KIS[5], KIS[3]], writes=[KIS[5]])
                k.op("dve", lambda: V.tensor_scalar(yr, yr, cc(C_LNP + 2), cc(C_LNP + 3), ALU.mult, ALU.add), reads=[KIS[5], CONST], writes=[KIS[5]])
                k.op("dve", lambda: V.tensor_tensor(t1, yy, cs, ALU.mult), reads=[KIS[4], TAB], writes=[T1])
                k.op("dve", lambda: V.tensor_tensor(t2, yr, sn, ALU.mult), reads=[KIS[5], TAB], writes=[T2])
                k.op("dve", lambda: V.tensor_tensor(dst, t1, t2, ALU.add), reads=[T1, T2], writes=[dest_bufs[gi]])
    proj_rope.WB = [Buf(), Buf()]
    proj_rope.par = 0
    proj_rope.tb = 0
    proj_rope.T1 = [Buf(), Buf()]
    proj_rope.T2 = [Buf(), Buf()]
    proj_rope.KIS = [Buf() for _ in range(6)]
    SCRK = O_MIX

    def proj_v(col0):
        wv = sb(O_T8, [8, 512], BF16)
        WV = Buf()
        k.dma("pool", wv, wtm_d.rearrange("(kc p) c -> p kc c", p=128)[:, :, col0:col0 + 512], writes=[WV])
        for tt in range(32):
            pb = 4 + tt % 2

            def mms(tt=tt, pb=pb):
                inst = None
                for kc in range(8):
                    inst = T.matmul(banks[pb], hT_ap(kc, tt * 128, 128), wv[:, kc, :], start=(kc == 0), stop=(kc == 7))
                return inst
            k.op("pe", mms, reads=[WV] + HT[tt], writes=[BK[pb]])
            if tt % 2 == 0:
                k.op("act", lambda tt=tt, pb=pb: S.copy(vtm[:, tt, :], banks[pb]), reads=[BK[pb]], writes=[VT[tt]])
            else:
                k.op("dve", lambda tt=tt, pb=pb: V.tensor_copy(vtm[:, tt, :], banks[pb]), reads=[BK[pb]], writes=[VT[tt]])

    def dump(name, ap, bufs, shape, dt=F32):
        d = nc.dram_tensor(name, list(shape), dt, kind="ExternalOutput").ap()
        dbg_d[name] = d
        k.dma("sp", d, ap, reads=bufs)

    make_tables()
    k.op("dve", lambda: V.tensor_tensor(modT, banks[0][:, 0:48], badaT, ALU.add), reads=[BK[0], BA], writes=[CONST])
    k.op("dve", lambda: V.scalar_tensor_tensor(GB[:, 0:8], modT[:, 8:16], 1.0, cc(C_N1, 8), ALU.add, ALU.mult),
         reads=[CONST], writes=[CONST])
    k.op("dve", lambda: V.tensor_copy(GB[:, 8:16], modT[:, 0:8]), reads=[CONST], writes=[CONST])
    k.op("dve", lambda: V.scalar_tensor_tensor(GB[:, 16:24], modT[:, 32:40], 1.0, cc(C_N2, 8), ALU.add, ALU.mult),
         reads=[CONST], writes=[CONST])
    k.op("dve", lambda: V.tensor_copy(GB[:, 24:32], modT[:, 24:32]), reads=[CONST], writes=[CONST])
    k.barrier()
    if stage == 0:
        dump("d_modT", modT, [CONST], [128, 48], F32)
        dump("d_lam", lam_t, [LAMB], [128, 8], F32)
        k.finish("sp")
        return nc, dbg_d
    if stage == 0.5:
        dump("d_cos", cosT, [TAB], [128, L], BF16)
        dump("d_sin", sinT, [TAB], [128, L], BF16)
        k.finish("sp")
        return nc, dbg_d
    build_hT()
    k.barrier()
    if stage == 0.7:
        dump("d_hT", hT_own, sum(HT[:16], []), [128, 8, NOWN], BF16)
        k.finish("sp")
        return nc, dbg_d
    for j in range(4):
        proj_rope(j, lambda tg, j=j: kT[:, j, tg * 512:(tg + 1) * 512], KT[j], list(range(8)))
    proj_rope(4, lambda tg: kiT[:, tg * 512:(tg + 1) * 512], KI, list(range(8)), ki_mode=True)
    k.barrier()
    proj_v(0)
    k.barrier()
    for j in range(4):
        proj_rope(5 + j, lambda tg, j=j: qT[:, j, tg * 512:(tg + 1) * 512], QT[j], list(range(4)))
    for j in range(4):
        proj_rope(9 + j, lambda tg, j=j: iqT[:, j, tg * 512:(tg + 1) * 512], IQ[j], list(range(4)))
    k.barrier()
    wiw = sb(O_T8, [8, 8], BF16)
    WIW = Buf()
    k.dma("pool", wiw, wtm_d.rearrange("(kc p) c -> p kc c", p=128)[:, :, 1024:1032], writes=[WIW])
    for tt in range(16):
        def mms(tt=tt):
            inst = None
            for kc in range(8):
                inst = T.matmul(banks[4][:, tt * 8:(tt + 1) * 8], hT_ap(kc, tt * 128, 128), wiw[:, kc, :],
                                start=(kc == 0), stop=(kc == 7))
            return inst
        k.op("pe", mms, reads=[WIW] + HT[tt], writes=[BK[4]])
    k.op("dve", lambda: V.tensor_scalar(iw_sb.rearrange("p a b -> p (a b)"), banks[4][:, 0:128],
                                        float(8 ** -0.5 * 64 ** -0.5), None, ALU.mult), reads=[BK[4]], writes=[IW])

    if stage == 1:
        dump("d_hT", hT_own, sum(HT[:16], []), [128, 8, NOWN], BF16)
        dump("d_kT", kT, sum(KT, []), [128, 4, L], BF16)
        dump("d_kiT", kiT, KI, [128, L], BF16)
        dump("d_v", vtm, VT, [128, 32, 512], BF16)
        dump("d_qT", qT, sum(QT, []), [128, 4, NOWN], BF16)
        dump("d_iqT", iqT, sum(IQ, []), [128, 4, NOWN], BF16)
        dump("d_iw", iw_sb, [IW], [128, 16, 8], F32)
        dump("d_modT", modT, [CONST], [128, 48], F32)
        dump("d_lam", lam_t, [LAMB], [128, 8], F32)
        k.finish("sp")
        return nc, dbg_d
    k.barrier()

    MT = sb(O_HT, [32, 512], BF16)
    Ibufs = [sb(O_TAB, [L], F32), sb(O_T8, [L], F32)]
    JUNK = sb(O_TMPA + 8 * KB, [L], FP8)
    Mh = sb(O_TMPA + 8 * KB, [NOWN], BF16)
    NEB = 6
    EB = [sb(O_TMPA + i * KB, [512], BF16) for i in range(4)] + [sb(O_TMPA + (12 + i) * KB, [512], BF16) for i in range(2)]
    RB = [sb(O_TMPA + 4 * KB + i * 2 * KB, [512], F32) for i in range(2)]
    REC = [sb(O_TMPA + 14 * KB, [512], F32)] * 2
    MTB, MB = Buf("MT"), Buf("M")
    IBS = [Buf("I0"), Buf("I1")]
    EBB = [Buf() for _ in range(6)]
    RBB = [Buf() for _ in range(2)]
    RECB = [Buf("rec")] * 2
    nm, sacc, Hh, nH, uu = bis[:, 0, :], bis[:, 1, :], bis[:, 2, :], bis[:, 3, :], bis[:, 4, :]
    ectr = [0]
    rctr = [0]
    sctr = [0]

    def n_icomp(i):
        return 2 * ((i + 1) * 128 + 511) // 512 * 0 + 2 * (((i + 1) * 128 + 511) // 512) * 8

    def gen_icomp(i):
        Ibuf, IB = Ibufs[i % 2], IBS[i % 2]
        n1 = (i + 1) * 128
        for (ks, c0) in ((0, 0), (NOWN, n1)):
            for p0 in range(0, n1, 512):
                n = min(512, n1 - p0)
                for h in range(8):
                    pb = sctr[0] % 3
                    sctr[0] += 1
                    hp, hh = h // 2, h % 2
                    k.op("pe", lambda: T.matmul(
                        banks[pb][:, 0:n], iqT[hh * 64:(hh + 1) * 64, hp, i * 128:(i + 1) * 128],
                        kiT[hh * 64:(hh + 1) * 64, ks + p0:ks + p0 + n], start=True, stop=True),
                        reads=[IQ[hp][i // 4], KI[(ks + p0) // 512]], writes=[BK[pb]])
                    rb = rctr[0] % 2
                    rctr[0] += 1
                    dst = Ibuf[:, c0 + p0:c0 + p0 + n]
                    wcol = iw_sb[:, i, h:h + 1]
                    if h == 0:
                        k.op("dve", lambda: V.tensor_scalar(dst, banks[pb][:, 0:n], 0.0, wcol, ALU.max, ALU.mult),
                             reads=[BK[pb], IW], writes=[IB])
                    elif h % 2 == 1:
                        k.op("dve", lambda: V.tensor_scalar(RB[rb][:, 0:n], banks[pb][:, 0:n], 0.0, wcol, ALU.max, ALU.mult),
                             reads=[BK[pb], IW], writes=[RBB[rb]])
                        k.op("dve", lambda: V.tensor_tensor(dst, dst, RB[rb][:, 0:n], ALU.add),
                             reads=[RBB[rb], IB], writes=[IB])
                    else:
                        k.op("act", lambda: S.activation(RB[rb][:, 0:n], banks[pb][:, 0:n], AF.Relu),
                             reads=[BK[pb]], writes=[RBB[rb]])
                        k.op("dve", lambda: V.scalar_tensor_tensor(dst, RB[rb][:, 0:n], wcol, dst, ALU.mult, ALU.add),
                             reads=[RBB[rb], IW, IB], writes=[IB])
                    yield

    N_POST = NIT + 4

    def gen_post(i, g):
        Ibuf, IB = Ibufs[i % 2], IBS[i % 2]
        n1 = (i + 1) * 128
        j = i - 4 * g
        W2 = 2 * n1
        Iv = Ibuf[:, 0:W2]
        k.op("dve", lambda: V.tensor_reduce(stat[:, 100:101], Iv, AX.X, ALU.max, apply_absolute_value=True),
             reads=[IB, STAT], writes=[STAT])
        k.op("dve", lambda: V.tensor_tensor(Ibuf[:, i * 128:(i + 1) * 128], Ibuf[:, i * 128:(i + 1) * 128], trineg, ALU.add),
             reads=[IB, CONST], writes=[IB])
        k.op("dve", lambda: V.tensor_scalar(Ibuf[:, n1 + i * 128:n1 + (i + 1) * 128], Ibuf[:, n1 + i * 128:n1 + (i + 1) * 128],
                                            cc(C_NEGROLE), None, ALU.add), reads=[IB, CONST], writes=[IB])
        k.op("dve", lambda: V.tensor_reduce(stat[:, 101:102], Iv, AX.X, ALU.max), reads=[IB, STAT], writes=[STAT])
        yield
        k.op("dve", lambda: V.tensor_tensor(stat[:, 102:103], stat[:, 101:102], stat[:, 100:101], ALU.add),
             reads=[STAT], writes=[STAT])
        k.op("dve", lambda: V.memset(stat[:, 105:106], float(W2 - 2 * TOPK + 1)), reads=[STAT], writes=[STAT])
        k.op("dve", lambda: V.tensor_scalar(Hh[:, 0:NIT + 1], cc(C_POW2, NIT + 1), stat[:, 102:103], None, ALU.mult),
             reads=[STAT, CONST, BIS], writes=[BIS])
        k.op("dve", lambda: V.tensor_scalar(nH[:, 0:NIT + 1], cc(C_POW2, NIT + 1), stat[:, 102:103], -1.0, ALU.mult, ALU.mult),
             reads=[STAT, CONST, BIS], writes=[BIS])
        k.op("dve", lambda: V.tensor_tensor(nm[:, 0:1], stat[:, 100:101], Hh[:, 0:1], ALU.subtract),
             reads=[BIS, STAT], writes=[BIS])
        k.op("dve", lambda: V.memset(sacc, 0.0), reads=[BIS], writes=[BIS])
        yield
        for it in range(NIT):
            k.op("act", lambda: S.activation(JUNK[:, 0:W2], Iv, AF.Sign, bias=nm[:, it:it + 1], scale=1.0,
                                             accum_out=sacc[:, it:it + 1], saturate=False), reads=[IB, BIS, MB], writes=[MB, BIS])
            k.op("act", lambda: S.activation(uu[:, it:it + 1], sacc[:, it:it + 1], AF.Sign, bias=stat[:, 105:106], scale=1.0),
                 reads=[BIS, STAT], writes=[BIS])
            k.op("act", lambda: S.activation(nm[:, it + 1:it + 2], uu[:, it:it + 1], AF.Identity,
                                             bias=nm[:, it:it + 1], scale=nH[:, it + 1:it + 2]), reads=[BIS], writes=[BIS])
            yield
        k.op("dve", lambda: V.tensor_scalar(stat[:, 104:105], nm[:, NIT:NIT + 1], -1.0, Hh[:, NIT:NIT + 1], ALU.mult, ALU.subtract),
             reads=[BIS, STAT], writes=[STAT])
        for (c0, slot0) in ((0, 0), (n1, 4 * g + 4)):
            k.op("dve", lambda: V.tensor_scalar(Mh[:, 0:n1], Ibuf[:, c0:c0 + n1], stat[:, 104:105], None, ALU.is_ge),
                 reads=[IB, STAT, MB], writes=[MB])
            for cb in range(0, i + 1, 8):
                nb = min(8, i + 1 - cb)
                pb = 3 + (sctr[0] % 2)
                sctr[0] += 1
                psb = bank_bf(pb).rearrange("p (a b) -> p a b", b=128)

                def tps():
                    inst = None
                    for q in range(nb):
                        inst = T.transpose(psb[:, q, :], Mh[:, (cb + q) * 128:(cb + q + 1) * 128], ident)
                    return inst
                k.op("pe", tps, reads=[MB, CONST], writes=[BK[pb]])
                k.op("act", lambda: S.copy(MT[:, slot0 + cb:slot0 + cb + nb, j * 128:(j + 1) * 128], psb[:, 0:nb, :]),
                     reads=[BK[pb]], writes=[MTB])
            yield

    def interleave(ga, na, gb, nb_):
        ia = ib = 0
        da = db = False
        while not (da and db):
            if not da and (db or ia * max(nb_, 1) <= ib * max(na, 1)):
                try:
                    next(ga); ia += 1
                except StopIteration:
                    da = True
            elif not db:
                try:
                    next(gb); ib += 1
                except StopIteration:
                    db = True

    def attn_slots(g):
        r = []
        for c in range(4 * g + 4):
            r.append((c * 128, c, max(0, c - 4 * g), "own"))
        for c in range(4 * g + 4):
            r.append((NOWN + c * 128, 4 * g + 4 + c, max(0, c - 4 * g), "oth"))
        return r

    LAG = 2

    def dsa_attention(g):
        slots = attn_slots(g)
        items = [(p, si) for p in range(4) for si in range(len(slots))]
        st = {}

        def stage_a(t):
            p, si = items[t]
            ks, slot, j0, kind = slots[si]
            c0 = j0 * 128
            pbs = []
            for hh in range(2):
                pb = sctr[0] % 3
                sctr[0] += 1
                pbs.append(pb)

            def qk():
                inst = None
                for hh in range(2):
                    inst = T.matmul(banks[pbs[hh]][:, c0:512], kT[hh * 64:(hh + 1) * 64, p, ks:ks + 128],
                                    qT[hh * 64:(hh + 1) * 64, p, g * 512 + c0:(g + 1) * 512], start=True, stop=True)
                return inst
            k.op("pe", qk, reads=[KT[p][ks // 512], QT[p][g]], writes=[BK[pbs[0]], BK[pbs[1]]])
            ebs = []
            for hh in range(2):
                eb = ectr[0] % NEB
                ectr[0] += 1
                ebs.append(eb)
                k.op("act", lambda: S.activation(EB[eb][:, c0:512], banks[pbs[hh]][:, c0:512], AF.Exp, scale=0.125),
                     reads=[BK[pbs[hh]]], writes=[EBB[eb]])
                k.op("dve", lambda: V.tensor_tensor(EB[eb][:, c0:512], EB[eb][:, c0:512], MT[:, slot, c0:512], ALU.mult),
                     reads=[EBB[eb], MTB], writes=[EBB[eb]])
            st[t] = ebs

        def stage_b(t):
            p, si = items[t]
            ks, slot, j0, kind = slots[si]
            c0 = j0 * 128
            kc = ks // 128
            ebs = st.pop(t)
            ob, sbk = (4, 5) if p % 2 == 0 else (6, 7)
            first, last = (si == 0), (si == len(slots) - 1)

            def pv():
                inst = None
                for hh in range(2):
                    T.matmul(banks[ob][hh * 64:(hh + 1) * 64, c0:512], vtm[:, kc, (2 * p + hh) * 64:(2 * p + hh + 1) * 64],
                             EB[ebs[hh]][:, c0:512], start=first, stop=last)
                for hh in range(2):
                    inst = T.matmul(banks[sbk][hh * 64:(hh + 1) * 64, c0:512], ones_bf[:, 0:64],
                                    EB[ebs[hh]][:, c0:512], start=first, stop=last)
                return inst
            k.op("pe", pv, reads=[EBB[ebs[0]], EBB[ebs[1]], VT[kc], CONST], writes=[BK[ob], BK[sbk]])
            if last:
                rc = p % 2
                k.op("dve", lambda: V.reciprocal(REC[rc], banks[sbk]), reads=[BK[sbk]], writes=[RECB[rc]])
                k.op("dve", lambda: V.tensor_tensor(mixT[:, p, g * 512:(g + 1) * 512], banks[ob], REC[rc], ALU.mult),
                     reads=[BK[ob], RECB[rc]], writes=[MIX[p]])
        n = len(items)
        for t in range(n + LAG):
            if t < n:
                stage_a(t)
            if t >= LAG:
                stage_b(t - LAG)
            yield

    def run(gen):
        for _ in gen:
            pass

    run(gen_icomp(0))
    for i in range(16):
        g = i // 4
        if i + 1 < 16:
            interleave(gen_post(i, g), N_POST, gen_icomp(i + 1), n_icomp(i + 1))
        else:
            run(gen_post(i, g))
        if i % 4 == 3:
            run(dsa_attention(g))
            if stage == 2 and g == 1:
                dump("d_MT", MT, [MTB], [128, 32, 512], BF16)
                dump("d_mix", mixT, MIX, [128, 8, NOWN], BF16)
                k.finish("sp")
                return nc, dbg_d
    k.barrier()

    make_tables()
    build_hT()
    k.barrier()
    for j in range(4):
        proj_rope(13 + j, lambda tg, j=j: kT[:, j, tg * 512:(tg + 1) * 512], KT[j], list(range(8)))
    k.barrier()
    proj_v(512)
    k.barrier()
    for j in range(4):
        proj_rope(17 + j, lambda tg, j=j: qT[:, j, tg * 512:(tg + 1) * 512], QT[j], list(range(4)))
    k.barrier()

    FT = [sb(O_HT + i * 2 * KB, [512], F32) for i in range(8)]
    FTB = [Buf() for _ in range(8)]
    EB = [sb(O_HT + 16 * KB + i * KB, [512], BF16) for i in range(6)]

    EACC4 = [[sb(O_HT + 24 * KB + (2 * hp + i) * 2 * KB, [512], F32) for i in range(2)] for hp in range(2)]
    EACCB4 = [[Buf(), Buf()], [Buf(), Buf()]]

    def diff_attention(g):
        slots = attn_slots(g)
        items = [(h, si) for h in range(4) for si in range(len(slots))]
        st = {}

        def stage_a(t):
            h, si = items[t]
            EACC, EACCB = EACC4[h % 2], EACCB4[h % 2]
            ks, slot, j0, kind = slots[si]
            c0 = j0 * 128
            c_abs = (ks % NOWN) // 128
            pbs = []
            for c in range(2):
                pbs.append(sctr[0] % 3)
                sctr[0] += 1

            def qk():
                inst = None
                for c in range(2):
                    inst = T.matmul(banks[pbs[c]][:, c0:512], kT[c * 64:(c + 1) * 64, h, ks:ks + 128],
                                    qT[c * 64:(c + 1) * 64, h, g * 512 + c0:(g + 1) * 512], start=True, stop=True)
                return inst
            k.op("pe", qk, reads=[KT[h][ks // 512], QT[h][g]], writes=[BK[pbs[0]], BK[pbs[1]]])
            ebs = []
            for c in range(2):
                eb = ectr[0] % NEB
                ectr[0] += 1
                ebs.append(eb)
                k.op("act", lambda: S.activation(EB[eb][:, c0:512], banks[pbs[c]][:, c0:512], AF.Exp, scale=0.125),
                     reads=[BK[pbs[c]]], writes=[EBB[eb]])
                if c_abs >= 4 * g:
                    blk = EB[eb][:, c0:c0 + 128]
                    if kind == "own":
                        k.op("dve", lambda: V.tensor_tensor(blk, blk, triT, ALU.mult), reads=[EBB[eb], CONST], writes=[EBB[eb]])
                    else:
                        k.op("dve", lambda: V.tensor_scalar(blk, blk, cc(C_ROLE), None, ALU.mult), reads=[EBB[eb], CONST], writes=[EBB[eb]])
                if si == 0:
                    k.op("dve", lambda: V.tensor_copy(EACC[c], EB[eb]), reads=[EBB[eb]], writes=[EACCB[c]])
                else:
                    k.op("dve", lambda: V.tensor_tensor(EACC[c][:, c0:512], EACC[c][:, c0:512], EB[eb][:, c0:512], ALU.add),
                         reads=[EBB[eb], EACCB[c]], writes=[EACCB[c]])
            st[t] = ebs

        def stage_b(t):
            h, si = items[t]
            ks, slot, j0, kind = slots[si]
            c0 = j0 * 128
            kc = ks // 128
            ebs = st.pop(t)
            EACC, EACCB = EACC4[h % 2], EACCB4[h % 2]
            first, last = (si == 0), (si == len(slots) - 1)

            def pv():
                inst = None
                for c in range(2):
                    inst = T.matmul(banks[3 + c][:, c0:512], vtm[:, kc, h * 128:(h + 1) * 128], EB[ebs[c]][:, c0:512],
                                    start=first, stop=last)
                return inst
            k.op("pe", pv, reads=[EBB[ebs[0]], EBB[ebs[1]], VT[kc], CONST], writes=[BK[3], BK[4]])
            if not last:
                return
            def sm():
                T.matmul(banks[5], ones_f, EACC[0], start=True, stop=True)
                return T.matmul(banks[6], ones_f, EACC[1], start=True, stop=True)
            k.op("pe", sm, reads=[EACCB[0], EACCB[1], CONST], writes=[BK[5], BK[6]])
            k.op("dve", lambda: V.reciprocal(FT[0], banks[5]), reads=[BK[5]], writes=[FTB[0]])
            k.op("dve", lambda: V.reciprocal(FT[1], banks[6]), reads=[BK[6]], writes=[FTB[1]])
            k.op("dve", lambda: V.scalar_tensor_tensor(FT[2], banks[3], 1.0 / 128.0, FT[0], ALU.mult, ALU.mult), reads=[BK[3], FTB[0]], writes=[FTB[2]])
            k.op("dve", lambda: V.scalar_tensor_tensor(FT[3], banks[4], 1.0 / 128.0, FT[1], ALU.mult, ALU.mult), reads=[BK[4], FTB[1]], writes=[FTB[3]])
            k.op("dve", lambda: V.scalar_tensor_tensor(FT[4], FT[3], neglam, FT[2], ALU.mult, ALU.add),
                 reads=[FTB[3], FTB[2], LAMB], writes=[FTB[4]])
            k.op("act", lambda: S.activation(FT[5], FT[4], AF.Square), reads=[FTB[4]], writes=[FTB[5]])
            k.op("pe", lambda: T.matmul(banks[7], ones_f, FT[5], start=True, stop=True), reads=[FTB[5], CONST], writes=[BK[7]])
            k.op("act", lambda: S.activation(FT[6], banks[7], AF.Sqrt, bias=cc(C_EPS), scale=1.0), reads=[BK[7], CONST], writes=[FTB[6]])
            k.op("dve", lambda: V.reciprocal(FT[6], FT[6]), reads=[FTB[6]], writes=[FTB[6]])
            k.op("dve", lambda: V.scalar_tensor_tensor(mixT[:, 4 + h, g * 512:(g + 1) * 512], FT[4], cc(C_SUBW), FT[6],
                                                       ALU.mult, ALU.mult), reads=[FTB[4], FTB[6], CONST], writes=[MIX[4 + h]])
        n = len(items)
        for t in range(n + LAG):
            if t < n:
                stage_a(t)
            if t >= LAG:
                stage_b(t - LAG)
            yield

    for g in range(4):
        run(diff_attention(g))
    if stage == 3:
        dump("d_mix", mixT, MIX, [128, 8, NOWN], BF16)
        k.finish("sp")
        return nc, dbg_d
    k.barrier()

    O_WO = 38 * KB
    O_X1 = 54 * KB
    O_H2 = 118 * KB
    O_C = 150 * KB
    wout = sb(O_WO, [8, D], BF16)
    x1 = sb(O_X1, [16, D], F32)
    h2T = sb(O_H2, [8, NOWN], BF16)
    gbc = [sb(O_C + i * 4 * KB, [D], F32) for i in range(2)]
    nfw = sb(O_C + 8 * KB, [D], F32)
    WO, GBC, NFW = Buf(), Buf(), Buf()
    X1 = [Buf() for _ in range(16)]
    H2 = [[Buf(), Buf()] for _ in range(16)]
    k.dma("pool", wout, wout_d.rearrange("(c p) n -> p c n", p=128), writes=[WO])
    k.dma("sp", nfw, nfw_d.partition_broadcast(128), writes=[NFW])
    dg = sb(O_C + 12 * KB, [128], F32)
    DG = Buf()
    onesf1 = sb(O_C + 13 * KB, [128], F32)
    k.op("dve", lambda: V.memset(onesf1, 1.0), writes=[DG])
    for which, base in ((0, 16), (1, 40)):
        for half in range(2):
            for jj in range(4):
                j = half * 4 + jj
                k.op("dve", lambda j=j, base=base: V.tensor_scalar(dg, identf, modT[:, base + j:base + j + 1], None, ALU.mult),
                     reads=[CONST, DG], writes=[DG])
                k.op("pe", lambda jj=jj: T.matmul(banks[0][:, jj * 128:(jj + 1) * 128], onesf1, dg, start=True, stop=True),
                     reads=[DG], writes=[BK[0]])
            k.op("act", lambda which=which, half=half: S.copy(gbc[which][:, half * 512:(half + 1) * 512], banks[0]),
                 reads=[BK[0]], writes=[GBC])

    xt2 = [sb(O_C + 16 * KB + i * 4 * KB, [D], F32) for i in range(2)]
    tmpc = [sb(O_C + 24 * KB + i * 4 * KB, [D], F32) for i in range(2)]
    xn2 = [sb(O_C + 32 * KB + i * 2 * KB, [D], BF16) for i in range(2)]
    junkc = sb(O_C + 36 * KB, [D], F32)
    XT2, TMPC, XN2, JKC = [Buf(), Buf()], [Buf(), Buf()], [Buf(), Buf()], Buf()
    k.op("dve", lambda: V.memset(stat, 0.0), reads=[STAT], writes=[STAT])
    def c_stage_a(i):
        b = i % 2
        k.dma("sp", xt2[b], x_d[i * 128:(i + 1) * 128, :], writes=[XT2[b]])
        for half in range(2):
            pb = 2 * b + half

            def mmo():
                inst = None
                for ch in range(8):
                    inst = T.matmul(banks[pb], mixT[:, ch, i * 128:(i + 1) * 128], wout[:, ch, half * 512:(half + 1) * 512],
                                    start=(ch == 0), stop=(ch == 7))
                return inst
            k.op("pe", mmo, reads=MIX + [WO], writes=[BK[pb]])
            k.op("dve", lambda: V.tensor_tensor(tmpc[b][:, half * 512:(half + 1) * 512], banks[pb],
                                                gbc[0][:, half * 512:(half + 1) * 512], ALU.mult),
                 reads=[BK[pb], GBC], writes=[TMPC[b]])
        k.op("pool", lambda: G.tensor_tensor(x1[:, i, :], tmpc[b], xt2[b], ALU.add), reads=[TMPC[b], XT2[b]], writes=[X1[i]])
        k.op("act", lambda: S.activation(junkc, x1[:, i, :], AF.Square, accum_out=stat[:, i:i + 1]),
             reads=[X1[i], STAT], writes=[JKC, STAT])
        k.op("act", lambda: S.activation(stat[:, 32 + i:33 + i], stat[:, i:i + 1], AF.Sqrt, bias=cc(C_EPS), scale=1.0 / D),
             reads=[STAT, CONST], writes=[STAT])

    def c_stage_a2(i):
        b = i % 2
        k.op("dve", lambda: V.reciprocal(stat[:, 64 + i:65 + i], stat[:, 32 + i:33 + i]), reads=[STAT], writes=[STAT])
        k.op("dve", lambda: V.tensor_scalar(xn2[b], x1[:, i, :], stat[:, 64 + i:65 + i], None, ALU.mult),
             reads=[X1[i], STAT], writes=[XN2[b]])

    def c_stage_b(i):
        b = i % 2
        pbs = (4 + 2 * b, 5 + 2 * b)
        psbs = [bank_bf(pb_).rearrange("p (a b) -> p a b", b=128) for pb_ in pbs]

        def tps():
            inst = None
            for kc in range(8):
                inst = T.transpose(psbs[kc // 4][:, kc % 4, :], xn2[b][:, kc * 128:(kc + 1) * 128], ident)
            return inst
        k.op("pe", tps, reads=[XN2[b], CONST], writes=[BK[pbs[0]], BK[pbs[1]]])
        for kc in range(8):
            dst = h2T[:, kc, i * 128:(i + 1) * 128]
            src = psbs[kc // 4][:, kc % 4, :]
            if kc < 4:
                k.op("act", lambda: S.activation(dst, src, AF.Identity, bias=GB[:, 24 + kc:25 + kc], scale=GB[:, 16 + kc:17 + kc]),
                     reads=[BK[pbs[0]], CONST], writes=[H2[i][0]])
            else:
                k.op("dve", lambda: V.tensor_scalar(dst, src, GB[:, 16 + kc:17 + kc], GB[:, 24 + kc:25 + kc], ALU.mult, ALU.add),
                     reads=[BK[pbs[1]], CONST], writes=[H2[i][1]])
    for t in range(17):
        if t < 16:
            c_stage_a(t)
        if t >= 1:
            c_stage_b(t - 1)
        if t < 16:
            c_stage_a2(t)
    if stage == 4:
        dump("d_x1", x1, X1, [128, 16, D], F32)
        dump("d_h2T", h2T, sum(H2, []), [128, 8, NOWN], BF16)
        dump("d_gbc", gbc[0], [GBC], [128, D], F32)
        k.finish("sp")
        return nc, dbg_d
    k.barrier()

    O_HID = 150 * KB + 16 * KB
    hid = sb(O_HID, [32, 512], BF16)
    HID = [Buf() for _ in range(32)]
    w1b = [sb(O_MIX + i * 8 * KB, [8, 512], BF16) for i in range(2)]
    w2b = [sb(O_MIX + 16 * KB + i * 8 * KB, [4, D], BF16) for i in range(2)]
    W1B, W2B = [Buf(), Buf()], [Buf(), Buf()]
    sqb = [sb(O_WO + i * 2 * KB, [512], F32) for i in range(2)]
    SQB = [Buf(), Buf()]
    tmpd = [sb(O_WO + 4 * KB + i * 4 * KB, [D], F32) for i in range(2)] + [sb(198 * KB + i * 4 * KB, [D], F32) for i in range(2)]
    TMPD = [Buf(), Buf(), Buf(), Buf()]
    junkd = sb(O_WO + 12 * KB, [D], F32)
    JKD = Buf()
    OT = [Buf(), Buf()]
    ot = [sb(O_C + 12 * KB, [D], F32), sb(O_C + 0 * KB, [D], F32)]
    w1v = wff1_d
    w2v = wff2_d.rearrange("(fb f p) c -> fb p f c", f=4, p=128)
    k.op("dve", lambda: V.memset(stat, 0.0), reads=[STAT], writes=[STAT])
    wctr = [0, 0]
    for g in range(4):
        for fb in range(8):
            wi = wctr[0] % 2
            wctr[0] += 1
            k.dma("pool", w1b[wi], w1v[fb].rearrange("p (a b) -> p a b", b=512), writes=[W1B[wi]])
            for f4 in range(4):
                fc = fb * 4 + f4
                pb = fc % 4

                def mm1(wi=wi, f4=f4, pb=pb, g=g):
                    inst = None
                    for kc in range(8):
                        inst = T.matmul(banks[pb], w1b[wi][:, kc, f4 * 128:(f4 + 1) * 128], h2T[:, kc, g * 512:(g + 1) * 512],
                                        start=(kc == 0), stop=(kc == 7))
                    return inst
                k.op("pe", mm1, reads=[W1B[wi]] + sum(H2[4 * g:4 * g + 4], []), writes=[BK[pb]])
                sq = fc % 2
                k.op("act", lambda sq=sq, pb=pb: S.activation(sqb[sq], banks[pb], AF.Relu), reads=[BK[pb]], writes=[SQB[sq]])
                k.op("dve", lambda sq=sq, fc=fc: V.tensor_tensor(hid[:, fc, :], sqb[sq], sqb[sq], ALU.mult),
                     reads=[SQB[sq]], writes=[HID[fc]])
        for fb in range(8):
            wi = wctr[1] % 2
            wctr[1] += 1
            k.dma("pool", w2b[wi], w2v[fb], writes=[W2B[wi]])

            def mm2(wi=wi, fb=fb):
                inst = None
                for f4 in range(4):
                    fc = fb * 4 + f4
                    for t in range(4):
                        for half in range(2):
                            inst = T.matmul(banks[t * 2 + half], hid[:, fc, t * 128:(t + 1) * 128],
                                            w2b[wi][:, f4, half * 512:(half + 1) * 512], start=(fc == 0), stop=(fc == 31))
                return inst
            k.op("pe", mm2, reads=[W2B[wi]] + HID[fb * 4:fb * 4 + 4], writes=BK)
        for t in range(4):
            for half in range(2):
                pb = t * 2 + half
                k.op("dve", lambda pb=pb, half=half, t=t: V.tensor_tensor(tmpd[t][:, half * 512:(half + 1) * 512], banks[pb],
                                                                         gbc[1][:, half * 512:(half + 1) * 512], ALU.mult),
                     reads=[BK[pb], GBC], writes=[TMPD[t]])
        for t in range(4):
            i = 4 * g + t
            b = i % 2
            k.op("pool", lambda i=i, t=t: G.tensor_tensor(tmpd[t], tmpd[t], x1[:, i, :], ALU.add), reads=[TMPD[t], X1[i]], writes=[TMPD[t]])
            k.op("act", lambda i=i, t=t: S.activation(junkd, tmpd[t], AF.Square, accum_out=stat[:, i:i + 1]),
                 reads=[TMPD[t], STAT], writes=[JKD, STAT])
            k.op("act", lambda i=i: S.activation(stat[:, 32 + i:33 + i], stat[:, i:i + 1], AF.Sqrt, bias=cc(C_EPS), scale=1.0 / D),
                 reads=[STAT, CONST], writes=[STAT])
            k.op("dve", lambda i=i: V.reciprocal(stat[:, 64 + i:65 + i], stat[:, 32 + i:33 + i]), reads=[STAT], writes=[STAT])
            k.op("dve", lambda i=i, b=b, t=t: V.scalar_tensor_tensor(ot[b], tmpd[t], stat[:, 64 + i:65 + i], nfw, ALU.mult, ALU.mult),
                 reads=[TMPD[t], STAT, NFW], writes=[OT[b]])
            k.dma("sp", out_d[i * 128:(i + 1) * 128, :], ot[b], reads=[OT[b]])
    k.finish("sp")
    return nc, dbg_d


def _partner_perm(n):
    idx = np.arange(n)
    return np.where((idx % 64) < 32, idx + 32, idx - 32)


def _chunked(w):
    n = w.shape[1] // 128
    return np.ascontiguousarray(w.reshape(8, 128, n, 128).transpose(2, 1, 0, 3).reshape(n, 128, 1024))


def prepare_inputs(x, c, positions, w_ada, b_ada, norm1_w, w_in, idx_k_ln_w, idx_k_ln_b,
                   lambda_q1, lambda_k1, lambda_q2, lambda_k2, subln_w, w_out, norm2_w,
                   w_ff1, w_ff2, norm_f_w):
    f32 = np.float32
    w_in = np.asarray(w_in[0], f32)
    aq, ak, av = w_in[:, 0:512], w_in[:, 512:1024], w_in[:, 1024:1536]
    iq, ik, iw = w_in[:, 1536:2048], w_in[:, 2048:2112], w_in[:, 2112:2120]
    dq, dk, dv = w_in[:, 2120:2632], w_in[:, 2632:3144], w_in[:, 3144:3656]
    ikd = np.concatenate([ik, ik], axis=1)
    fm = np.concatenate([ak, ikd, aq, iq, dk, dq], axis=1)
    fm_rot = fm[:, _partner_perm(fm.shape[1])]
    w_fm = _chunked(fm)
    w_fm_rot = _chunked(fm_rot)
    w_tm = np.ascontiguousarray(np.concatenate([av, dv, iw], axis=1))
    wff1 = np.asarray(w_ff1[0], f32)
    wff1_l = np.ascontiguousarray(wff1.reshape(8, 128, 8, 512).transpose(2, 1, 0, 3).reshape(8, 128, 4096))
    p = np.arange(128)
    consts = np.zeros((128, NCONST), f32)
    inv_freq = (10000.0 ** (-np.arange(32, dtype=f32) / 32)).astype(f32)
    consts[:, C_INVF] = inv_freq[p % 32]
    consts[:, C_SGN] = np.where((p % 64) < 32, -1.0, 1.0)
    consts[:, C_EPS] = NORM_EPS
    consts[:, C_LNEPS] = LN_EPS
    consts[:, C_ONE] = 1.0
    consts[:, C_POW2:C_POW2 + NIT + 1] = (0.5 ** np.arange(1, NIT + 2))[None, :]
    lnw = np.asarray(idx_k_ln_w[0], f32)
    lnb = np.asarray(idx_k_ln_b[0], f32)
    pp = _partner_perm(64)
    consts[:, C_LNP + 0] = lnw[p % 64]
    consts[:, C_LNP + 1] = lnb[p % 64]
    consts[:, C_LNP + 2] = lnw[pp][p % 64]
    consts[:, C_LNP + 3] = lnb[pp][p % 64]
    consts[:, C_SUBW] = np.asarray(subln_w[0], f32) * f32(1.0 - LAM_INIT)
    consts[:, C_N1:C_N1 + 8] = np.asarray(norm1_w[0], f32).reshape(8, 128).T
    consts[:, C_N2:C_N2 + 8] = np.asarray(norm2_w[0], f32).reshape(8, 128).T
    cmat = np.zeros((128, 3, 128), f32)
    cmat[:, 0, :] = np.eye(128)
    cmat[:, 1, :] = (p[:, None] <= p[None, :])
    cmat[:, 2, :] = np.where(p[None, :] <= p[:, None], 0.0, NEG)
    lamv = np.concatenate([lambda_q1[0], lambda_k1[0], lambda_q2[0], lambda_k2[0]]).astype(f32)[None, :]
    shared = {
        "w_ada": np.ascontiguousarray(w_ada[0], dtype=f32),
        "b_adaT": np.ascontiguousarray(np.asarray(b_ada[0], f32).reshape(48, 128).T),
        "w_fm": w_fm, "w_fm_rot": w_fm_rot, "w_tm": w_tm,
        "w_out": np.ascontiguousarray(w_out[0], dtype=f32),
        "w_ff1": wff1_l, "w_ff2": np.ascontiguousarray(w_ff2[0], dtype=f32),
        "norm_f_w": np.asarray(norm_f_w, f32)[None, :],
        "cmat": cmat, "lamv": lamv,
    }
    in_maps = []
    perms = []
    for core in range(8):
        b, r = core // 2, core % 2
        blocks = np.concatenate([2 * np.arange(16) + r, 2 * np.arange(16) + 1 - r])
        tok = (blocks[:, None] * 128 + np.arange(128)[None, :]).reshape(-1)
        perms.append(tok[:NOWN])
        cst = consts.copy()
        cst[:, C_ROLE] = float(r)
        cst[:, C_NEGROLE] = 0.0 if r == 1 else NEG
        m = dict(shared)
        m["x"] = np.ascontiguousarray(np.asarray(x[b], f32)[tok])
        m["pos"] = np.ascontiguousarray(np.asarray(positions[b], np.int32)[tok])[None, :]
        m["cT"] = np.ascontiguousarray(np.asarray(c[b], f32).reshape(8, 128).T)
        m["consts"] = cst
        in_maps.append(m)
    return in_maps, perms


_CACHE = {}


def kernel(**inputs):
    in_maps, perms = prepare_inputs(**{k_: np.asarray(v) for k_, v in inputs.items()})
    if "nc" not in _CACHE:
        _CACHE["nc"] = build_program()[0]
    res = run_bass_kernel_spmd(_CACHE["nc"], in_maps, core_ids=list(range(8)))
    out = np.zeros((4, L, D), np.float32)
    for core in range(8):
        out[core // 2, perms[core]] = res.results[core]["out"]
    return out
```

```python
import os
import math
import numpy as np
import concourse.bass as bass
import concourse.mybir as mybir
from concourse.bass_utils import run_bass_kernel_spmd

F32 = mybir.dt.float32
BF16 = mybir.dt.bfloat16
I32 = mybir.dt.int32
U8 = mybir.dt.uint8
FP8 = mybir.dt.float8e4
ALU = mybir.AluOpType
AF = mybir.ActivationFunctionType
AX = mybir.AxisListType

D = 1024
L = 4096
NOWN = 2048
NIT = 18
TOPK = 256
LAM_INIT = 0.8 - 0.6 * math.exp(-0.3 * 0)
NORM_EPS = 1e-6
LN_EPS = 1e-5
NEG = -1.0e30
TWO_PI = float(2 * np.pi)

C_INVF, C_SGN, C_ROLE, C_NEGROLE, C_EPS, C_LNEPS, C_ONE = 0, 1, 2, 3, 4, 5, 6
C_POW2 = 8
C_LNP = 40
C_SUBW = 44
C_N1 = 48
C_N2 = 56
NCONST = 64


class Buf:
    __slots__ = ("name", "w", "r", "x")

    def __init__(self, name="b", x=False):
        self.name = name
        self.w = None
        self.r = {}
        self.x = x


class K:
    EPOCH = 30000

    def __init__(self, nc):
        self.nc = nc
        self.eng = {"pe": nc.tensor, "act": nc.scalar, "dve": nc.vector,
                    "pool": nc.gpsimd, "sp": nc.sync}
        self.cnt = {k: 0 for k in self.eng}
        self.sems = {k: [] for k in self.eng}
        self.waited = {k: {} for k in self.eng}
        self.dma_id = 0
        self._dslots = {}
        self.n_inst = 0

    def _sem(self, key, epoch):
        lst = self.sems[key]
        while len(lst) <= epoch:
            lst.append(self.nc.alloc_semaphore(name=f"s_{key}_{len(lst)}"))
        return lst[epoch]

    def _wait(self, e, tok):
        src, c = tok
        if self.waited[e].get(src, 0) >= c:
            return
        ep = (c - 1) // self.EPOCH
        val = c - ep * self.EPOCH
        self.eng[e].wait_ge(self._sem(src, ep), val)
        self.waited[e][src] = c

    def _wait_dma(self, e, tok):
        _, sem, val, uid, slot = tok
        key = ("dma", slot)
        if self.waited[e].get(key, 0) >= val:
            return
        self.eng[e].wait_ge(sem, val)
        self.waited[e][key] = val

    def _dep(self, e, tok):
        if tok is None:
            return
        if tok[0] == "dma":
            self._wait_dma(e, tok)
        else:
            self._wait(e, tok)

    def deps(self, e, reads=(), writes=()):
        for b in reads:
            if b.w is not None:
                self._dep(e, b.w)
        for b in writes:
            if b.w is not None and (b.w[0] != e or e != "pe"):
                self._dep(e, b.w)
            for re_, rc in b.r.items():
                if re_ == e and e == "pe":
                    continue
                if isinstance(re_, tuple):
                    self._wait_dma(e, rc)
                else:
                    self._wait(e, (re_, rc))

    def done(self, e, inst, reads=(), writes=()):
        self.cnt[e] += 1
        c = self.cnt[e]
        ep = (c - 1) // self.EPOCH
        inst.then_inc(self._sem(e, ep), 1)
        tok = (e, c)
        for b in reads:
            b.r[e] = c
        for b in writes:
            b.w = tok
            b.r = {}
        return tok

    def op(self, e, fn, reads=(), writes=()):
        xs = [b for b in reads if b.x and b not in writes]
        if xs:
            reads = [b for b in reads if not b.x]
            writes = list(writes) + xs
        self.deps(e, reads, writes)
        inst = fn()
        self.n_inst += 1
        return self.done(e, inst, reads, writes)

    def dma(self, q, out, in_, reads=(), writes=(), **kw):
        self.deps(q, reads, writes)
        st = self._dslots.setdefault(q, {"sems": [], "vals": [], "i": 0, "pend": []})
        NS = 14
        i = st["i"] % NS
        st["i"] += 1
        if len(st["sems"]) <= i:
            st["sems"].append(self.nc.alloc_semaphore(name=f"d_{q}_{i}"))
            st["vals"].append(0)
            st["pend"].append(None)
        prev = st["pend"][i]
        if prev is not None:
            self._wait_dma(q, prev)
        st["vals"][i] += 16
        self.dma_id += 1
        tok = ("dma", st["sems"][i], st["vals"][i], self.dma_id, (q, i))
        st["pend"][i] = tok
        inst = self.eng[q].dma_start(out=out, in_=in_, **kw)
        inst.then_inc(st["sems"][i], 16)
        for b in writes:
            b.w = tok
            b.r = {}
        for b in reads:
            b.r[("dma", self.dma_id)] = tok
        return tok

    def barrier(self, engines=("pe", "act", "dve", "pool", "sp")):
        for e in engines:
            for f in self.eng:
                if f != e and self.cnt[f] > 0:
                    self._wait(e, (f, self.cnt[f]))
            for q, st in self._dslots.items():
                for p in st["pend"]:
                    if p is not None:
                        self._wait_dma(e, p)

    def finish(self, e="sp"):
        self.barrier(engines=(e,))


def build_program(stage=99, dbg=False):
    nc = bass.Bass("TRN2", target_bir_lowering=False)
    k = K(nc)
    V, S, P, T, G = nc.vector, nc.scalar, nc.gpsimd, nc.tensor, nc.gpsimd

    def din(name, shape, dt=F32):
        return nc.dram_tensor(name, list(shape), dt, kind="ExternalInput").ap()

    x_d = din("x", [L, D])
    pos_d = din("pos", [1, L], I32)
    ct_d = din("cT", [128, 8])
    consts_d = din("consts", [128, NCONST])
    cmat_d = din("cmat", [128, 3, 128])
    lamv_d = din("lamv", [1, 256])
    wada_d = din("w_ada", [D, 6 * D])
    badaT_d = din("b_adaT", [128, 48])
    wfm_d = din("w_fm", [21, 128, 1024])
    wfmr_d = din("w_fm_rot", [21, 128, 1024])
    wtm_d = din("w_tm", [D, 1032])
    wout_d = din("w_out", [D, D])
    wff1_d = din("w_ff1", [8, 128, 8 * 512])
    wff2_d = din("w_ff2", [4 * D, D])
    nfw_d = din("norm_f_w", [1, D])
    out_d = nc.dram_tensor("out", [NOWN, D], F32, kind="ExternalOutput").ap()
    dbg_d = {}

    KB = 1024
    ARENA = 206 * KB
    arena = nc.alloc_sbuf_tensor("arena", [128, ARENA], U8).ap()

    def sb(off, shape, dt):
        esz = {F32: 4, BF16: 2, I32: 4, FP8: 1}[dt]
        n = int(np.prod(shape)) * esz
        assert off + n <= ARENA, (off, n)
        ap = arena[:, off:off + n].bitcast(dt)
        if len(shape) == 2:
            ap = ap.rearrange("p (a b) -> p a b", b=shape[1])
        elif len(shape) == 3:
            ap = ap.rearrange("p (a b c) -> p a b c", b=shape[1], c=shape[2])
        return ap

    banks = [nc.alloc_psum_tensor(f"bank{i}", [128, 512], F32).ap() for i in range(8)]
    BK = [Buf(f"bank{i}", x=True) for i in range(8)]

    def bank_bf(i):
        return banks[i].bitcast(BF16)

    o = 0
    consts = sb(o, [NCONST], F32); o += NCONST * 4
    ident = sb(o, [128], BF16); o += 256
    triT = sb(o, [128], BF16); o += 256
    trineg = sb(o, [128], F32); o += 512
    identf = sb(o, [128], F32); o += 512
    ones_bf = sb(o, [128], BF16); o += 256
    ones_f = sb(o, [128], F32); o += 512
    modT = sb(o, [48], F32); o += 192
    GB = sb(o, [32], F32); o += 128
    scT = sb(o, [8], F32); o += 32
    stat = sb(o, [128], F32); o += 512
    bis = sb(o, [6, 32], F32); o += 768
    iw_sb = sb(o, [16, 8], F32); o += 512
    lam_t = sb(o, [8], F32); o += 32
    assert o <= 6 * KB
    CONST = Buf("const")
    STAT = Buf("stat")
    BIS = Buf("bis")
    IW = Buf("iw")

    def cc(col, n=1):
        return consts[:, col:col + n]

    O_MIX = 6 * KB
    O_K = 38 * KB
    O_V = 70 * KB
    O_KI = 102 * KB
    O_HT = 110 * KB
    O_TAB = 174 * KB
    O_T8 = 190 * KB
    O_TMPA = 22 * KB

    mixT = sb(O_MIX, [8, NOWN], BF16)
    MIX = [Buf(f"mix{i}") for i in range(8)]
    kT = sb(O_K, [4, L], BF16)
    vtm = sb(O_V, [32, 512], BF16)
    kiT = sb(O_KI, [L], BF16)
    hT_own = sb(O_HT, [8, NOWN], BF16)
    hT_oth = sb(O_HT + 32 * KB, [8, NOWN], BF16)
    cosT = sb(O_TAB, [L], BF16)
    sinT = sb(O_TAB + 8 * KB, [L], BF16)
    qT = sb(O_HT + 32 * KB, [4, NOWN], BF16)
    iqT = sb(O_HT + 48 * KB, [4, NOWN], BF16)
    TAB = Buf("tab")
    HT = [[Buf(f"ht{t}_{p}") for p in range(2)] for t in range(32)]
    KT = [[Buf(f"kT{j}_{g}") for g in range(8)] for j in range(4)]
    KI = [Buf(f"ki{g}") for g in range(8)]
    VT = [Buf(f"v{t}") for t in range(32)]
    QT = [[Buf(f"qT{j}_{g}") for g in range(4)] for j in range(4)]
    IQ = [[Buf(f"iq{j}_{g}") for g in range(4)] for j in range(4)]

    def hT_ap(kc, t0, n):
        if t0 < NOWN:
            return hT_own[:, kc, t0:t0 + n]
        return hT_oth[:, kc, t0 - NOWN:t0 - NOWN + n]

    def ht_bufs(t0, n):
        r = []
        for tt in range(t0 // 128, (t0 + n) // 128):
            r += HT[tt]
        return r

    SCR = 38 * KB
    SCR0 = 190 * KB
    k.dma("sp", consts, consts_d, writes=[CONST])
    cm32 = sb(SCR0, [3, 128], F32)
    CM = Buf("cm")
    k.dma("sp", cm32, cmat_d, writes=[CM])
    k.op("dve", lambda: V.tensor_copy(ident, cm32[:, 0, :]), reads=[CM], writes=[CONST])
    k.op("dve", lambda: V.tensor_copy(identf, cm32[:, 0, :]), reads=[CM], writes=[CONST])
    k.op("dve", lambda: V.tensor_copy(triT, cm32[:, 1, :]), reads=[CM], writes=[CONST])
    k.op("dve", lambda: V.tensor_copy(trineg, cm32[:, 2, :]), reads=[CM], writes=[CONST])
    k.op("dve", lambda: V.memset(ones_bf, 1.0), writes=[CONST])
    k.op("dve", lambda: V.memset(ones_f, 1.0 / 128.0), writes=[CONST])
    k.op("dve", lambda: V.memset(stat, 0.0), writes=[STAT])
    ct = sb(SCR0 + 2 * KB, [8], F32)
    CT = Buf("ct")
    k.dma("sp", ct, ct_d, writes=[CT])
    k.op("act", lambda: S.activation(scT, ct, AF.Silu), reads=[CT], writes=[CONST])
    badaT = sb(SCR0 + 3 * KB, [48], F32)
    BA = Buf("ba")
    k.dma("sp", badaT, badaT_d, writes=[BA])
    lv = sb(SCR0 + 4 * KB, [256], F32)
    LV = Buf("lv")
    k.dma("sp", lv, lamv_d.partition_broadcast(128), writes=[LV])
    lj = sb(SCR0 + 5 * KB, [64], F32)
    LJ = Buf("lj")
    LAMB = Buf("lam")
    k.op("dve", lambda: V.memset(lam_t, 0.0), writes=[LAMB])
    k.op("dve", lambda: V.scalar_tensor_tensor(lj, lv[:, 0:64], 1.0, lv[:, 64:128], ALU.mult, ALU.mult,
                                               accum_out=lam_t[:, 0:1]), reads=[LV, LAMB], writes=[LJ, LAMB])
    k.op("dve", lambda: V.scalar_tensor_tensor(lj, lv[:, 128:192], 1.0, lv[:, 192:256], ALU.mult, ALU.mult,
                                               accum_out=lam_t[:, 1:2]), reads=[LV, LAMB], writes=[LJ, LAMB])
    k.op("act", lambda: S.activation(lam_t[:, 2:4], lam_t[:, 0:2], AF.Exp), reads=[LAMB], writes=[LAMB])
    k.op("dve", lambda: V.tensor_tensor(lam_t[:, 4:5], lam_t[:, 3:4], lam_t[:, 2:3], ALU.subtract),
         reads=[LAMB], writes=[LAMB])
    k.op("dve", lambda: V.tensor_scalar(lam_t[:, 5:6], lam_t[:, 4:5], -LAM_INIT, None, ALU.add),
         reads=[LAMB], writes=[LAMB])
    neglam = lam_t[:, 5:6]

    WAB = [Buf("wa0"), Buf("wa1")]
    wa = [sb(110 * KB + i * 32 * KB, [8, 1024], F32) for i in range(2)]
    wada_v = wada_d.rearrange("(kc p) c -> p kc c", p=128)
    for blk in range(6):
        b = blk % 2
        k.dma("sp", wa[b], wada_v[:, :, blk * 1024:(blk + 1) * 1024], writes=[WAB[b]])

        def mm_mod(b=b, blk=blk):
            inst = None
            for j in range(8):
                col = blk * 8 + j
                for kc in range(8):
                    inst = T.matmul(banks[0][:, col:col + 1], wa[b][:, kc, j * 128:(j + 1) * 128],
                                    scT[:, kc:kc + 1], start=(kc == 0), stop=(kc == 7))
            return inst
        k.op("pe", mm_mod, reads=[WAB[b], CONST], writes=[BK[0]])
    _early = []

    def make_tables():
        posi = sb(SCR, [L], I32)
        a0 = sb(SCR + 16 * KB, [L], F32)
        q_ = sb(SCR + 32 * KB, [L], F32)
        m_ = sb(SCR + 48 * KB, [L], F32)
        PI_, A0, Q_, M_ = Buf(), Buf(), Buf(), Buf()
        k.dma("sp", posi, pos_d.partition_broadcast(128), writes=[PI_])
        k.op("dve", lambda: V.tensor_copy(a0, posi), reads=[PI_], writes=[A0])
        k.op("dve", lambda: V.tensor_scalar(a0, a0, cc(C_INVF), None, ALU.mult), reads=[A0, CONST], writes=[A0])
        for which in range(2):
            if which == 1:
                k.op("dve", lambda: V.tensor_scalar(a0, a0, float(np.pi / 2), None, ALU.add), reads=[A0], writes=[A0])
            k.op("dve", lambda: V.tensor_scalar(q_, a0, 1.0 / TWO_PI, None, ALU.mult), reads=[A0], writes=[Q_])
            k.op("dve", lambda: V.tensor_copy(posi, q_), reads=[Q_], writes=[PI_])
            k.op("dve", lambda: V.tensor_copy(q_, posi), reads=[PI_], writes=[Q_])
            k.op("dve", lambda: V.scalar_tensor_tensor(q_, q_, -TWO_PI, a0, ALU.mult, ALU.add), reads=[Q_, A0], writes=[Q_])
            k.op("dve", lambda: V.tensor_scalar(m_, q_, float(np.pi), -TWO_PI, ALU.is_gt, ALU.mult), reads=[Q_], writes=[M_])
            k.op("dve", lambda: V.tensor_tensor(q_, q_, m_, ALU.add), reads=[Q_, M_], writes=[Q_])
            k.op("dve", lambda: V.tensor_scalar(q_, q_, 3.141592, -3.141592, ALU.min, ALU.max), reads=[Q_], writes=[Q_])
            if which == 0:
                k.op("act", lambda: S.activation(sinT, q_, AF.Sin, scale=cc(C_SGN)), reads=[Q_, CONST], writes=[TAB])
            else:
                k.op("act", lambda: S.activation(cosT, q_, AF.Sin), reads=[Q_], writes=[TAB])
        k.barrier()

    def build_hT():
        xt = [sb(O_T8 + 0, [D], F32), sb(O_TMPA, [D], F32), sb(O_T8 + 4 * KB, [D], F32)]
        xn = [sb(O_TMPA + 4 * KB, [D], BF16), sb(O_TMPA + 6 * KB, [D], BF16), sb(O_TMPA + 12 * KB, [D], BF16)]
        junk = sb(O_TMPA + 8 * KB, [D], F32)
        XT, XN, JK = [Buf(), Buf(), Buf()], [Buf(), Buf(), Buf()], Buf()
        SQ = [Buf(), Buf(), Buf()]
        k.op("dve", lambda: V.memset(stat, 0.0), reads=[STAT], writes=[STAT])

        def stage_a(tt):
            b = tt % 3
            k.dma("sp", xt[b], x_d[tt * 128:(tt + 1) * 128, :], writes=[XT[b]])
            k.op("act", lambda: S.activation(junk, xt[b], AF.Square, accum_out=stat[:, tt:tt + 1]),
                 reads=[XT[b], STAT], writes=[JK, STAT])
            k.op("act", lambda: S.activation(stat[:, 32 + tt:33 + tt], stat[:, tt:tt + 1], AF.Sqrt,
                                             bias=cc(C_EPS), scale=1.0 / D), reads=[STAT, CONST], writes=[SQ[b]])

        def stage_a2(tt):
            b = tt % 3
            k.op("dve", lambda: V.reciprocal(stat[:, 64 + tt:65 + tt], stat[:, 32 + tt:33 + tt]),
                 reads=[SQ[b]], writes=[STAT])
            k.op("dve", lambda: V.tensor_scalar(xn[b], xt[b], stat[:, 64 + tt:65 + tt], None, ALU.mult),
                 reads=[XT[b], STAT], writes=[XN[b]])

        def stage_b(tt):
            b = tt % 3
            pbs = (4 + 2 * (tt % 2), 5 + 2 * (tt % 2))
            psbs = [bank_bf(pb_).rearrange("p (a b) -> p a b", b=128) for pb_ in pbs]

            def tps():
                inst = None
                for kc in range(8):
                    inst = T.transpose(psbs[kc // 4][:, kc % 4, :], xn[b][:, kc * 128:(kc + 1) * 128], ident)
                return inst
            k.op("pe", tps, reads=[XN[b], CONST], writes=[BK[pbs[0]], BK[pbs[1]]])
            for kc in range(8):
                dst = hT_ap(kc, tt * 128, 128)
                src = psbs[kc // 4][:, kc % 4, :]
                if kc < 4:
                    k.op("act", lambda: S.activation(dst, src, AF.Identity, bias=GB[:, 8 + kc:9 + kc], scale=GB[:, kc:kc + 1]),
                         reads=[BK[pbs[0]], CONST], writes=[HT[tt][0]])
                else:
                    k.op("dve", lambda: V.tensor_scalar(dst, src, GB[:, kc:kc + 1], GB[:, 8 + kc:9 + kc], ALU.mult, ALU.add),
                         reads=[BK[pbs[1]], CONST], writes=[HT[tt][1]])
        for t in range(33):
            if t < 32:
                stage_a(t)
            if t >= 1:
                stage_b(t - 1)
            if t < 32:
                stage_a2(t)

    wb_i = [0]

    def proj_rope(chunk, dest_fn, dest_bufs, tgs, ki_mode=False):
        i = wb_i[0] % 2
        wb_i[0] += 1
        w0 = sb(O_T8 + i * 4 * KB, [8, 128], BF16)
        w1 = sb(O_T8 + i * 4 * KB + 2 * KB, [8, 128], BF16)
        WB = proj_rope.WB[i]
        k.dma("pool", w0, wfm_d[chunk].rearrange("p (a b) -> p a b", b=128), writes=[WB])
        k.dma("pool", w1, wfmr_d[chunk].rearrange("p (a b) -> p a b", b=128), writes=[WB])
        for gi, tg in enumerate(tgs):
            pa, pr = (0, 1) if proj_rope.par == 0 else (2, 3)
            proj_rope.par ^= 1
            t0 = tg * 512

            def mms(pa=pa, pr=pr, t0=t0):
                inst = None
                for kc in range(8):
                    T.matmul(banks[pa], w0[:, kc, :], hT_ap(kc, t0, 512), start=(kc == 0), stop=(kc == 7))
                for kc in range(8):
                    inst = T.matmul(banks[pr], w1[:, kc, :], hT_ap(kc, t0, 512), start=(kc == 0), stop=(kc == 7))
                return inst
            k.op("pe", mms, reads=[WB] + ht_bufs(t0, 512), writes=[BK[pa], BK[pr]])
            tb = proj_rope.tb
            proj_rope.tb ^= 1
            t1 = sb(O_TMPA + tb * 4 * KB, [512], F32)
            t2 = sb(O_TMPA + tb * 4 * KB + 2 * KB, [512], F32)
            T1, T2 = proj_rope.T1[tb], proj_rope.T2[tb]
            cs = cosT[:, t0:t0 + 512]
            sn = sinT[:, t0:t0 + 512]
            dst = dest_fn(tg)
            if not ki_mode:
                k.op("dve", lambda: V.tensor_tensor(t1, banks[pa], cs, ALU.mult), reads=[BK[pa], TAB], writes=[T1])
                k.op("dve", lambda: V.tensor_tensor(t2, banks[pr], sn, ALU.mult), reads=[BK[pr], TAB], writes=[T2])
                k.op("dve", lambda: V.tensor_tensor(dst, t1, t2, ALU.add), reads=[T1, T2], writes=[dest_bufs[gi]])
            else:
                KIS = proj_rope.KIS
                kb = 8 * KB
                psb_ = sb(SCRK + 0 * kb // 4, [512], F32)
                psq = sb(SCRK + 1 * kb // 4, [512], F32)
                mean = sb(SCRK + 2 * kb // 4, [512], F32)
                var = sb(SCRK + 3 * kb // 4, [512], F32)
                yy = sb(SCRK + 4 * kb // 4, [512], F32)
                yr = sb(SCRK + 5 * kb // 4, [512], F32)
                k.op("act", lambda: S.copy(psb_, banks[pa]), reads=[BK[pa]], writes=[KIS[0]])
                k.op("act", lambda: S.activation(psq, banks[pa], AF.Square), reads=[BK[pa]], writes=[KIS[1]])

                def mm2():
                    T.matmul(banks[4], ones_f, psb_, start=True, stop=True)
                    return T.matmul(banks[5], ones_f, psq, start=True, stop=True)
                k.op("pe", mm2, reads=[KIS[0], KIS[1], CONST], writes=[BK[4], BK[5]])
                k.op("act", lambda: S.copy(mean, banks[4]), reads=[BK[4]], writes=[KIS[2]])
                k.op("dve", lambda: V.tensor_tensor(var, mean, mean, ALU.mult), reads=[KIS[2]], writes=[KIS[3]])
                k.op("dve", lambda: V.tensor_tensor(var, banks[5], var, ALU.subtract), reads=[BK[5], KIS[3]], writes=[KIS[3]])
                k.op("act", lambda: S.activation(var, var, AF.Sqrt, bias=cc(C_LNEPS), scale=1.0), reads=[KIS[3], CONST], writes=[KIS[3]])
                k.op("dve", lambda: V.reciprocal(var, var), reads=[KIS[3]], writes=[KIS[3]])
                k.op("dve", lambda: V.tensor_tensor(yy, psb_, mean, ALU.subtract), reads=[KIS[0], KIS[2]], writes=[KIS[4]])
                k.op("dve", lambda: V.tensor_tensor(yy, yy, var, ALU.mult), reads=[KIS[4], KIS[3]], writes=[KIS[4]])
                k.op("dve", lambda: V.tensor_scalar(yy, yy, cc(C_LNP), cc(C_LNP + 1), ALU.mult, ALU.add), reads=[KIS[4], CONST], writes=[KIS[4]])
                k.op("dve", lambda: V.tensor_tensor(yr, banks[pr], mean, ALU.subtract), reads=[BK[pr], KIS[2]], writes=[KIS[5]])
                k.op("dve", lambda: V.tensor_tensor(yr, yr, var, ALU.mult), reads=[KIS[5], KIS[3]], writes=[KIS[5]])
                k.op("dve", lambda: V.tensor_scalar(yr, yr, cc(C_LNP + 2), cc(C_LNP + 3), ALU.mult, ALU.add), reads=[KIS[5], CONST], writes=[KIS[5]])
                k.op("dve", lambda: V.tensor_tensor(t1, yy, cs, ALU.mult), reads=[KIS[4], TAB], writes=[T1])
                k.op("dve", lambda: V.tensor_tensor(t2, yr, sn, ALU.mult), reads=[KIS[5], TAB], writes=[T2])
                k.op("dve", lambda: V.tensor_tensor(dst, t1, t2, ALU.add), reads=[T1, T2], writes=[dest_bufs[gi]])
    proj_rope.WB = [Buf(), Buf()]
    proj_rope.par = 0
    proj_rope.tb = 0
    proj_rope.T1 = [Buf(), Buf()]
    proj_rope.T2 = [Buf(), Buf()]
    proj_rope.KIS = [Buf() for _ in range(6)]
    SCRK = O_MIX

    def proj_v(col0):
        wv = sb(O_T8, [8, 512], BF16)
        WV = Buf()
        k.dma("pool", wv, wtm_d.rearrange("(kc p) c -> p kc c", p=128)[:, :, col0:col0 + 512], writes=[WV])
        for tt in range(32):
            pb = 4 + tt % 2

            def mms(tt=tt, pb=pb):
                inst = None
                for kc in range(8):
                    inst = T.matmul(banks[pb], hT_ap(kc, tt * 128, 128), wv[:, kc, :], start=(kc == 0), stop=(kc == 7))
                return inst
            k.op("pe", mms, reads=[WV] + HT[tt], writes=[BK[pb]])
            if tt % 2 == 0:
                k.op("act", lambda tt=tt, pb=pb: S.copy(vtm[:, tt, :], banks[pb]), reads=[BK[pb]], writes=[VT[tt]])
            else:
                k.op("dve", lambda tt=tt, pb=pb: V.tensor_copy(vtm[:, tt, :], banks[pb]), reads=[BK[pb]], writes=[VT[tt]])

    def dump(name, ap, bufs, shape, dt=F32):
        d = nc.dram_tensor(name, list(shape), dt, kind="ExternalOutput").ap()
        dbg_d[name] = d
        k.dma("sp", d, ap, reads=bufs)

    make_tables()
    k.op("dve", lambda: V.tensor_tensor(modT, banks[0][:, 0:48], badaT, ALU.add), reads=[BK[0], BA], writes=[CONST])
    k.op("dve", lambda: V.scalar_tensor_tensor(GB[:, 0:8], modT[:, 8:16], 1.0, cc(C_N1, 8), ALU.add, ALU.mult),
         reads=[CONST], writes=[CONST])
    k.op("dve", lambda: V.tensor_copy(GB[:, 8:16], modT[:, 0:8]), reads=[CONST], writes=[CONST])
    k.op("dve", lambda: V.scalar_tensor_tensor(GB[:, 16:24], modT[:, 32:40], 1.0, cc(C_N2, 8), ALU.add, ALU.mult),
         reads=[CONST], writes=[CONST])
    k.op("dve", lambda: V.tensor_copy(GB[:, 24:32], modT[:, 24:32]), reads=[CONST], writes=[CONST])
    k.barrier()
    if stage == 0:
        dump("d_modT", modT, [CONST], [128, 48], F32)
        dump("d_lam", lam_t, [LAMB], [128, 8], F32)
        k.finish("sp")
        return nc, dbg_d
    if stage == 0.5:
        dump("d_cos", cosT, [TAB], [128, L], BF16)
        dump("d_sin", sinT, [TAB], [128, L], BF16)
        k.finish("sp")
        return nc, dbg_d
    build_hT()
    k.barrier()
    if stage == 0.7:
        dump("d_hT", hT_own, sum(HT[:16], []), [128, 8, NOWN], BF16)
        k.finish("sp")
        return nc, dbg_d
    for j in range(4):
        proj_rope(j, lambda tg, j=j: kT[:, j, tg * 512:(tg + 1) * 512], KT[j], list(range(8)))
    proj_rope(4, lambda tg: kiT[:, tg * 512:(tg + 1) * 512], KI, list(range(8)), ki_mode=True)
    k.barrier()
    proj_v(0)
    k.barrier()
    for j in range(4):
        proj_rope(5 + j, lambda tg, j=j: qT[:, j, tg * 512:(tg + 1) * 512], QT[j], list(range(4)))
    for j in range(4):
        proj_rope(9 + j, lambda tg, j=j: iqT[:, j, tg * 512:(tg + 1) * 512], IQ[j], list(range(4)))
    k.barrier()
    wiw = sb(O_T8, [8, 8], BF16)
    WIW = Buf()
    k.dma("pool", wiw, wtm_d.rearrange("(kc p) c -> p kc c", p=128)[:, :, 1024:1032], writes=[WIW])
    for tt in range(16):
        def mms(tt=tt):
            inst = None
            for kc in range(8):
                inst = T.matmul(banks[4][:, tt * 8:(tt + 1) * 8], hT_ap(kc, tt * 128, 128), wiw[:, kc, :],
                                start=(kc == 0), stop=(kc == 7))
            return inst
        k.op("pe", mms, reads=[WIW] + HT[tt], writes=[BK[4]])
    k.op("dve", lambda: V.tensor_scalar(iw_sb.rearrange("p a b -> p (a b)"), banks[4][:, 0:128],
                                        float(8 ** -0.5 * 64 ** -0.5), None, ALU.mult), reads=[BK[4]], writes=[IW])

    if stage == 1:
        dump("d_hT", hT_own, sum(HT[:16], []), [128, 8, NOWN], BF16)
        dump("d_kT", kT, sum(KT, []), [128, 4, L], BF16)
        dump("d_kiT", kiT, KI, [128, L], BF16)
        dump("d_v", vtm, VT, [128, 32, 512], BF16)
        dump("d_qT", qT, sum(QT, []), [128, 4, NOWN], BF16)
        dump("d_iqT", iqT, sum(IQ, []), [128, 4, NOWN], BF16)
        dump("d_iw", iw_sb, [IW], [128, 16, 8], F32)
        dump("d_modT", modT, [CONST], [128, 48], F32)
        dump("d_lam", lam_t, [LAMB], [128, 8], F32)
        k.finish("sp")
        return nc, dbg_d
    k.barrier()

    MT = sb(O_HT, [32, 512], BF16)
    Ibufs = [sb(O_TAB, [L], F32), sb(O_T8, [L], F32)]
    JUNK = sb(O_TMPA + 8 * KB, [L], FP8)
    Mh = sb(O_TMPA + 8 * KB, [NOWN], BF16)
    NEB = 6
    EB = [sb(O_TMPA + i * KB, [512], BF16) for i in range(4)] + [sb(O_TMPA + (12 + i) * KB, [512], BF16) for i in range(2)]
    RB = [sb(O_TMPA + 4 * KB + i * 2 * KB, [512], F32) for i in range(2)]
    REC = [sb(O_TMPA + 14 * KB, [512], F32)] * 2
    MTB, MB = Buf("MT"), Buf("M")
    IBS = [Buf("I0"), Buf("I1")]
    EBB = [Buf() for _ in range(6)]
    RBB = [Buf() for _ in range(2)]
    RECB = [Buf("rec")] * 2
    nm, sacc, Hh, nH, uu = bis[:, 0, :], bis[:, 1, :], bis[:, 2, :], bis[:, 3, :], bis[:, 4, :]
    ectr = [0]
    rctr = [0]
    sctr = [0]

    def n_icomp(i):
        return 2 * ((i + 1) * 128 + 511) // 512 * 0 + 2 * (((i + 1) * 128 + 511) // 512) * 8

    def gen_icomp(i):
        Ibuf, IB = Ibufs[i % 2], IBS[i % 2]
        n1 = (i + 1) * 128
        for (ks, c0) in ((0, 0), (NOWN, n1)):
            for p0 in range(0, n1, 512):
                n = min(512, n1 - p0)
                for h in range(8):
                    pb = sctr[0] % 3
                    sctr[0] += 1
                    hp, hh = h // 2, h % 2
                    k.op("pe", lambda: T.matmul(
                        banks[pb][:, 0:n], iqT[hh * 64:(hh + 1) * 64, hp, i * 128:(i + 1) * 128],
                        kiT[hh * 64:(hh + 1) * 64, ks + p0:ks + p0 + n], start=True, stop=True),
                        reads=[IQ[hp][i // 4], KI[(ks + p0) // 512]], writes=[BK[pb]])
                    rb = rctr[0] % 2
                    rctr[0] += 1
                    dst = Ibuf[:, c0 + p0:c0 + p0 + n]
                    wcol = iw_sb[:, i, h:h + 1]
                    if h == 0:
                        k.op("dve", lambda: V.tensor_scalar(dst, banks[pb][:, 0:n], 0.0, wcol, ALU.max, ALU.mult),
                             reads=[BK[pb], IW], writes=[IB])
                    elif h != 4:
                        k.op("dve", lambda: V.tensor_scalar(RB[rb][:, 0:n], banks[pb][:, 0:n], 0.0, wcol, ALU.max, ALU.mult),
                             reads=[BK[pb], IW], writes=[RBB[rb]])
                        k.op("dve", lambda: V.tensor_tensor(dst, dst, RB[rb][:, 0:n], ALU.add),
                             reads=[RBB[rb], IB], writes=[IB])
                    else:
                        k.op("act", lambda: S.activation(RB[rb][:, 0:n], banks[pb][:, 0:n], AF.Relu),
                             reads=[BK[pb]], writes=[RBB[rb]])
                        k.op("dve", lambda: V.scalar_tensor_tensor(dst, RB[rb][:, 0:n], wcol, dst, ALU.mult, ALU.add),
                             reads=[RBB[rb], IW, IB], writes=[IB])
                    yield

    N_POST = NIT + 4

    def gen_post(i, g):
        Ibuf, IB = Ibufs[i % 2], IBS[i % 2]
        n1 = (i + 1) * 128
        j = i - 4 * g
        W2 = 2 * n1
        Iv = Ibuf[:, 0:W2]
        k.op("dve", lambda: V.tensor_reduce(stat[:, 100:101], Iv, AX.X, ALU.max, apply_absolute_value=True),
             reads=[IB, STAT], writes=[STAT])
        k.op("dve", lambda: V.tensor_tensor(Ibuf[:, i * 128:(i + 1) * 128], Ibuf[:, i * 128:(i + 1) * 128], trineg, ALU.add),
             reads=[IB, CONST], writes=[IB])
        k.op("dve", lambda: V.tensor_scalar(Ibuf[:, n1 + i * 128:n1 + (i + 1) * 128], Ibuf[:, n1 + i * 128:n1 + (i + 1) * 128],
                                            cc(C_NEGROLE), None, ALU.add), reads=[IB, CONST], writes=[IB])
        k.op("dve", lambda: V.tensor_reduce(stat[:, 101:102], Iv, AX.X, ALU.max), reads=[IB, STAT], writes=[STAT])
        yield
        k.op("dve", lambda: V.tensor_tensor(stat[:, 102:103], stat[:, 101:102], stat[:, 100:101], ALU.add),
             reads=[STAT], writes=[STAT])
        k.op("dve", lambda: V.memset(stat[:, 105:106], float(W2 - 2 * TOPK + 1)), reads=[STAT], writes=[STAT])
        k.op("dve", lambda: V.tensor_scalar(Hh[:, 0:NIT + 1], cc(C_POW2, NIT + 1), stat[:, 102:103], None, ALU.mult),
             reads=[STAT, CONST, BIS], writes=[BIS])
        k.op("dve", lambda: V.tensor_scalar(nH[:, 0:NIT + 1], cc(C_POW2, NIT + 1), stat[:, 102:103], -1.0, ALU.mult, ALU.mult),
             reads=[STAT, CONST, BIS], writes=[BIS])
        k.op("dve", lambda: V.tensor_tensor(nm[:, 0:1], stat[:, 100:101], Hh[:, 0:1], ALU.subtract),
             reads=[BIS, STAT], writes=[BIS])
        k.op("dve", lambda: V.memset(sacc, 0.0), reads=[BIS], writes=[BIS])
        yield
        for it in range(NIT):
            k.op("act", lambda: S.activation(JUNK[:, 0:W2], Iv, AF.Sign, bias=nm[:, it:it + 1], scale=1.0,
                                             accum_out=sacc[:, it:it + 1], saturate=False), reads=[IB, BIS, MB], writes=[MB, BIS])
            k.op("act", lambda: S.activation(uu[:, it:it + 1], sacc[:, it:it + 1], AF.Sign, bias=stat[:, 105:106], scale=1.0),
                 reads=[BIS, STAT], writes=[BIS])
            k.op("act", lambda: S.activation(nm[:, it + 1:it + 2], uu[:, it:it + 1], AF.Identity,
                                             bias=nm[:, it:it + 1], scale=nH[:, it + 1:it + 2]), reads=[BIS], writes=[BIS])
            yield
        k.op("dve", lambda: V.tensor_scalar(stat[:, 104:105], nm[:, NIT:NIT + 1], -1.0, Hh[:, NIT:NIT + 1], ALU.mult, ALU.subtract),
             reads=[BIS, STAT], writes=[STAT])
        for (c0, slot0) in ((0, 0), (n1, 4 * g + 4)):
            k.op("dve", lambda: V.tensor_scalar(Mh[:, 0:n1], Ibuf[:, c0:c0 + n1], stat[:, 104:105], None, ALU.is_ge),
                 reads=[IB, STAT, MB], writes=[MB])
            for cb in range(0, i + 1, 8):
                nb = min(8, i + 1 - cb)
                pb = 3 + (sctr[0] % 2)
                sctr[0] += 1
                psb = bank_bf(pb).rearrange("p (a b) -> p a b", b=128)

                def tps():
                    inst = None
                    for q in range(nb):
                        inst = T.transpose(psb[:, q, :], Mh[:, (cb + q) * 128:(cb + q + 1) * 128], ident)
                    return inst
                k.op("pe", tps, reads=[MB, CONST], writes=[BK[pb]])
                k.op("act", lambda: S.copy(MT[:, slot0 + cb:slot0 + cb + nb, j * 128:(j + 1) * 128], psb[:, 0:nb, :]),
                     reads=[BK[pb]], writes=[MTB])
            yield

    def interleave(ga, na, gb, nb_):
        ia = ib = 0
        da = db = False
        while not (da and db):
            if not da and (db or ia * max(nb_, 1) <= ib * max(na, 1)):
                try:
                    next(ga); ia += 1
                except StopIteration:
                    da = True
            elif not db:
                try:
                    next(gb); ib += 1
                except StopIteration:
                    db = True

    def attn_slots(g):
        r = []
        for c in range(4 * g + 4):
            r.append((c * 128, c, max(0, c - 4 * g), "own"))
        for c in range(4 * g + 4):
            r.append((NOWN + c * 128, 4 * g + 4 + c, max(0, c - 4 * g), "oth"))
        return r

    LAG = 2

    def dsa_attention(g):
        slots = attn_slots(g)
        items = [(p, si) for p in range(4) for si in range(len(slots))]
        st = {}

        def stage_a(t):
            p, si = items[t]
            ks, slot, j0, kind = slots[si]
            c0 = j0 * 128
            pbs = []
            for hh in range(2):
                pb = sctr[0] % 3
                sctr[0] += 1
                pbs.append(pb)

            def qk():
                inst = None
                for hh in range(2):
                    inst = T.matmul(banks[pbs[hh]][:, c0:512], kT[hh * 64:(hh + 1) * 64, p, ks:ks + 128],
                                    qT[hh * 64:(hh + 1) * 64, p, g * 512 + c0:(g + 1) * 512], start=True, stop=True)
                return inst
            k.op("pe", qk, reads=[KT[p][ks // 512], QT[p][g]], writes=[BK[pbs[0]], BK[pbs[1]]])
            ebs = []
            for hh in range(2):
                eb = ectr[0] % NEB
                ectr[0] += 1
                ebs.append(eb)
                k.op("act", lambda: S.activation(EB[eb][:, c0:512], banks[pbs[hh]][:, c0:512], AF.Exp, scale=0.125),
                     reads=[BK[pbs[hh]]], writes=[EBB[eb]])
                k.op("dve", lambda: V.tensor_tensor(EB[eb][:, c0:512], EB[eb][:, c0:512], MT[:, slot, c0:512], ALU.mult),
                     reads=[EBB[eb], MTB], writes=[EBB[eb]])
            st[t] = ebs

        def stage_b(t):
            p, si = items[t]
            ks, slot, j0, kind = slots[si]
            c0 = j0 * 128
            kc = ks // 128
            ebs = st.pop(t)
            ob, sbk = (4, 5) if p % 2 == 0 else (6, 7)
            first, last = (si == 0), (si == len(slots) - 1)

            def pv():
                inst = None
                for hh in range(2):
                    T.matmul(banks[ob][hh * 64:(hh + 1) * 64, c0:512], vtm[:, kc, (2 * p + hh) * 64:(2 * p + hh + 1) * 64],
                             EB[ebs[hh]][:, c0:512], start=first, stop=last)
                for hh in range(2):
                    inst = T.matmul(banks[sbk][hh * 64:(hh + 1) * 64, c0:512], ones_bf[:, 0:64],
                                    EB[ebs[hh]][:, c0:512], start=first, stop=last)
                return inst
            k.op("pe", pv, reads=[EBB[ebs[0]], EBB[ebs[1]], VT[kc], CONST], writes=[BK[ob], BK[sbk]])
            if last:
                rc = p % 2
                k.op("dve", lambda: V.reciprocal(REC[rc], banks[sbk]), reads=[BK[sbk]], writes=[RECB[rc]])
                k.op("dve", lambda: V.tensor_tensor(mixT[:, p, g * 512:(g + 1) * 512], banks[ob], REC[rc], ALU.mult),
                     reads=[BK[ob], RECB[rc]], writes=[MIX[p]])
        n = len(items)
        for t in range(n + LAG):
            if t < n:
                stage_a(t)
            if t >= LAG:
                stage_b(t - LAG)
            yield

    def run(gen):
        for _ in gen:
            pass

    run(gen_icomp(0))
    for i in range(16):
        g = i // 4
        if i + 1 < 16:
            interleave(gen_post(i, g), N_POST, gen_icomp(i + 1), n_icomp(i + 1))
        else:
            run(gen_post(i, g))
        if i % 4 == 3:
            run(dsa_attention(g))
            if stage == 2 and g == 1:
                dump("d_MT", MT, [MTB], [128, 32, 512], BF16)
                dump("d_mix", mixT, MIX, [128, 8, NOWN], BF16)
                k.finish("sp")
                return nc, dbg_d
    k.barrier()

    make_tables()
    build_hT()
    k.barrier()
    for j in range(4):
        proj_rope(13 + j, lambda tg, j=j: kT[:, j, tg * 512:(tg + 1) * 512], KT[j], list(range(8)))
    k.barrier()
    proj_v(512)
    k.barrier()
    for j in range(4):
        proj_rope(17 + j, lambda tg, j=j: qT[:, j, tg * 512:(tg + 1) * 512], QT[j], list(range(4)))
    k.barrier()

    FT = [sb(O_HT + i * 2 * KB, [512], F32) for i in range(8)]
    FTB = [Buf() for _ in range(8)]
    EB = [sb(O_HT + 16 * KB + i * KB, [512], BF16) for i in range(6)]

    EACC4 = [[sb(O_HT + 24 * KB + (2 * hp + i) * 2 * KB, [512], F32) for i in range(2)] for hp in range(2)]
    EACCB4 = [[Buf(), Buf()], [Buf(), Buf()]]

    def diff_attention(g):
        slots = attn_slots(g)
        items = [(h, si) for h in range(4) for si in range(len(slots))]
        st = {}

        def stage_a(t):
            h, si = items[t]
            EACC, EACCB = EACC4[h % 2], EACCB4[h % 2]
            ks, slot, j0, kind = slots[si]
            c0 = j0 * 128
            c_abs = (ks % NOWN) // 128
            pbs = []
            for c in range(2):
                pbs.append(sctr[0] % 3)
                sctr[0] += 1

            def qk():
                inst = None
                for c in range(2):
                    inst = T.matmul(banks[pbs[c]][:, c0:512], kT[c * 64:(c + 1) * 64, h, ks:ks + 128],
                                    qT[c * 64:(c + 1) * 64, h, g * 512 + c0:(g + 1) * 512], start=True, stop=True)
                return inst
            k.op("pe", qk, reads=[KT[h][ks // 512], QT[h][g]], writes=[BK[pbs[0]], BK[pbs[1]]])
            ebs = []
            for c in range(2):
                eb = ectr[0] % NEB
                ectr[0] += 1
                ebs.append(eb)
                k.op("act", lambda: S.activation(EB[eb][:, c0:512], banks[pbs[c]][:, c0:512], AF.Exp, scale=0.125),
                     reads=[BK[pbs[c]]], writes=[EBB[eb]])
                if c_abs >= 4 * g:
                    blk = EB[eb][:, c0:c0 + 128]
                    if kind == "own":
                        k.op("dve", lambda: V.tensor_tensor(blk, blk, triT, ALU.mult), reads=[EBB[eb], CONST], writes=[EBB[eb]])
                    else:
                        k.op("dve", lambda: V.tensor_scalar(blk, blk, cc(C_ROLE), None, ALU.mult), reads=[EBB[eb], CONST], writes=[EBB[eb]])
                if si == 0:
                    k.op("dve", lambda: V.tensor_copy(EACC[c], EB[eb]), reads=[EBB[eb]], writes=[EACCB[c]])
                else:
                    k.op("dve", lambda: V.tensor_tensor(EACC[c][:, c0:512], EACC[c][:, c0:512], EB[eb][:, c0:512], ALU.add),
                         reads=[EBB[eb], EACCB[c]], writes=[EACCB[c]])
            st[t] = ebs

        def stage_b(t):
            h, si = items[t]
            ks, slot, j0, kind = slots[si]
            c0 = j0 * 128
            kc = ks // 128
            ebs = st.pop(t)
            EACC, EACCB = EACC4[h % 2], EACCB4[h % 2]
            first, last = (si == 0), (si == len(slots) - 1)

            def pv():
                inst = None
                for c in range(2):
                    inst = T.matmul(banks[3 + c][:, c0:512], vtm[:, kc, h * 128:(h + 1) * 128], EB[ebs[c]][:, c0:512],
                                    start=first, stop=last)
                return inst
            k.op("pe", pv, reads=[EBB[ebs[0]], EBB[ebs[1]], VT[kc], CONST], writes=[BK[3], BK[4]])
            if not last:
                return
            def sm():
                T.matmul(banks[5], ones_f, EACC[0], start=True, stop=True)
                return T.matmul(banks[6], ones_f, EACC[1], start=True, stop=True)
            k.op("pe", sm, reads=[EACCB[0], EACCB[1], CONST], writes=[BK[5], BK[6]])
            k.op("dve", lambda: V.reciprocal(FT[0], banks[5]), reads=[BK[5]], writes=[FTB[0]])
            k.op("dve", lambda: V.reciprocal(FT[1], banks[6]), reads=[BK[6]], writes=[FTB[1]])
            k.op("dve", lambda: V.scalar_tensor_tensor(FT[2], banks[3], 1.0 / 128.0, FT[0], ALU.mult, ALU.mult), reads=[BK[3], FTB[0]], writes=[FTB[2]])
            k.op("dve", lambda: V.scalar_tensor_tensor(FT[3], banks[4], 1.0 / 128.0, FT[1], ALU.mult, ALU.mult), reads=[BK[4], FTB[1]], writes=[FTB[3]])
            k.op("dve", lambda: V.scalar_tensor_tensor(FT[4], FT[3], neglam, FT[2], ALU.mult, ALU.add),
                 reads=[FTB[3], FTB[2], LAMB], writes=[FTB[4]])
            k.op("act", lambda: S.activation(FT[5], FT[4], AF.Square), reads=[FTB[4]], writes=[FTB[5]])
            k.op("pe", lambda: T.matmul(banks[7], ones_f, FT[5], start=True, stop=True), reads=[FTB[5], CONST], writes=[BK[7]])
            k.op("act", lambda: S.activation(FT[6], banks[7], AF.Sqrt, bias=cc(C_EPS), scale=1.0), reads=[BK[7], CONST], writes=[FTB[6]])
            k.op("dve", lambda: V.reciprocal(FT[6], FT[6]), reads=[FTB[6]], writes=[FTB[6]])
            k.op("dve", lambda: V.scalar_tensor_tensor(mixT[:, 4 + h, g * 512:(g + 1) * 512], FT[4], cc(C_SUBW), FT[6],
                                                       ALU.mult, ALU.mult), reads=[FTB[4], FTB[6], CONST], writes=[MIX[4 + h]])
        n = len(items)
        for t in range(n + LAG):
            if t < n:
                stage_a(t)
            if t >= LAG:
                stage_b(t - LAG)
            yield

    for g in range(4):
        run(diff_attention(g))
    if stage == 3:
        dump("d_mix", mixT, MIX, [128, 8, NOWN], BF16)
        k.finish("sp")
        return nc, dbg_d
    k.barrier()

    O_WO = 38 * KB
    O_X1 = 54 * KB
    O_H2 = 118 * KB
    O_C = 150 * KB
    wout = sb(O_WO, [8, D], BF16)
    x1 = sb(O_X1, [16, D], F32)
    h2T = sb(O_H2, [8, NOWN], BF16)
    gbc = [sb(O_C + i * 4 * KB, [D], F32) for i in range(2)]
    nfw = sb(O_C + 8 * KB, [D], F32)
    WO, GBC, NFW = Buf(), Buf(), Buf()
    X1 = [Buf() for _ in range(16)]
    H2 = [[Buf(), Buf()] for _ in range(16)]
    k.dma("pool", wout, wout_d.rearrange("(c p) n -> p c n", p=128), writes=[WO])
    k.dma("sp", nfw, nfw_d.partition_broadcast(128), writes=[NFW])
    dg = sb(O_C + 12 * KB, [128], F32)
    DG = Buf()
    onesf1 = sb(O_C + 13 * KB, [128], F32)
    k.op("dve", lambda: V.memset(onesf1, 1.0), writes=[DG])
    for which, base in ((0, 16), (1, 40)):
        for half in range(2):
            for jj in range(4):
                j = half * 4 + jj
                k.op("dve", lambda j=j, base=base: V.tensor_scalar(dg, identf, modT[:, base + j:base + j + 1], None, ALU.mult),
                     reads=[CONST, DG], writes=[DG])
                k.op("pe", lambda jj=jj: T.matmul(banks[0][:, jj * 128:(jj + 1) * 128], onesf1, dg, start=True, stop=True),
                     reads=[DG], writes=[BK[0]])
            k.op("act", lambda which=which, half=half: S.copy(gbc[which][:, half * 512:(half + 1) * 512], banks[0]),
                 reads=[BK[0]], writes=[GBC])

    xt2 = [sb(O_C + 16 * KB + i * 4 * KB, [D], F32) for i in range(2)]
    tmpc = [sb(O_C + 24 * KB + i * 4 * KB, [D], F32) for i in range(2)]
    xn2 = [sb(O_C + 32 * KB + i * 2 * KB, [D], BF16) for i in range(2)]
    junkc = sb(O_C + 36 * KB, [D], F32)
    XT2, TMPC, XN2, JKC = [Buf(), Buf()], [Buf(), Buf()], [Buf(), Buf()], Buf()
    k.op("dve", lambda: V.memset(stat, 0.0), reads=[STAT], writes=[STAT])
    def c_stage_a(i):
        b = i % 2
        k.dma("sp", xt2[b], x_d[i * 128:(i + 1) * 128, :], writes=[XT2[b]])
        for half in range(2):
            pb = 2 * b + half

            def mmo():
                inst = None
                for ch in range(8):
                    inst = T.matmul(banks[pb], mixT[:, ch, i * 128:(i + 1) * 128], wout[:, ch, half * 512:(half + 1) * 512],
                                    start=(ch == 0), stop=(ch == 7))
                return inst
            k.op("pe", mmo, reads=MIX + [WO], writes=[BK[pb]])
            k.op("dve", lambda: V.tensor_tensor(tmpc[b][:, half * 512:(half + 1) * 512], banks[pb],
                                                gbc[0][:, half * 512:(half + 1) * 512], ALU.mult),
                 reads=[BK[pb], GBC], writes=[TMPC[b]])
        k.op("pool", lambda: G.tensor_tensor(x1[:, i, :], tmpc[b], xt2[b], ALU.add), reads=[TMPC[b], XT2[b]], writes=[X1[i]])
        k.op("act", lambda: S.activation(junkc, x1[:, i, :], AF.Square, accum_out=stat[:, i:i + 1]),
             reads=[X1[i], STAT], writes=[JKC, STAT])
        k.op("act", lambda: S.activation(stat[:, 32 + i:33 + i], stat[:, i:i + 1], AF.Sqrt, bias=cc(C_EPS), scale=1.0 / D),
             reads=[STAT, CONST], writes=[STAT])

    def c_stage_a2(i):
        b = i % 2
        k.op("dve", lambda: V.reciprocal(stat[:, 64 + i:65 + i], stat[:, 32 + i:33 + i]), reads=[STAT], writes=[STAT])
        k.op("dve", lambda: V.tensor_scalar(xn2[b], x1[:, i, :], stat[:, 64 + i:65 + i], None, ALU.mult),
             reads=[X1[i], STAT], writes=[XN2[b]])

    def c_stage_b(i):
        b = i % 2
        pbs = (4 + 2 * b, 5 + 2 * b)
        psbs = [bank_bf(pb_).rearrange("p (a b) -> p a b", b=128) for pb_ in pbs]

        def tps():
            inst = None
            for kc in range(8):
                inst = T.transpose(psbs[kc // 4][:, kc % 4, :], xn2[b][:, kc * 128:(kc + 1) * 128], ident)
            return inst
        k.op("pe", tps, reads=[XN2[b], CONST], writes=[BK[pbs[0]], BK[pbs[1]]])
        for kc in range(8):
            dst = h2T[:, kc, i * 128:(i + 1) * 128]
            src = psbs[kc // 4][:, kc % 4, :]
            if kc < 4:
                k.op("act", lambda: S.activation(dst, src, AF.Identity, bias=GB[:, 24 + kc:25 + kc], scale=GB[:, 16 + kc:17 + kc]),
                     reads=[BK[pbs[0]], CONST], writes=[H2[i][0]])
            else:
                k.op("dve", lambda: V.tensor_scalar(dst, src, GB[:, 16 + kc:17 + kc], GB[:, 24 + kc:25 + kc], ALU.mult, ALU.add),
                     reads=[BK[pbs[1]], CONST], writes=[H2[i][1]])
    for t in range(17):
        if t < 16:
            c_stage_a(t)
        if t >= 1:
            c_stage_b(t - 1)
        if t < 16:
            c_stage_a2(t)
    if stage == 4:
        dump("d_x1", x1, X1, [128, 16, D], F32)
        dump("d_h2T", h2T, sum(H2, []), [128, 8, NOWN], BF16)
        dump("d_gbc", gbc[0], [GBC], [128, D], F32)
        k.finish("sp")
        return nc, dbg_d
    k.barrier()

    O_HID = 150 * KB + 16 * KB
    hid = sb(O_HID, [32, 512], BF16)
    HID = [Buf() for _ in range(32)]
    w1b = [sb(O_MIX + i * 8 * KB, [8, 512], BF16) for i in range(2)]
    w2b = [sb(O_MIX + 16 * KB + i * 8 * KB, [4, D], BF16) for i in range(2)]
    W1B, W2B = [Buf(), Buf()], [Buf(), Buf()]
    sqb = [sb(O_WO + i * 2 * KB, [512], F32) for i in range(2)]
    SQB = [Buf(), Buf()]
    tmpd = [sb(O_WO + 4 * KB + i * 4 * KB, [D], F32) for i in range(2)] + [sb(198 * KB + i * 4 * KB, [D], F32) for i in range(2)]
    TMPD = [Buf(), Buf(), Buf(), Buf()]
    junkd = sb(O_WO + 12 * KB, [D], F32)
    JKD = Buf()
    OT = [Buf(), Buf()]
    ot = [sb(O_C + 12 * KB, [D], F32), sb(O_C + 0 * KB, [D], F32)]
    w1v = wff1_d
    w2v = wff2_d.rearrange("(fb f p) c -> fb p f c", f=4, p=128)
    k.op("dve", lambda: V.memset(stat, 0.0), reads=[STAT], writes=[STAT])
    wctr = [0, 0]
    for g in range(4):
        for fb in range(8):
            wi = wctr[0] % 2
            wctr[0] += 1
            k.dma("pool", w1b[wi], w1v[fb].rearrange("p (a b) -> p a b", b=512), writes=[W1B[wi]])
            for f4 in range(4):
                fc = fb * 4 + f4
                pb = fc % 4

                def mm1(wi=wi, f4=f4, pb=pb, g=g):
                    inst = None
                    for kc in range(8):
                        inst = T.matmul(banks[pb], w1b[wi][:, kc, f4 * 128:(f4 + 1) * 128], h2T[:, kc, g * 512:(g + 1) * 512],
                                        start=(kc == 0), stop=(kc == 7))
                    return inst
                k.op("pe", mm1, reads=[W1B[wi]] + sum(H2[4 * g:4 * g + 4], []), writes=[BK[pb]])
                sq = fc % 2
                k.op("act", lambda sq=sq, pb=pb: S.activation(sqb[sq], banks[pb], AF.Relu), reads=[BK[pb]], writes=[SQB[sq]])
                k.op("dve", lambda sq=sq, fc=fc: V.tensor_tensor(hid[:, fc, :], sqb[sq], sqb[sq], ALU.mult),
                     reads=[SQB[sq]], writes=[HID[fc]])
        for fb in range(8):
            wi = wctr[1] % 2
            wctr[1] += 1
            k.dma("pool", w2b[wi], w2v[fb], writes=[W2B[wi]])

            def mm2(wi=wi, fb=fb):
                inst = None
                for f4 in range(4):
                    fc = fb * 4 + f4
                    for t in range(4):
                        for half in range(2):
                            inst = T.matmul(banks[t * 2 + half], hid[:, fc, t * 128:(t + 1) * 128],
                                            w2b[wi][:, f4, half * 512:(half + 1) * 512], start=(fc == 0), stop=(fc == 31))
                return inst
            k.op("pe", mm2, reads=[W2B[wi]] + HID[fb * 4:fb * 4 + 4], writes=BK)
        for t in range(4):
            for half in range(2):
                pb = t * 2 + half
                k.op("dve", lambda pb=pb, half=half, t=t: V.tensor_tensor(tmpd[t][:, half * 512:(half + 1) * 512], banks[pb],
                                                                         gbc[1][:, half * 512:(half + 1) * 512], ALU.mult),
                     reads=[BK[pb], GBC], writes=[TMPD[t]])
        for t in range(4):
            i = 4 * g + t
            b = i % 2
            k.op("pool", lambda i=i, t=t: G.tensor_tensor(tmpd[t], tmpd[t], x1[:, i, :], ALU.add), reads=[TMPD[t], X1[i]], writes=[TMPD[t]])
            k.op("act", lambda i=i, t=t: S.activation(junkd, tmpd[t], AF.Square, accum_out=stat[:, i:i + 1]),
                 reads=[TMPD[t], STAT], writes=[JKD, STAT])
            k.op("act", lambda i=i: S.activation(stat[:, 32 + i:33 + i], stat[:, i:i + 1], AF.Sqrt, bias=cc(C_EPS), scale=1.0 / D),
                 reads=[STAT, CONST], writes=[STAT])
            k.op("dve", lambda i=i: V.reciprocal(stat[:, 64 + i:65 + i], stat[:, 32 + i:33 + i]), reads=[STAT], writes=[STAT])
            k.op("dve", lambda i=i, b=b, t=t: V.scalar_tensor_tensor(ot[b], tmpd[t], stat[:, 64 + i:65 + i], nfw, ALU.mult, ALU.mult),
                 reads=[TMPD[t], STAT, NFW], writes=[OT[b]])
            k.dma("sp", out_d[i * 128:(i + 1) * 128, :], ot[b], reads=[OT[b]])
    k.finish("sp")
    return nc, dbg_d


def _partner_perm(n):
    idx = np.arange(n)
    return np.where((idx % 64) < 32, idx + 32, idx - 32)


def _chunked(w):
    n = w.shape[1] // 128
    return np.ascontiguousarray(w.reshape(8, 128, n, 128).transpose(2, 1, 0, 3).reshape(n, 128, 1024))


def prepare_inputs(x, c, positions, w_ada, b_ada, norm1_w, w_in, idx_k_ln_w, idx_k_ln_b,
                   lambda_q1, lambda_k1, lambda_q2, lambda_k2, subln_w, w_out, norm2_w,
                   w_ff1, w_ff2, norm_f_w):
    f32 = np.float32
    w_in = np.asarray(w_in[0], f32)
    aq, ak, av = w_in[:, 0:512], w_in[:, 512:1024], w_in[:, 1024:1536]
    iq, ik, iw = w_in[:, 1536:2048], w_in[:, 2048:2112], w_in[:, 2112:2120]
    dq, dk, dv = w_in[:, 2120:2632], w_in[:, 2632:3144], w_in[:, 3144:3656]
    ikd = np.concatenate([ik, ik], axis=1)
    fm = np.concatenate([ak, ikd, aq, iq, dk, dq], axis=1)
    fm_rot = fm[:, _partner_perm(fm.shape[1])]
    w_fm = _chunked(fm)
    w_fm_rot = _chunked(fm_rot)
    w_tm = np.ascontiguousarray(np.concatenate([av, dv, iw], axis=1))
    wff1 = np.asarray(w_ff1[0], f32)
    wff1_l = np.ascontiguousarray(wff1.reshape(8, 128, 8, 512).transpose(2, 1, 0, 3).reshape(8, 128, 4096))
    p = np.arange(128)
    consts = np.zeros((128, NCONST), f32)
    inv_freq = (10000.0 ** (-np.arange(32, dtype=f32) / 32)).astype(f32)
    consts[:, C_INVF] = inv_freq[p % 32]
    consts[:, C_SGN] = np.where((p % 64) < 32, -1.0, 1.0)
    consts[:, C_EPS] = NORM_EPS
    consts[:, C_LNEPS] = LN_EPS
    consts[:, C_ONE] = 1.0
    consts[:, C_POW2:C_POW2 + NIT + 1] = (0.5 ** np.arange(1, NIT + 2))[None, :]
    lnw = np.asarray(idx_k_ln_w[0], f32)
    lnb = np.asarray(idx_k_ln_b[0], f32)
    pp = _partner_perm(64)
    consts[:, C_LNP + 0] = lnw[p % 64]
    consts[:, C_LNP + 1] = lnb[p % 64]
    consts[:, C_LNP + 2] = lnw[pp][p % 64]
    consts[:, C_LNP + 3] = lnb[pp][p % 64]
    consts[:, C_SUBW] = np.asarray(subln_w[0], f32) * f32(1.0 - LAM_INIT)
    consts[:, C_N1:C_N1 + 8] = np.asarray(norm1_w[0], f32).reshape(8, 128).T
    consts[:, C_N2:C_N2 + 8] = np.asarray(norm2_w[0], f32).reshape(8, 128).T
    cmat = np.zeros((128, 3, 128), f32)
    cmat[:, 0, :] = np.eye(128)
    cmat[:, 1, :] = (p[:, None] <= p[None, :])
    cmat[:, 2, :] = np.where(p[None, :] <= p[:, None], 0.0, NEG)
    lamv = np.concatenate([lambda_q1[0], lambda_k1[0], lambda_q2[0], lambda_k2[0]]).astype(f32)[None, :]
    shared = {
        "w_ada": np.ascontiguousarray(w_ada[0], dtype=f32),
        "b_adaT": np.ascontiguousarray(np.asarray(b_ada[0], f32).reshape(48, 128).T),
        "w_fm": w_fm, "w_fm_rot": w_fm_rot, "w_tm": w_tm,
        "w_out": np.ascontiguousarray(w_out[0], dtype=f32),
        "w_ff1": wff1_l, "w_ff2": np.ascontiguousarray(w_ff2[0], dtype=f32),
        "norm_f_w": np.asarray(norm_f_w, f32)[None, :],
        "cmat": cmat, "lamv": lamv,
    }
    in_maps = []
    perms = []
    for core in range(8):
        b, r = core // 2, core % 2
        blocks = np.concatenate([2 * np.arange(16) + r, 2 * np.arange(16) + 1 - r])
        tok = (blocks[:, None] * 128 + np.arange(128)[None, :]).reshape(-1)
        perms.append(tok[:NOWN])
        cst = consts.copy()
        cst[:, C_ROLE] = float(r)
        cst[:, C_NEGROLE] = 0.0 if r == 1 else NEG
        m = dict(shared)
        m["x"] = np.ascontiguousarray(np.asarray(x[b], f32)[tok])
        m["pos"] = np.ascontiguousarray(np.asarray(positions[b], np.int32)[tok])[None, :]
        m["cT"] = np.ascontiguousarray(np.asarray(c[b], f32).reshape(8, 128).T)
        m["consts"] = cst
        in_maps.append(m)
    return in_maps, perms


_CACHE = {}


def kernel(**inputs):
    in_maps, perms = prepare_inputs(**{k_: np.asarray(v) for k_, v in inputs.items()})
    if "nc" not in _CACHE:
        _CACHE["nc"] = build_program()[0]
    res = run_bass_kernel_spmd(_CACHE["nc"], in_maps, core_ids=list(range(8)))
    out = np.zeros((4, L, D), np.float32)
    for core in range(8):
        out[core // 2, perms[core]] = res.results[core]["out"]
    return out
```
